# Optimizing a Trainium2 kernel written in Bass

```python
import jax, jax.numpy as jnp
from jax import lax
import numpy as np

D_MODEL = 4096
BATCH = 8
SEQ = 2048
DEPTH = 2

HEAD_DIM = 128
D_MIX = D_MODEL
N_MIXERS = 4
GROUP_WIDTH = D_MIX // N_MIXERS
N_HEADS = GROUP_WIDTH // HEAD_DIM

MOBA_BLOCK = 256
MOBA_TOPK = 3
MOBA_Q_CHUNK = 16

SGU_CHUNK = 128

Q_LORA_RANK = 768
KV_LORA_RANK = 256
QK_NOPE_DIM = 128
QK_ROPE_DIM = 64
V_HEAD_DIM = 128
ROPE_THETA = 10000.0

Q_BLOCK = 128

D_FF = 11008
CONV_WIDTH = 3

NORM_EPS = 1e-6
NEG_INF = -1e30

SPLIT_SIZES = (GROUP_WIDTH, GROUP_WIDTH, GROUP_WIDTH,
               GROUP_WIDTH, GROUP_WIDTH,
               Q_LORA_RANK, KV_LORA_RANK, QK_ROPE_DIM,
               GROUP_WIDTH, GROUP_WIDTH, GROUP_WIDTH, N_HEADS)
IN_COLS = sum(SPLIT_SIZES)

kernel_name = "hymba_style_moba_gmlp_mla_fox_convffn"


def split_points():
    return [int(s) for s in np.cumsum(SPLIT_SIZES)[:-1]]


def rmsnorm(x, g):
    xf = x.astype(jnp.float32)
    y = xf * lax.rsqrt(jnp.mean(xf * xf, axis=-1, keepdims=True) + NORM_EPS)
    return (y * g.astype(jnp.float32)).astype(x.dtype)


def layernorm(x, g, b):
    xf = x.astype(jnp.float32)
    xc = xf - jnp.mean(xf, axis=-1, keepdims=True)
    var = jnp.mean(xc * xc, axis=-1, keepdims=True)
    y = xc * lax.rsqrt(var + NORM_EPS) * g.astype(jnp.float32) + b.astype(jnp.float32)
    return y.astype(x.dtype)


def alibi_slopes(n):
    return jnp.exp2(-8.0 * jnp.arange(1, n + 1, dtype=jnp.float32) / n)


def apply_rope(x, pos):
    half = x.shape[-1] // 2
    inv_freq = ROPE_THETA ** (-jnp.arange(half, dtype=jnp.float32) / half)
    ang = pos.astype(jnp.float32)[:, None] * inv_freq[None, :]
    cos, sin = jnp.cos(ang), jnp.sin(ang)
    xf = x.astype(jnp.float32)
    x1, x2 = xf[..., :half], xf[..., half:]
    return jnp.concatenate([x1 * cos - x2 * sin, x2 * cos + x1 * sin], axis=-1).astype(x.dtype)


def to_heads(t, n_heads):
    b, s, w = t.shape
    return t.reshape(b, s, n_heads, w // n_heads).transpose(0, 2, 1, 3)


def merge_heads(t):
    b, h, s, d = t.shape
    return t.transpose(0, 2, 1, 3).reshape(b, s, h * d)


def causal_block_attention(q, k, v, scale, cum_log_f=None):
    S = q.shape[2]
    outs = []
    for i in range(S // Q_BLOCK):
        lo, hi = i * Q_BLOCK, (i + 1) * Q_BLOCK
        s = jnp.einsum("bhqd,bhkd->bhqk", q[:, :, lo:hi], k[:, :, :hi]).astype(jnp.float32) * scale
        if cum_log_f is not None:
            s = s + cum_log_f[:, :, lo:hi, None] - cum_log_f[:, :, None, :hi]
        t_pos = jnp.arange(lo, hi)[:, None]
        s_pos = jnp.arange(hi)[None, :]
        s = jnp.where(s_pos <= t_pos, s, NEG_INF)
        p = jax.nn.softmax(s, axis=-1).astype(v.dtype)
        outs.append(jnp.einsum("bhqk,bhkd->bhqd", p, v[:, :, :hi]))
    return jnp.concatenate(outs, axis=2)


def moba_attention(q, k, v, slopes):
    B, H, S, Dh = q.shape
    n_blk = -(-S // MOBA_BLOCK)
    pad = n_blk * MOBA_BLOCK - S
    k_blk = jnp.pad(k, ((0, 0), (0, 0), (0, pad), (0, 0))).reshape(B, H, n_blk, MOBA_BLOCK, Dh)
    v_blk = jnp.pad(v, ((0, 0), (0, 0), (0, pad), (0, 0))).reshape(B, H, n_blk, MOBA_BLOCK, Dh)
    k_mean = jnp.mean(k_blk.astype(jnp.float32), axis=3)
    top_k = min(MOBA_TOPK, max(n_blk - 1, 1))
    scale = Dh ** -0.5
    b_ix = jnp.arange(B)[:, None, None, None]
    h_ix = jnp.arange(H)[None, :, None, None]
    offs = jnp.arange(MOBA_BLOCK)
    m = slopes[None, :, None, None]

    def one_chunk(start):
        blk = start // MOBA_BLOCK
        q_c = lax.dynamic_slice_in_dim(q, start, MOBA_Q_CHUNK, axis=2)
        t = start + jnp.arange(MOBA_Q_CHUNK)
        k_own = lax.dynamic_index_in_dim(k_blk, blk, axis=2, keepdims=False)
        v_own = lax.dynamic_index_in_dim(v_blk, blk, axis=2, keepdims=False)
        own_pos = blk * MOBA_BLOCK + offs
        s_own = (jnp.einsum("bhqd,bhkd->bhqk", q_c, k_own).astype(jnp.float32) * scale
                 - m * jnp.abs(t[:, None] - own_pos[None, :]).astype(jnp.float32))
        s_own = jnp.where(own_pos[None, :] <= t[:, None], s_own, NEG_INF)
        gate = jnp.einsum("bhqd,bhnd->bhqn", q_c.astype(jnp.float32), k_mean)
        gate = jnp.where(jnp.arange(n_blk) < blk, gate, -jnp.inf)
        _, idx = lax.top_k(gate, top_k)
        k_sel = k_blk[b_ix, h_ix, idx]
        v_sel = v_blk[b_ix, h_ix, idx]
        sel_pos = idx[..., None] * MOBA_BLOCK + offs
        s_sel = (jnp.einsum("bhqd,bhqnkd->bhqnk", q_c, k_sel).astype(jnp.float32) * scale
                 - m[..., None] * jnp.abs(t[:, None, None] - sel_pos).astype(jnp.float32))
        s_sel = jnp.where((idx < blk)[..., None], s_sel, NEG_INF)
        logits = jnp.concatenate(
            [s_own, s_sel.reshape(B, H, MOBA_Q_CHUNK, top_k * MOBA_BLOCK)], axis=-1)
        p = jax.nn.softmax(logits, axis=-1).astype(v.dtype)
        p_own = p[..., :MOBA_BLOCK]
        p_sel = p[..., MOBA_BLOCK:].reshape(B, H, MOBA_Q_CHUNK, top_k, MOBA_BLOCK)
        return (jnp.einsum("bhqk,bhkd->bhqd", p_own, v_own)
                + jnp.einsum("bhqnk,bhqnkd->bhqd", p_sel, v_sel))

    starts = jnp.arange(S // MOBA_Q_CHUNK, dtype=jnp.int32) * MOBA_Q_CHUNK
    outs = lax.map(one_chunk, starts)
    return outs.transpose(1, 2, 0, 3, 4).reshape(B, H, S, Dh)


def chunked_spatial_gating(u, v, ln_g, ln_b, w_s, b_s):
    B, S, _ = v.shape
    vg = layernorm(v.reshape(B, S, N_HEADS, HEAD_DIM), ln_g, ln_b)
    vc = vg.reshape(B, S // SGU_CHUNK, SGU_CHUNK, N_HEADS, HEAD_DIM)
    causal = jnp.tril(jnp.ones((SGU_CHUNK, SGU_CHUNK), dtype=bool))
    w = jnp.where(causal[None], w_s, jnp.zeros_like(w_s))
    mixed = jnp.einsum("gts,bcsgd->bctgd", w, vc) + b_s.T[:, :, None]
    return u * mixed.reshape(B, S, N_HEADS * HEAD_DIM)


def latent_attention(c_q, c_kv, k_rope_raw, pos, q_norm_g, kv_norm_g, w_uq, w_ukv):
    B, S, _ = c_q.shape
    q = to_heads(rmsnorm(c_q, q_norm_g) @ w_uq, N_HEADS)
    q_nope, q_rope = q[..., :QK_NOPE_DIM], q[..., QK_NOPE_DIM:]
    kv = to_heads(rmsnorm(c_kv, kv_norm_g) @ w_ukv, N_HEADS)
    k_nope, v = kv[..., :QK_NOPE_DIM], kv[..., QK_NOPE_DIM:]
    q_rope = apply_rope(q_rope, pos)
    k_rope = apply_rope(k_rope_raw[:, None], pos)
    q_full = jnp.concatenate([q_nope, q_rope], axis=-1)
    k_full = jnp.concatenate(
        [k_nope, jnp.broadcast_to(k_rope, (B, N_HEADS, S, QK_ROPE_DIM))], axis=-1)
    return causal_block_attention(q_full, k_full, v, (QK_NOPE_DIM + QK_ROPE_DIM) ** -0.5)


def forgetting_attention(q, k, v, f_logit, b_f):
    log_f = jax.nn.log_sigmoid(f_logit.astype(jnp.float32) + b_f.astype(jnp.float32))
    cum = jnp.cumsum(log_f, axis=1).transpose(0, 2, 1)
    return causal_block_attention(to_heads(q, N_HEADS), to_heads(k, N_HEADS),
                                  to_heads(v, N_HEADS), HEAD_DIM ** -0.5, cum)


def causal_depthwise_conv(g, w, b):
    S = g.shape[1]
    gp = jnp.pad(g, ((0, 0), (CONV_WIDTH - 1, 0), (0, 0)))
    y = b
    for j in range(CONV_WIDTH):
        y = y + w[j] * gp[:, j:j + S]
    return y


def hybrid_layer(x, pos, slopes, norm_mix_g, w_in, sgu_ln_g, sgu_ln_b, sgu_w, sgu_b,
                 mla_q_norm_g, mla_kv_norm_g, mla_w_uq, mla_w_ukv, fox_b_f, group_norm_g,
                 w_o, norm_ffn_g, w_gate, w_val, conv_w, conv_b, w_down):
    h = rmsnorm(x, norm_mix_g)
    proj = h @ w_in
    (a_q, a_k, a_v, b_u, b_v, c_q, c_kv, c_kr,
     d_q, d_k, d_v, d_f) = jnp.split(proj, split_points(), axis=-1)
    y_a = merge_heads(moba_attention(to_heads(a_q, N_HEADS), to_heads(a_k, N_HEADS),
                                     to_heads(a_v, N_HEADS), slopes))
    y_b = chunked_spatial_gating(jax.nn.gelu(b_u), jax.nn.gelu(b_v),
                                 sgu_ln_g, sgu_ln_b, sgu_w, sgu_b)
    y_c = merge_heads(latent_attention(c_q, c_kv, c_kr, pos, mla_q_norm_g, mla_kv_norm_g,
                                       mla_w_uq, mla_w_ukv))
    y_d = merge_heads(forgetting_attention(d_q, d_k, d_v, d_f, fox_b_f))
    groups = [y_a, y_b, y_c, y_d]
    y = jnp.concatenate([rmsnorm(g, group_norm_g[i]) for i, g in enumerate(groups)], axis=-1)
    x = x + y @ w_o
    h = rmsnorm(x, norm_ffn_g)
    gate = causal_depthwise_conv(h @ w_gate, conv_w, conv_b)
    x = x + (jax.nn.silu(gate) * (h @ w_val)) @ w_down
    return x


def setup_inputs(seed: int = 0) -> dict:
    key = jax.random.key(seed)
    ks = jax.random.split(key, 24)
    f32 = jnp.float32

    def nrm(k, shape, scale):
        return jax.random.normal(k, shape, f32) * scale

    def gain(k, shape):
        return 1.0 + 0.02 * jax.random.normal(k, shape, f32)

    L = DEPTH
    return {
        "x": jax.random.normal(ks[0], (BATCH, SEQ, D_MODEL), f32),
        "norm_mix_g": gain(ks[1], (L, D_MODEL)),
        "w_in": nrm(ks[2], (L, D_MODEL, IN_COLS), D_MODEL ** -0.5),
        "sgu_ln_g": gain(ks[3], (L, N_HEADS, HEAD_DIM)),
        "sgu_ln_b": nrm(ks[4], (L, N_HEADS, HEAD_DIM), 0.02),
        "sgu_w": nrm(ks[5], (L, N_HEADS, SGU_CHUNK, SGU_CHUNK), SGU_CHUNK ** -0.5),
        "sgu_b": gain(ks[6], (L, N_HEADS, SGU_CHUNK)),
        "mla_q_norm_g": gain(ks[7], (L, Q_LORA_RANK)),
        "mla_kv_norm_g": gain(ks[8], (L, KV_LORA_RANK)),
        "mla_w_uq": nrm(ks[9], (L, Q_LORA_RANK, N_HEADS * (QK_NOPE_DIM + QK_ROPE_DIM)),
                        Q_LORA_RANK ** -0.5),
        "mla_w_ukv": nrm(ks[10], (L, KV_LORA_RANK, N_HEADS * (QK_NOPE_DIM + V_HEAD_DIM)),
                         KV_LORA_RANK ** -0.5),
        "fox_b_f": nrm(ks[11], (L, N_HEADS), 0.1),
        "group_norm_g": gain(ks[12], (L, N_MIXERS, GROUP_WIDTH)),
        "w_o": nrm(ks[13], (L, D_MIX, D_MODEL), D_MIX ** -0.5),
        "norm_ffn_g": gain(ks[14], (L, D_MODEL)),
        "w_gate": nrm(ks[15], (L, D_MODEL, D_FF), D_MODEL ** -0.5),
        "w_val": nrm(ks[16], (L, D_MODEL, D_FF), D_MODEL ** -0.5),
        "conv_w": nrm(ks[17], (L, CONV_WIDTH, D_FF), CONV_WIDTH ** -0.5),
        "conv_b": nrm(ks[18], (L, D_FF), 0.01),
        "w_down": nrm(ks[19], (L, D_FF, D_MODEL), D_FF ** -0.5),
        "final_norm_g": gain(ks[20], (D_MODEL,)),
    }


def reference(x, norm_mix_g, w_in, sgu_ln_g, sgu_ln_b, sgu_w, sgu_b, mla_q_norm_g,
              mla_kv_norm_g, mla_w_uq, mla_w_ukv, fox_b_f, group_norm_g, w_o, norm_ffn_g,
              w_gate, w_val, conv_w, conv_b, w_down, final_norm_g):
    pos = jnp.arange(x.shape[1])
    slopes = alibi_slopes(N_HEADS)
    for l in range(DEPTH):
        x = hybrid_layer(x, pos, slopes, norm_mix_g[l], w_in[l], sgu_ln_g[l], sgu_ln_b[l],
                         sgu_w[l], sgu_b[l], mla_q_norm_g[l], mla_kv_norm_g[l], mla_w_uq[l],
                         mla_w_ukv[l], fox_b_f[l], group_norm_g[l], w_o[l], norm_ffn_g[l],
                         w_gate[l], w_val[l], conv_w[l], conv_b[l], w_down[l])
    return rmsnorm(x, final_norm_g)
```

```python
import math
import numpy as np
from contextlib import ExitStack
import concourse.bass as bass
import concourse.mybir as mybir
from concourse.bass_utils import run_bass_kernel_spmd

F32 = mybir.dt.float32
BF16 = mybir.dt.bfloat16
I32 = mybir.dt.int32
AF = mybir.ActivationFunctionType
ALU = mybir.AluOpType
AX = mybir.AxisListType

COMPUTE = ("pe", "act", "dve", "pool")
ALL_ENG = ("pe", "act", "dve", "pool", "sp")
EPS = 1e-6
NEG = -30000.0


class Tok:
    __slots__ = ("name", "writers", "readers")

    def __init__(self, name=""):
        self.name = name
        self.writers = {}
        self.readers = {}


class Op:
    __slots__ = ("eng", "fn", "deps", "marked", "milestone", "dma_sem", "dma_val", "idx", "key")


class Prog:
    def __init__(self, nc):
        self.nc = nc
        self.ops = {e: [] for e in ALL_ENG}
        self.dma_sem_count = {}
        self.last = {}
        self.barrier_deps = {e: {} for e in ALL_ENG}
        self.n = 0

    def op(self, eng, fn, reads=(), writes=(), dma_sem=None, partial=False):
        o = Op()
        o.eng, o.fn, o.marked, o.milestone = eng, fn, False, None
        o.dma_sem, o.dma_val = dma_sem, None
        o.idx = self.n
        self.n += 1
        o.key = ("dma", dma_sem) if dma_sem is not None else eng
        deps = {}

        def add(d):
            for kk, dd in d.items():
                cur = deps.get(kk)
                if cur is None or cur.idx < dd.idx:
                    deps[kk] = dd

        if self.barrier_deps[eng]:
            add(self.barrier_deps[eng])
            self.barrier_deps[eng] = {}
        for r in reads:
            add(r.writers)
        for w in writes:
            if not partial:
                add(w.writers)
            add(w.readers)
        for r in reads:
            r.readers[o.key] = o
        for w in writes:
            if partial:
                w.writers[o.key] = o
            else:
                w.writers = {o.key: o}
            w.readers = {}
        if dma_sem is not None:
            v = self.dma_sem_count.get(dma_sem, 0) + 16
            self.dma_sem_count[dma_sem] = v
            o.dma_val = v
        fd = []
        for kk, d in deps.items():
            if d is o:
                continue
            if d.dma_sem is None:
                if d.eng == eng and eng == "pe":
                    continue
                d.marked = True
            fd.append(d)
        o.deps = fd
        self.ops[eng].append(o)
        self.last[o.key] = o
        return o

    def barrier(self):
        for e in ALL_ENG:
            self.barrier_deps[e] = dict(self.last)

    def emit(self):
        nc = self.nc
        with ExitStack() as st:
            esem = {e: st.enter_context(nc.semaphore("s_" + e)) for e in COMPUTE}
            dsem = {k: st.enter_context(nc.semaphore("d_%s" % (k,))) for k in self.dma_sem_count}
            for e in COMPUTE:
                c = 0
                for o in self.ops[e]:
                    if o.dma_sem is None and o.marked:
                        c += 1
                        o.milestone = c
                assert c < 60000, (e, c)
            for k_, v_ in self.dma_sem_count.items():
                assert v_ < 60000, (k_, v_)
            block = st.enter_context(nc.Block())

            def gen(ename):
                def body(eng):
                    seen = {}
                    for o in self.ops[ename]:
                        need = {}
                        for d in o.deps:
                            if d.dma_sem is not None:
                                s, v = dsem[d.dma_sem], d.dma_val
                            else:
                                s, v = esem[d.eng], d.milestone
                            key = id(s)
                            if seen.get(key, 0) >= v:
                                continue
                            if key not in need or need[key][1] < v:
                                need[key] = (s, v)
                        for key, (s, v) in need.items():
                            eng.wait_ge(s, v)
                            seen[key] = v
                        inst = o.fn(eng)
                        if o.dma_sem is not None:
                            inst.then_inc(dsem[o.dma_sem], 16)
                        elif o.marked:
                            inst.then_inc(esem[ename], 1)
                    if ename == "sp":
                        for k, v in self.dma_sem_count.items():
                            eng.wait_ge(dsem[k], v)
                        for e2 in COMPUTE:
                            m = 0
                            for o in self.ops[e2]:
                                if o.milestone:
                                    m = o.milestone
                            if m:
                                eng.wait_ge(esem[e2], m)
                return body

            block.tensor(gen("pe"))
            block.scalar(gen("act"))
            block.vector(gen("dve"))
            block.gpsimd(gen("pool"))
            block.sync(gen("sp"))


class Rot:
    def __init__(self, items):
        self.items = items
        self.i = 0

    def next(self):
        it = self.items[self.i % len(self.items)]
        self.i += 1
        return it


class Cfg:
    def __init__(self, S=2048, D=4096, H=8, QL=768, KVL=256, DFF=11008, L=2, theta=10000.0):
        self.S, self.D, self.H, self.QL, self.KVL, self.DFF, self.L, self.theta = S, D, H, QL, KVL, DFF, L, theta
        self.GW = H * 128
        self.DMIX = 4 * self.GW
        self.KC = D // 128
        self.NCH = S // 512
        self.NT = S // 128
        self.NF = DFF // 128
        GW = self.GW
        self.oAq, self.oAk, self.oAv, self.oBu, self.oBv = 0, GW, 2 * GW, 3 * GW, 4 * GW
        self.oCq = 5 * GW
        self.oCkv = self.oCq + QL
        self.oCkr = self.oCkv + KVL
        self.oDq = self.oCkr + 64
        self.oDk = self.oDq + GW
        self.oDv = self.oDk + GW
        self.oDf = self.oDv + GW
        self.INC = self.oDf + H
        self.NBLK = S // 256


class K:
    pass


def build_program(cfg, dbg=False):
    nc = bass.Bass("TRN2", target_bir_lowering=False)
    k = K()
    k.nc, k.cfg, k.P = nc, cfg, Prog(nc)
    P = k.P
    S, D, H, GW, L = cfg.S, cfg.D, cfg.H, cfg.GW, cfg.L
    KC, NCH, NT, NF = cfg.KC, cfg.NCH, cfg.NT, cfg.NF

    def din(name, shape):
        return nc.dram_tensor(name, list(shape), F32, kind="ExternalInput").ap()

    I = {}
    I["x"] = din("x", [S, D])
    I["norm_mix_g"] = din("norm_mix_g", [L, D])
    I["w_in"] = din("w_in", [L, D, cfg.INC])
    I["sgu_ln_g"] = din("sgu_ln_g", [L, H, 128])
    I["sgu_ln_b"] = din("sgu_ln_b", [L, H, 128])
    I["sgu_w"] = din("sgu_w", [L, H, 128, 128])
    I["sgu_b"] = din("sgu_b", [L, H, 128])
    I["mla_q_norm_g"] = din("mla_q_norm_g", [L, cfg.QL])
    I["mla_kv_norm_g"] = din("mla_kv_norm_g", [L, cfg.KVL])
    I["mla_w_uq"] = din("mla_w_uq", [L, cfg.QL, H * 192])
    I["mla_w_ukv"] = din("mla_w_ukv", [L, cfg.KVL, H * 256])
    I["fox_b_f"] = din("fox_b_f", [L, H])
    I["group_norm_g"] = din("group_norm_g", [L, 4, GW])
    I["w_o"] = din("w_o", [L, cfg.DMIX, D])
    I["norm_ffn_g"] = din("norm_ffn_g", [L, D])
    I["w_gate"] = din("w_gate", [L, D, cfg.DFF])
    I["w_val"] = din("w_val", [L, D, cfg.DFF])
    I["conv_w"] = din("conv_w", [L, 3, cfg.DFF])
    I["conv_b"] = din("conv_b", [L, cfg.DFF])
    I["w_down"] = din("w_down", [L, cfg.DFF, D])
    I["final_norm_g"] = din("final_norm_g", [D])
    k.I = I
    out = nc.dram_tensor("out", [S, D], F32, kind="ExternalOutput").ap()
    k.out = out
    skind = "ExternalOutput" if dbg else "Internal"

    def scratch(name, shape, dt=F32):
        return nc.dram_tensor(name, list(shape), dt, kind=skind).ap()

    k.xT = scratch("xT", [D, S])
    k.projT = scratch("projT", [cfg.INC, S])
    k.krT = scratch("krT", [64, S])
    k.qmT = scratch("qmT", [H * 192, S])
    k.kvT = scratch("kvT", [H * 256, S])
    k.ycatT = scratch("ycatT", [cfg.DMIX, S])
    k.aT = scratch("aT", [cfg.DFF, S], BF16)
    k.cosT = scratch("cosT", [64, S])
    k.sinT = scratch("sinT", [64, S])
    k.t_xT = [Tok("xT%d" % i) for i in range(KC)]
    k.t_proj = Tok("proj")
    k.t_kr = Tok("kr")
    k.t_qm = Tok("qm")
    k.t_kv = Tok("kv")
    k.t_ycat = Tok("ycat")
    k.t_aT = Tok("aT")
    k.t_rope = Tok("rope")
    k.t_out = Tok("out")
    k.wdb = scratch("wdb", [KC, 128, NF * 128], BF16)
    k.t_wdb = Tok("wdb")

    with ExitStack() as gst:
        k.ps = [gst.enter_context(nc.psum_tensor("ps%d" % i, [128, 512], F32)) for i in range(8)]
        k.tps = [Tok("ps%d" % i) for i in range(8)]
        k.ident = gst.enter_context(nc.sbuf_tensor("ident", [128, 128], F32))
        k.ones_bf = gst.enter_context(nc.sbuf_tensor("ones_bf", [128, 128], BF16))
        k.ones_f = gst.enter_context(nc.sbuf_tensor("ones_f", [128, 128], F32))
        k.t_const = Tok("const")
        k.rk = gst.enter_context(nc.sbuf_tensor("rstd_keep", [128, NCH, 512], F32))
        k.t_rk = Tok("rk")
        with ExitStack() as st:
            io = st.enter_context(nc.sbuf_tensor("io_tmp", [128, 128], F32))
            P.op("pool", lambda e: e.iota(io[:], pattern=[[1, 128]], base=0, channel_multiplier=-1,
                                          allow_small_or_imprecise_dtypes=True), writes=[k.t_const])
            P.op("dve", lambda e: e.tensor_single_scalar(out=k.ident[:], in_=io[:], scalar=0.0, op=ALU.is_equal),
                 reads=[k.t_const], writes=[k.t_const])
            P.op("dve", lambda e: e.memset(k.ones_f[:], 1.0), writes=[k.t_const], partial=True)
            P.op("dve", lambda e: e.memset(k.ones_bf[:], 1.0), writes=[k.t_const], partial=True)
            P.barrier()
        phase_rope_tables(k)
        phase_transpose_in(k)
        for l in range(L):
            phase_norm_inproj(k, l)
            phase_mla_proj(k, l)
            phase_attention(k, l)
            phase_gmlp(k, l)
            phase_wo(k, l)
            phase_ffn_up(k, l)
            phase_ffn_down(k, l)
        phase_final(k)
        P.emit()
    return nc


def new_phase(k):
    k.P.barrier()
    st = ExitStack()
    k.cnt = getattr(k, "cnt", 0) + 1
    pfx = "p%d_" % k.cnt

    def sb(name, shape, dt=F32):
        return st.enter_context(k.nc.sbuf_tensor(pfx + name, list(shape), dt))

    return st, sb


def mkrot(sb, name, n, shape, dt=F32, key=None):
    items = []
    for i in range(n):
        t = sb("%s%d" % (name, i), shape, dt)
        items.append((t, Tok("%s%d" % (name, i)), "%s%d" % (key or name, i)))
    return Rot(items)


def load_vec_fm(k, sb, name, vec_ap, n, rows=128):
    P = k.P
    tmp = sb(name + "_t", [n, rows], F32)
    dst = sb(name, [rows, n], F32)
    ttok, dtok = Tok(), Tok()
    if not hasattr(k, "t_vser"):
        k.t_vser = Tok("vser")
    P.op("sp", lambda e: e.dma_start(out=tmp[:], in_=vec_ap.rearrange("(n p) -> n p", p=rows)),
         writes=[ttok, k.t_vser], dma_sem="vec")
    ps, pt = k.ps[7], k.tps[7]
    P.op("pe", lambda e: e.transpose(out=ps[0:rows, 0:n], in_=tmp[0:n, 0:rows], identity=k.ident[0:n, 0:n]),
         reads=[ttok, k.t_const], writes=[pt])
    P.op("dve", lambda e: e.tensor_copy(out=dst[:], in_=ps[0:rows, 0:n]), reads=[pt], writes=[dtok])
    return dst, dtok


def rms_norm_fm(k, sb, src_fn, nK, nfeat, gT, gtok, dst_fn, nch, tagp="", rstd_pre=False):
    P = k.P
    P.barrier()
    G = min(4, nK)
    xr = mkrot(sb, tagp + "nx", 3, [128, G, 512], F32, key="nx")
    sq = mkrot(sb, tagp + "nsq", 3, [128, 512], BF16)
    rs = mkrot(sb, tagp + "nrs", 2, [128, 512], F32)
    banks = Rot([(k.ps[6], k.tps[6]), (k.ps[7], k.tps[7])])
    groups = [(g0, min(G, nK - g0)) for g0 in range(0, nK, G)]
    nq_ = [0]
    for c in range(nch):
        ps, pt = banks.next()
        for (g0, n) in ([] if rstd_pre else groups):
            x, xt, xk = xr.next()
            src, stoks = src_fn(g0, n, c)
            nq_[0] += 1
            P.op("sp" if nq_[0] % 2 else "pool", lambda e, x=x, src=src, n=n: e.dma_start(out=x[:, 0:n, :], in_=src),
                 reads=stoks, writes=[xt], dma_sem=xk)
            for i in range(n):
                kc = g0 + i
                s, sqt, _ = sq.next()
                P.op("act", lambda e, s=s, x=x, i=i: e.activation(out=s[:], in_=x[:, i, :], func=AF.Square), reads=[xt],
                     writes=[sqt])
                P.op("pe", lambda e, ps=ps, s=s, kc=kc: e.matmul(ps[:], lhsT=k.ones_bf[:], rhs=s[:], start=(kc == 0),
                                                                stop=(kc == nK - 1)),
                     reads=[sqt, k.t_const], writes=[pt])
        if rstd_pre:
            r, rt = k.rk[:, c, :], k.t_rk
        else:
            r_, rt, _ = rs.next()
            r = r_[:]
            P.op("act", lambda e, r=r, ps=ps: e.activation(out=r, in_=ps[:], func=AF.Sqrt, scale=1.0 / nfeat, bias=EPS),
                 reads=[pt], writes=[rt])
            P.op("dve", lambda e, r=r: e.reciprocal(out=r, in_=r), reads=[rt], writes=[rt])
        for (g0, n) in groups:
            x, xt, xk = xr.next()
            src, stoks = src_fn(g0, n, c)
            nq_[0] += 1
            P.op("sp" if nq_[0] % 2 else "pool", lambda e, x=x, src=src, n=n: e.dma_start(out=x[:, 0:n, :], in_=src),
                 reads=stoks, writes=[xt], dma_sem=xk)
            for i in range(n):
                kc = g0 + i
                dst, dtok = dst_fn(kc, c)
                P.op("dve", lambda e, x=x, dst=dst, kc=kc, r=r, i=i: e.scalar_tensor_tensor(
                    out=dst, in0=x[:, i, :], scalar=gT[:, kc:kc + 1], in1=r, op0=ALU.mult, op1=ALU.mult),
                     reads=[xt, rt, gtok], writes=[dtok], partial=True)


def linear(k, w2d, nK, col_tiles, act_fn, nch, evac, wrot, banks, wsrc=None, wq="pool", wreads=()):
    P = k.P
    wv = w2d.rearrange("(kc p) n -> p kc n", p=128) if w2d is not None else None
    for j, (pieces, M) in enumerate(col_tiles):
        wt, wtok, wkey = wrot.next()
        off = 0
        if wsrc is not None:
            src = wsrc(j)
            P.op(wq, lambda e, wt=wt, src=src: e.dma_start(out=wt[:, 0:nK, :], in_=src), reads=list(wreads),
                 writes=[wtok], dma_sem=wkey)
            pieces = []
        for (c0, n) in pieces:
            P.op("pool", lambda e, wt=wt, off=off, c0=c0, n=n: e.dma_start(out=wt[:, 0:nK, off:off + n],
                                                                          in_=wv[:, :, c0:c0 + n]),
                 writes=[wtok], dma_sem=wkey, partial=(off > 0))
            off += n
        for c in range(nch):
            ps, pt = banks.next()
            for kc in range(nK):
                a, atok = act_fn(kc, c)
                P.op("pe", lambda e, ps=ps, wt=wt, kc=kc, a=a, M=M: e.matmul(ps[0:M, :], lhsT=wt[:, kc, 0:M], rhs=a,
                                                                            start=(kc == 0), stop=(kc == nK - 1)),
                     reads=[wtok, atok], writes=[pt])
            evac(j, c, ps, pt, M)


class SumSq:
    def __init__(self, k, sb, acc_of_c, nj):
        self.k, self.acc_of_c, self.nj = k, acc_of_c, nj
        self.sq = mkrot(sb, "ssq", 4, [128, 512], BF16)
        self.pending = []

    def add(self, j, c, xo, xot):
        k, P = self.k, self.k.P
        s, st_, _ = self.sq.next()
        P.op("act", lambda e: e.activation(out=s[:], in_=xo[:], func=AF.Square), reads=[xot], writes=[st_])
        self.pending.append((j, c, s, st_))
        if len(self.pending) > 2:
            self.flush(1)

    def flush(self, n=None):
        k, P = self.k, self.k.P
        D = k.cfg.D
        n = len(self.pending) if n is None else n
        for _ in range(n):
            j, c, s, st_ = self.pending.pop(0)
            acc, acct = self.acc_of_c(c)
            P.op("pe", lambda e, acc=acc, s=s, j=j: e.matmul(acc[:], lhsT=k.ones_bf[:], rhs=s[:], start=(j == 0),
                                                            stop=(j == self.nj - 1)),
                 reads=[st_, k.t_const], writes=[acct], partial=(j > 0))
            if j == self.nj - 1:
                P.op("act", lambda e, acc=acc, c=c: e.activation(out=k.rk[:, c, :], in_=acc[:], func=AF.Sqrt,
                                                                 scale=1.0 / D, bias=EPS),
                     reads=[acct], writes=[k.t_rk], partial=True)
                P.op("dve", lambda e, c=c: e.reciprocal(out=k.rk[:, c, :], in_=k.rk[:, c, :]), reads=[k.t_rk],
                     writes=[k.t_rk], partial=True)


def make_resid_evac(k, sb, coff_fn, ssq=None):
    P = k.P
    xr = mkrot(sb, "xr", 3, [128, 512], F32)
    xo = mkrot(sb, "xo", 4, [128, 512], F32)

    def ev(j, c, ps, pt, M):
        c0 = coff_fn(c)
        dst = k.xT[j * 128:(j + 1) * 128, c0:c0 + 512]
        r, rt, rk = xr.next()
        o, ot, ok = xo.next()
        P.op("sp", lambda e: e.dma_start(out=r[:], in_=dst), reads=[k.t_xT[j]], writes=[rt], dma_sem=rk)
        P.op("dve", lambda e: e.tensor_tensor(out=o[:], in0=ps[:], in1=r[:], op=ALU.add), reads=[pt, rt], writes=[ot])
        P.op("sp", lambda e: e.dma_start(out=dst, in_=o[:]), reads=[ot], writes=[k.t_xT[j]], dma_sem=ok, partial=True)
        if ssq is not None:
            ssq.add(j, coff_fn(c) // 512, o, ot)

    return ev


def phase_rope_tables(k):
    cfg, P = k.cfg, k.P
    S = cfg.S
    st, sb = new_phase(k)
    with st:
        pidx = sb("pidx", [64, 1])
        invf = sb("invf", [64, 1])
        pos = sb("pos", [64, S])
        ang = sb("ang", [64, S])
        kk = sb("kk", [64, S])
        ki = sb("ki", [64, S], I32)
        res = sb("res", [64, S])
        T = Tok("rt")
        P.op("pool", lambda e: e.iota(pidx[:], pattern=[[0, 1]], base=0, channel_multiplier=1,
                                      allow_small_or_imprecise_dtypes=True), writes=[T])
        P.op("pool", lambda e: e.iota(pos[:], pattern=[[1, S]], base=0, channel_multiplier=0,
                                      allow_small_or_imprecise_dtypes=True), reads=[T], writes=[T])
        P.op("act", lambda e: e.activation(out=invf[:], in_=pidx[:], func=AF.Exp, scale=-math.log(cfg.theta) / 32.0),
             reads=[T], writes=[T])
        P.op("dve", lambda e: e.tensor_scalar_mul(out=invf[32:64, :], in0=invf[32:64, :], scalar1=float(cfg.theta)),
             reads=[T], writes=[T])
        twopi = 2.0 * math.pi
        for which, shift, dstT in (("sin", 0.0, k.sinT), ("cos", math.pi / 2.0, k.cosT)):
            P.op("dve", lambda e, shift=shift: e.tensor_scalar(out=ang[:], in0=pos[:], scalar1=invf[:, 0:1], scalar2=shift,
                                                              op0=ALU.mult, op1=ALU.add), reads=[T], writes=[T])
            P.op("dve", lambda e: e.tensor_scalar_mul(out=kk[:], in0=ang[:], scalar1=1.0 / twopi), reads=[T], writes=[T])
            P.op("dve", lambda e: e.tensor_copy(out=ki[:], in_=kk[:]), reads=[T], writes=[T])
            P.op("dve", lambda e: e.tensor_copy(out=kk[:], in_=ki[:]), reads=[T], writes=[T])
            P.op("dve", lambda e: e.scalar_tensor_tensor(out=ang[:], in0=kk[:], scalar=-twopi, in1=ang[:], op0=ALU.mult,
                                                         op1=ALU.add), reads=[T], writes=[T])
            P.op("dve", lambda e: e.tensor_scalar(out=ang[:], in0=ang[:], scalar1=math.pi, scalar2=-math.pi, op0=ALU.min,
                                                  op1=ALU.max), reads=[T], writes=[T])
            P.op("act", lambda e: e.activation(out=res[:], in_=ang[:], func=AF.Sin), reads=[T], writes=[T])
            if which == "sin":
                P.op("dve", lambda e: e.tensor_scalar_mul(out=res[0:32, :], in0=res[0:32, :], scalar1=-1.0), reads=[T],
                     writes=[T])
            P.op("sp", lambda e, dstT=dstT: e.dma_start(out=dstT, in_=res[:]), reads=[T], writes=[k.t_rope, T],
                 dma_sem="st0", partial=True)


def phase_transpose_in(k):
    cfg, P = k.cfg, k.P
    S, D, KC, NT = cfg.S, cfg.D, cfg.KC, cfg.NT
    st, sb = new_phase(k)
    with st:
        xin = mkrot(sb, "xin", 2, [128, D], F32)
        stg = mkrot(sb, "stg", 3, [128, 4, 128], F32)
        banks = Rot([(k.ps[i], k.tps[i]) for i in range(6)])
        tx = Tok("x")
        n = 0
        for tt in range(NT):
            x, xt, xk = xin.next()
            P.op("sp", lambda e, x=x, tt=tt: e.dma_start(out=x[:], in_=k.I["x"][tt * 128:(tt + 1) * 128, :]), reads=[tx],
                 writes=[xt], dma_sem=xk)
            for g in range(KC // 4):
                ps, pt = banks.next()
                for i in range(4):
                    kc = g * 4 + i
                    P.op("pe", lambda e, ps=ps, x=x, i=i, kc=kc: e.transpose(out=ps[:, i * 128:(i + 1) * 128],
                                                                            in_=x[:, kc * 128:(kc + 1) * 128],
                                                                            identity=k.ident[:]),
                         reads=[xt, k.t_const], writes=[pt], partial=(i > 0))
                s, stok, sk = stg.next()
                n += 1
                if n % 2:
                    P.op("act", lambda e, s=s, ps=ps: e.activation(out=s[:].rearrange("p a b -> p (a b)"), in_=ps[:],
                                                                  func=AF.Copy), reads=[pt], writes=[stok])
                else:
                    P.op("dve", lambda e, s=s, ps=ps: e.tensor_copy(out=s[:].rearrange("p a b -> p (a b)"), in_=ps[:]),
                         reads=[pt], writes=[stok])
                dst = k.xT[g * 512:(g + 1) * 512, tt * 128:(tt + 1) * 128].rearrange("(i p) t -> p i t", p=128)
                P.op("act", lambda e, dst=dst, s=s: e.dma_start(out=dst, in_=s[:]), reads=[stok],
                     writes=[k.t_xT[g * 4 + i] for i in range(4)], dma_sem=sk, partial=True)


def phase_norm_inproj(k, l):
    cfg, P, I = k.cfg, k.P, k.I
    S, D, H, GW, KC, NCH = cfg.S, cfg.D, cfg.H, cfg.GW, cfg.KC, cfg.NCH
    st, sb = new_phase(k)
    with st:
        hT = sb("hT", [128, KC, S], BF16)
        t_h = Tok("hT")
        gT, gtok = load_vec_fm(k, sb, "g1", I["norm_mix_g"][l], KC)
        with ExitStack() as st2:
            def sb2(name, shape, dt=F32):
                return st2.enter_context(k.nc.sbuf_tensor("n1_%d_%s" % (l, name), list(shape), dt))
            rms_norm_fm(k, sb2, lambda g0, n, c: (k.xT[g0 * 128:(g0 + n) * 128, c * 512:(c + 1) * 512].rearrange("(g p) t -> p g t", p=128), [k.t_xT[g0 + i] for i in range(n)]), KC, D,
                        gT, gtok, lambda kc, c: (hT[:, kc, c * 512:(c + 1) * 512], t_h), NCH, rstd_pre=(l > 0))
            P.barrier()
        cosS = sb("cos", [64, S])
        sinS = sb("sin", [64, S])
        t_cs = Tok("cs")
        P.op("sp", lambda e: e.dma_start(out=cosS[:], in_=k.cosT), reads=[k.t_rope], writes=[t_cs], dma_sem="ld0")
        P.op("sp", lambda e: e.dma_start(out=sinS[:], in_=k.sinT), reads=[k.t_rope], writes=[t_cs], dma_sem="ld1",
             partial=True)
        krraw = sb("krraw", [64, S])
        t_krraw = Tok("krraw")
        wrot = mkrot(sb, "w", 3, [128, KC, 128], BF16)
        banks = Rot([(k.ps[i], k.tps[i]) for i in range(6)])
        stg = mkrot(sb, "stg", 3, [128, 512], F32, key="st")
        tiles = []
        kinds = []
        sc_q = 128.0 ** -0.5
        for c0 in range(0, cfg.oCkr, 128):
            tiles.append(([(c0, 128)], 128))
            if c0 < GW:
                kinds.append(("scale", sc_q, c0))
            elif cfg.oBu <= c0 < cfg.oCq:
                kinds.append(("gelu", 1.0, c0))
            else:
                kinds.append(("scale", 1.0, c0))
        tiles.append(([(cfg.oCkr, 64)], 64))
        kinds.append(("kr", 1.0, 0))
        tiles.append(([(cfg.oCkr + 32, 32), (cfg.oCkr, 32)], 64))
        kinds.append(("krsw", 1.0, 0))
        for c0 in range(cfg.oDq, cfg.oDf, 128):
            tiles.append(([(c0, 128)], 128))
            kinds.append(("scale", sc_q if c0 < cfg.oDk else 1.0, c0))
        tiles.append(([(cfg.oDf, H)], H))
        kinds.append(("scale", 1.0, cfg.oDf))
        cnt = [0]

        def evac(j, c, ps, pt, M):
            kind, sc, r0 = kinds[j]
            cs = slice(c * 512, (c + 1) * 512)
            if kind == "kr":
                P.op("act", lambda e: e.activation(out=krraw[:, cs], in_=ps[0:64, :], func=AF.Copy), reads=[pt],
                     writes=[t_krraw], partial=True)
                return
            s, stok, sk = stg.next()
            if kind == "krsw":
                P.op("dve", lambda e: e.tensor_tensor(out=s[0:64, :], in0=ps[0:64, :], in1=sinS[:, cs], op=ALU.mult),
                     reads=[pt, t_cs], writes=[stok])
                P.op("dve", lambda e: e.tensor_tensor(out=krraw[:, cs], in0=krraw[:, cs], in1=cosS[:, cs], op=ALU.mult),
                     reads=[t_krraw, t_cs], writes=[t_krraw], partial=True)
                P.op("dve", lambda e: e.tensor_tensor(out=s[0:64, :], in0=s[0:64, :], in1=krraw[:, cs], op=ALU.add),
                     reads=[t_krraw, stok], writes=[stok])
                P.op("sp", lambda e: e.dma_start(out=k.krT[:, cs], in_=s[0:64, :]), reads=[stok], writes=[k.t_kr],
                     dma_sem=sk, partial=True)
                return
            if kind == "gelu":
                P.op("act", lambda e: e.activation(out=s[0:M, :], in_=ps[0:M, :], func=AF.Gelu_apprx_tanh), reads=[pt],
                     writes=[stok])
            else:
                cnt[0] += 1
                if cnt[0] % 2:
                    P.op("act", lambda e: e.activation(out=s[0:M, :], in_=ps[0:M, :], func=AF.Copy, scale=float(sc)),
                         reads=[pt], writes=[stok])
                else:
                    P.op("dve", lambda e: e.tensor_scalar_mul(out=s[0:M, :], in0=ps[0:M, :], scalar1=float(sc)),
                         reads=[pt], writes=[stok])
            P.op("sp", lambda e: e.dma_start(out=k.projT[r0:r0 + M, cs], in_=s[0:M, :]), reads=[stok], writes=[k.t_proj],
                 dma_sem=sk, partial=True)

        linear(k, I["w_in"][l], KC, tiles, lambda kc, c: (hT[:, kc, c * 512:(c + 1) * 512], t_h), NCH, evac, wrot, banks)


def phase_mla_proj(k, l):
    cfg, P, I = k.cfg, k.P, k.I
    S, H, NCH = cfg.S, cfg.H, cfg.NCH
    nq, nkv = cfg.QL // 128, cfg.KVL // 128
    st, sb = new_phase(k)
    with st:
        cqn = sb("cqn", [128, nq, S], BF16)
        ckvn = sb("ckvn", [128, nkv, S], BF16)
        t_cqn, t_ckvn = Tok(), Tok()
        gq, gqt = load_vec_fm(k, sb, "gq", I["mla_q_norm_g"][l], nq)
        gkv, gkvt = load_vec_fm(k, sb, "gkv", I["mla_kv_norm_g"][l], nkv)
        rms_norm_fm(k, sb, lambda g0, n, c: (k.projT[cfg.oCq + g0 * 128:cfg.oCq + (g0 + n) * 128, c * 512:(c + 1) * 512].rearrange(
            "(g p) t -> p g t", p=128), [k.t_proj]), nq, cfg.QL, gq, gqt,
                    lambda kc, c: (cqn[:, kc, c * 512:(c + 1) * 512], t_cqn), NCH, tagp="q")
        rms_norm_fm(k, sb, lambda g0, n, c: (k.projT[cfg.oCkv + g0 * 128:cfg.oCkv + (g0 + n) * 128, c * 512:(c + 1) * 512].rearrange(
            "(g p) t -> p g t", p=128), [k.t_proj]), nkv, cfg.KVL, gkv, gkvt,
                    lambda kc, c: (ckvn[:, kc, c * 512:(c + 1) * 512], t_ckvn), NCH, tagp="kv")
        cosS = sb("cos", [64, S])
        sinS = sb("sin", [64, S])
        t_cs = Tok("cs")
        P.op("sp", lambda e: e.dma_start(out=cosS[:], in_=k.cosT), reads=[k.t_rope], writes=[t_cs], dma_sem="ld0")
        P.op("sp", lambda e: e.dma_start(out=sinS[:], in_=k.sinT), reads=[k.t_rope], writes=[t_cs], dma_sem="ld1",
             partial=True)
        qraw = sb("qraw", [64, S])
        t_qraw = Tok()
        wq = mkrot(sb, "wq", 3, [128, nq, 128], BF16, key="w")
        wkv = mkrot(sb, "wkv", 3, [128, nkv, 128], BF16, key="w")
        banks = Rot([(k.ps[i], k.tps[i]) for i in range(6)])
        stg = mkrot(sb, "stg", 3, [128, 512], F32, key="st")
        sc = 192.0 ** -0.5
        tiles, kinds = [], []
        for h in range(H):
            tiles.append(([(h * 192, 128)], 128)); kinds.append(("nope", h))
            tiles.append(([(h * 192 + 128, 64)], 64)); kinds.append(("r", h))
            tiles.append(([(h * 192 + 160, 32), (h * 192 + 128, 32)], 64)); kinds.append(("rsw", h))

        def evac_q(j, c, ps, pt, M):
            kind, h = kinds[j]
            cs = slice(c * 512, (c + 1) * 512)
            if kind == "r":
                P.op("act", lambda e: e.activation(out=qraw[:, cs], in_=ps[0:64, :], func=AF.Copy), reads=[pt],
                     writes=[t_qraw], partial=True)
                return
            s, stok, sk = stg.next()
            if kind == "rsw":
                P.op("dve", lambda e: e.tensor_tensor(out=s[0:64, :], in0=ps[0:64, :], in1=sinS[:, cs], op=ALU.mult),
                     reads=[pt, t_cs], writes=[stok])
                P.op("dve", lambda e: e.tensor_tensor(out=qraw[:, cs], in0=qraw[:, cs], in1=cosS[:, cs], op=ALU.mult),
                     reads=[t_qraw, t_cs], writes=[t_qraw], partial=True)
                P.op("dve", lambda e: e.scalar_tensor_tensor(out=s[0:64, :], in0=s[0:64, :], scalar=1.0, in1=qraw[:, cs],
                                                             op0=ALU.mult, op1=ALU.add),
                     reads=[t_qraw, stok], writes=[stok])
                P.op("act", lambda e: e.activation(out=s[0:64, :], in_=s[0:64, :], func=AF.Copy, scale=sc), reads=[stok],
                     writes=[stok])
                P.op("sp", lambda e: e.dma_start(out=k.qmT[h * 192 + 128:h * 192 + 192, cs], in_=s[0:64, :]),
                     reads=[stok], writes=[k.t_qm], dma_sem=sk, partial=True)
                return
            P.op("act", lambda e: e.activation(out=s[:], in_=ps[:], func=AF.Copy, scale=sc), reads=[pt], writes=[stok])
            P.op("sp", lambda e: e.dma_start(out=k.qmT[h * 192:h * 192 + 128, cs], in_=s[:]), reads=[stok],
                 writes=[k.t_qm], dma_sem=sk, partial=True)

        linear(k, I["mla_w_uq"][l], nq, tiles, lambda kc, c: (cqn[:, kc, c * 512:(c + 1) * 512], t_cqn), NCH, evac_q, wq,
               banks)
        tiles2 = [([(j * 128, 128)], 128) for j in range(2 * H)]

        def evac_kv(j, c, ps, pt, M):
            cs = slice(c * 512, (c + 1) * 512)
            s, stok, sk = stg.next()
            P.op("dve", lambda e: e.tensor_copy(out=s[:], in_=ps[:]), reads=[pt], writes=[stok])
            P.op("sp", lambda e: e.dma_start(out=k.kvT[j * 128:(j + 1) * 128, cs], in_=s[:]), reads=[stok],
                 writes=[k.t_kv], dma_sem=sk, partial=True)

        linear(k, I["mla_w_ukv"][l], nkv, tiles2, lambda kc, c: (ckvn[:, kc, c * 512:(c + 1) * 512], t_ckvn), NCH, evac_kv,
               wkv, banks)


def phase_attention(k, l):
    cfg, P, I = k.cfg, k.P, k.I
    S, H, GW, NCH, NT, NBLK = cfg.S, cfg.H, cfg.GW, cfg.NCH, cfg.NT, cfg.NBLK
    NOH = max(NBLK, H)
    st, sb = new_phase(k)
    with st:
        T = Tok("acst")
        iot = sb("iot", [128, 512])
        negm = [sb("negm%d" % i, [128, 512]) for i in range(4)]
        pio = sb("pio", [NOH, 128])
        oh = [sb("oh%d" % n, [NOH, 128], BF16) for n in range(NOH)]
        ohm = [sb("ohm%d" % n, [65, 128], BF16) for n in range(NBLK)]
        iot16 = sb("iot16", [128, NT])
        arows = sb("arows", [65, S])
        P.op("pool", lambda e: e.iota(iot[:], pattern=[[1, 512]], base=0, channel_multiplier=-1,
                                      allow_small_or_imprecise_dtypes=True), writes=[T])
        P.op("pool", lambda e: e.iota(pio[:], pattern=[[0, 128]], base=0, channel_multiplier=1,
                                      allow_small_or_imprecise_dtypes=True), reads=[T], writes=[T])
        P.op("pool", lambda e: e.iota(iot16[:], pattern=[[128, NT]], base=0, channel_multiplier=1,
                                      allow_small_or_imprecise_dtypes=True), reads=[T], writes=[T])
        P.op("pool", lambda e: e.iota(arows[32:33, :].rearrange("p (a b) -> p a b", b=128), pattern=[[128, NT], [0, 128]],
                                      base=0, channel_multiplier=0, allow_small_or_imprecise_dtypes=True),
             reads=[T], writes=[T])
        P.op("pool", lambda e: e.iota(arows[64:65, :].rearrange("p (a b) -> p a b", b=128), pattern=[[0, NT], [1, 128]],
                                      base=0, channel_multiplier=0, allow_small_or_imprecise_dtypes=True),
             reads=[T], writes=[T])
        for i in range(4):
            P.op("dve", lambda e, i=i: e.tensor_scalar(out=negm[i][:], in0=iot[:], scalar1=float(128 * i), scalar2=NEG,
                                                      op0=ALU.is_lt, op1=ALU.mult), reads=[T], writes=[T])
        for n in range(NOH):
            P.op("dve", lambda e, n=n: e.tensor_single_scalar(out=oh[n][:], in_=pio[:], scalar=float(n), op=ALU.is_equal),
                 reads=[T], writes=[T])
        for n in range(NBLK):
            P.op("dve", lambda e, n=n: e.memset(ohm[n][:], 0.0), reads=[T], writes=[T])
            P.op("dve", lambda e, n=n: e.tensor_copy(out=ohm[n][0:NBLK, :], in_=oh[n][0:NBLK, :]), reads=[T], writes=[T])
            P.op("dve", lambda e, n=n: e.memset(ohm[n][32:33, :], 1.0), reads=[T], writes=[T])
            P.op("dve", lambda e, n=n: e.memset(ohm[n][64:65, :], 1.0), reads=[T], writes=[T])

        vio = sb("vio", [128, NT, 8])
        vmask = sb("vmask", [128, NT, 8])
        nfill = sb("nfill", [128, NT, 8])
        P.op("pool", lambda e: e.iota(vio[:], pattern=[[1, NT], [-2, 8]], base=0, channel_multiplier=0,
                                      allow_small_or_imprecise_dtypes=True), reads=[T], writes=[T])
        P.op("dve", lambda e: e.tensor_single_scalar(out=vmask[:], in_=vio[:], scalar=2.0, op=ALU.is_ge), reads=[T],
             writes=[T])
        P.op("dve", lambda e: e.tensor_scalar(out=nfill[:], in0=vio[:], scalar1=2.0, scalar2=-1e30, op0=ALU.is_lt,
                                              op1=ALU.mult), reads=[T], writes=[T])
        fA = sb("fA", [H, S])
        fB = sb("fB", [H, S])
        bfv = sb("bfv", [H, 1])
        cs3 = [sb("cs3_%d" % i, [H, S], BF16) for i in range(3)]
        csr = sb("csr", [H, S])
        cs_tm = sb("cs_tm", [128, NT, H])
        TF = Tok("fox")
        fox_state = {}

        def fox_prep():
            P.op("sp", lambda e: e.dma_start(out=fA[:], in_=k.projT[cfg.oDf:cfg.oDf + H, :]), reads=[k.t_proj], writes=[TF],
                 dma_sem="ld0")
            P.op("sp", lambda e: e.dma_start(out=bfv[:], in_=I["fox_b_f"][l].rearrange("(h o) -> h o", o=1)), writes=[TF],
                 dma_sem="ld1", partial=True)
            P.op("act", lambda e: e.activation(out=bfv[:], in_=bfv[:], func=AF.Copy, scale=-1.0), reads=[TF], writes=[TF])
            P.op("act", lambda e: e.activation(out=fB[:], in_=fA[:], func=AF.Exp, bias=bfv[:, 0:1], scale=-1.0), reads=[TF],
                 writes=[TF])
            P.op("act", lambda e: e.activation(out=fA[:], in_=fB[:], func=AF.Ln, bias=1.0), reads=[TF], writes=[TF])
            a, b = fA, fB
            d = 1
            while d < S:
                P.op("pool", lambda e, a=a, b=b, d=d: e.tensor_tensor(out=b[:, d:S], in0=a[:, d:S], in1=a[:, 0:S - d], op=ALU.add),
                     reads=[TF], writes=[TF])
                P.op("pool", lambda e, a=a, b=b, d=d: e.tensor_copy(out=b[:, 0:d], in_=a[:, 0:d]), reads=[TF], writes=[TF])
                a, b = b, a
                d *= 2
            cs = a
            oth = b
            P.op("act", lambda e: e.activation(out=oth[:], in_=cs[:], func=AF.Copy, scale=-1.0), reads=[TF], writes=[TF])
            for i in range(3):
                P.op("pool", lambda e, i=i: e.tensor_copy(out=cs3[i][:], in_=oth[:]), reads=[TF], writes=[TF])
                if i < 2:
                    P.op("pool", lambda e, i=i: e.tensor_copy(out=csr[:], in_=cs3[i][:]), reads=[TF], writes=[TF])
                    P.op("pool", lambda e: e.tensor_tensor(out=oth[:], in0=oth[:], in1=csr[:], op=ALU.subtract), reads=[TF],
                         writes=[TF])
            fox_state['cs'] = cs

        def fox_prep_pe():
            cs = fox_state['cs']
            for tt in range(NT):
                P.op("pe", lambda e, tt=tt: e.transpose(out=k.ps[7][:, tt * H:(tt + 1) * H],
                                                       in_=cs[0:H, tt * 128:(tt + 1) * 128],
                                                       identity=k.ident[0:H, 0:H]), reads=[TF, k.t_const],
                     writes=[k.tps[7]], partial=(tt > 0))
            P.op("dve", lambda e: e.tensor_copy(out=cs_tm[:].rearrange("p a b -> p (a b)"), in_=k.ps[7][:, 0:NT * H]),
                 reads=[k.tps[7]], writes=[TF], partial=True)

        qf = mkrot(sb, "qf", 2, [128, S], F32, key="hq")
        kf = mkrot(sb, "kf", 2, [128, S], F32, key="hk")
        vf = mkrot(sb, "vf", 2, [128, S], F32, key="hv")
        qb = mkrot(sb, "qb", 2, [128, S], BF16, key="hqb")
        kb = mkrot(sb, "kb", 2, [128, S], BF16, key="hkb")
        q2 = mkrot(sb, "q2", 2, [64, S], BF16, key="hq2")
        q2f = mkrot(sb, "q2f", 2, [64, S], F32, key="hq2f")
        vtm = mkrot(sb, "vtm", 2, [128, NT, 128], BF16)
        selbT = mkrot(sb, "selbT", 2, [65, S], BF16)
        mbias = mkrot(sb, "mbias", 2, [128, NT], F32)
        k2 = sb("k2", [64, S], BF16)
        t_k2 = Tok("k2")
        kmean = sb("kmean", [128, 8])
        g8 = sb("g8", [128, NT, 8])
        cmp4 = sb("cmp4", [128, NT, 8, 8])
        cnt8 = sb("cnt8", [128, NT, 8])
        selb = sb("selb", [128, NT, 8])
        TG = Tok("gate")
        lnd = mkrot(sb, "lnd", 2, [128, 512], F32)
        pT = mkrot(sb, "pT", 3, [128, 512], BF16)
        tmp = mkrot(sb, "tmp", 3, [128, 512], F32)
        rden = mkrot(sb, "rden", 2, [128, 512], F32)
        yst = mkrot(sb, "yst", 2, [128, 512], F32, key="st")
        sb_rot = Rot([(k.ps[i], k.tps[i]) for i in range(3)])
        o_rot = Rot([(k.ps[3], k.tps[3]), (k.ps[4], k.tps[4])])
        d_rot = Rot([(k.ps[5], k.tps[5]), (k.ps[6], k.tps[6])])
        misc, tmisc = k.ps[7], k.tps[7]

        P.op("pool", lambda e: e.dma_start(out=k2[:], in_=k.krT), reads=[k.t_kr], writes=[t_k2], dma_sem="k2")

        heads = []
        for h in range(H):
            heads.append(("moba", h))
        for h in range(H):
            heads.append(("mla", h))
        for h in range(H):
            heads.append(("fox", h))
        NHD = len(heads)
        HS = {}
        KCd = cfg.KC
        pc_per = (KCd + NHD - 1) // NHD
        wdv = I["w_down"][l].rearrange("(kc p) n -> p kc n", p=128)

        def prep_dma(hi):
            kind, h = heads[hi]
            stt = {}
            HS[hi] = stt
            if kind == "moba":
                qsrc = k.projT[cfg.oAq + h * 128:cfg.oAq + (h + 1) * 128, :]
                ksrc = k.projT[cfg.oAk + h * 128:cfg.oAk + (h + 1) * 128, :]
                vsrc = k.projT[cfg.oAv + h * 128:cfg.oAv + (h + 1) * 128, :]
                rtoks = [k.t_proj]
                stt["ydst"] = k.ycatT[h * 128:(h + 1) * 128, :]
                stt["slope"] = 2.0 ** (-8.0 * (h + 1) / H)
            elif kind == "mla":
                qsrc = k.qmT[h * 192:h * 192 + 128, :]
                ksrc = k.kvT[h * 256:h * 256 + 128, :]
                vsrc = k.kvT[h * 256 + 128:h * 256 + 256, :]
                rtoks = [k.t_qm, k.t_kv]
                stt["ydst"] = k.ycatT[2 * GW + h * 128:2 * GW + (h + 1) * 128, :]
            else:
                qsrc = k.projT[cfg.oDq + h * 128:cfg.oDq + (h + 1) * 128, :]
                ksrc = k.projT[cfg.oDk + h * 128:cfg.oDk + (h + 1) * 128, :]
                vsrc = k.projT[cfg.oDv + h * 128:cfg.oDv + (h + 1) * 128, :]
                rtoks = [k.t_proj]
                stt["ydst"] = k.ycatT[3 * GW + h * 128:3 * GW + (h + 1) * 128, :]
            v_, vt, vk = vf.next()
            stt["v"] = (v_, vt)
            P.op("sp", lambda e: e.dma_start(out=v_[:], in_=vsrc), reads=rtoks, writes=[vt], dma_sem=vk)
            qb_, qbt, qbk = qb.next()
            kb_, kbt, kbk = kb.next()
            stt["qb"] = (qb_, qbt)
            stt["kb"] = (kb_, kbt)
            q_, qt, qk = qf.next()
            k_, kt_, kk = kf.next()
            stt["qf"] = (q_, qt)
            stt["kf"] = (k_, kt_)
            P.op("sp", lambda e: e.dma_start(out=q_[:], in_=qsrc), reads=rtoks, writes=[qt], dma_sem=qk)
            P.op("sp", lambda e: e.dma_start(out=k_[:], in_=ksrc), reads=rtoks, writes=[kt_], dma_sem=kk)
            P.op("act", lambda e: e.activation(out=qb_[:], in_=q_[:], func=AF.Copy), reads=[qt], writes=[qbt])
            P.op("dve", lambda e: e.tensor_copy(out=kb_[:], in_=k_[:]), reads=[kt_], writes=[kbt])
            if kind == "mla":
                q2_, q2t, q2k = q2.next()
                q2f_, q2ft, q2fk = q2f.next()
                stt["q2"] = (q2_, q2t)
                P.op("sp", lambda e: e.dma_start(out=q2f_[:], in_=k.qmT[h * 192 + 128:h * 192 + 192, :]),
                     reads=[k.t_qm], writes=[q2ft], dma_sem=q2fk)
                P.op("dve", lambda e: e.tensor_copy(out=q2_[:], in_=q2f_[:]), reads=[q2ft], writes=[q2t])
            for j in range(hi * pc_per, min(KCd, (hi + 1) * pc_per)):
                P.op("pool", lambda e, j=j: e.dma_start(out=k.wdb[j].rearrange("p (kc n) -> p kc n", n=128),
                                                        in_=wdv[:, :, j * 128:(j + 1) * 128]),
                     writes=[k.t_wdb], dma_sem="pc%d" % (j % 2), partial=True)

        def prep_pe1(hi):
            kind, h = heads[hi]
            stt = HS[hi]
            if kind != "moba":
                return
            q_, qt = stt["qf"]
            k_, kt_ = stt["kf"]
            sT, sTt, _ = selbT.next()
            stt["sT"] = (sT, sTt)
            P.op("dve", lambda e: e.memset(kmean[:], 0.0), reads=[TG], writes=[TG])
            P.op("dve", lambda e: e.reduce_sum(out=kmean[:, 0:NBLK], in_=k_[:].rearrange("p (n b) -> p n b", b=256),
                                               axis=AX.X), reads=[kt_, TG], writes=[TG])
            P.op("dve", lambda e: e.memset(sT[:], 0.0), writes=[sTt])
            slope_ = stt["slope"]
            P.op("dve", lambda e: e.tensor_scalar_mul(out=sT[32:33, :], in0=arows[32:33, :], scalar1=-slope_),
                 reads=[T, sTt], writes=[sTt], partial=True)
            P.op("dve", lambda e: e.tensor_scalar_mul(out=sT[64:65, :], in0=arows[64:65, :], scalar1=-slope_),
                 reads=[T, sTt], writes=[sTt], partial=True)
            mb, mbt, _ = mbias.next()
            stt["mb"] = (mb, mbt)
            P.op("dve", lambda e: e.tensor_scalar_mul(out=mb[:], in0=iot16[:], scalar1=slope_), reads=[T], writes=[mbt])
            for tt in range(NT):
                P.op("pe", lambda e, tt=tt: e.matmul(misc[:, tt * 8:(tt + 1) * 8], lhsT=q_[:, tt * 128:(tt + 1) * 128],
                                                    rhs=kmean[:], start=True, stop=True),
                     reads=[qt, TG], writes=[tmisc], partial=(tt > 0))
            g8f = g8[:].rearrange("p a b -> p (a b)")
            P.op("dve", lambda e: e.tensor_tensor(out=g8f, in0=misc[:, 0:NT * 8], in1=vmask[:].rearrange("p a b -> p (a b)"),
                                                  op=ALU.mult), reads=[tmisc, T, TG], writes=[TG])
            P.op("dve", lambda e: e.tensor_tensor(out=g8f, in0=g8f, in1=nfill[:].rearrange("p a b -> p (a b)"), op=ALU.add),
                 reads=[TG, T], writes=[TG])
            P.op("dve", lambda e: e.tensor_tensor(out=cmp4[:], in0=g8[:].unsqueeze(2).to_broadcast([128, NT, 8, 8]),
                                                  in1=g8[:].unsqueeze(3).to_broadcast([128, NT, 8, 8]), op=ALU.is_gt),
                 reads=[TG], writes=[TG])
            P.op("dve", lambda e: e.reduce_sum(out=cnt8[:], in_=cmp4[:], axis=AX.X), reads=[TG], writes=[TG])
            P.op("dve", lambda e: e.tensor_scalar(out=selb[:], in0=cnt8[:], scalar1=3.0, scalar2=NEG, op0=ALU.is_ge,
                                                  op1=ALU.mult), reads=[TG], writes=[TG])
            P.op("dve", lambda e: e.tensor_tensor(out=selb[:], in0=selb[:], in1=vmask[:], op=ALU.mult), reads=[TG, T],
                 writes=[TG])

        def prep_pe2(hi):
            kind, h = heads[hi]
            stt = HS[hi]
            v_, vt = stt["v"]
            if kind == "moba":
                sT, sTt = stt["sT"]
                for g in range(NT // 4):
                    for i in range(4):
                        tt = g * 4 + i
                        P.op("pe", lambda e, tt=tt, i=i: e.transpose(out=misc[0:8, i * 128:(i + 1) * 128],
                                                                    in_=selb[:, tt, :], identity=k.ident[:]),
                             reads=[TG, k.t_const], writes=[tmisc], partial=(i > 0))
                    P.op("dve", lambda e, g=g: e.tensor_copy(out=sT[0:NBLK, g * 512:(g + 1) * 512], in_=misc[0:NBLK, :]),
                         reads=[tmisc, sTt], writes=[sTt], partial=True)
            vm, vmt, _ = vtm.next()
            stt["vm"] = (vm, vmt)
            for g in range(NT // 4):
                for i in range(4):
                    tt = g * 4 + i
                    P.op("pe", lambda e, tt=tt, i=i: e.transpose(out=misc[:, i * 128:(i + 1) * 128],
                                                                in_=v_[:, tt * 128:(tt + 1) * 128], identity=k.ident[:]),
                         reads=[vt, k.t_const], writes=[tmisc], partial=(i > 0))
                P.op("act", lambda e, g=g: e.activation(out=vm[:, g * 4:(g + 1) * 4, :].rearrange("p a b -> p (a b)"),
                                                        in_=misc[:], func=AF.Copy),
                     reads=[tmisc], writes=[vmt], partial=True)

        def make_blocks(hi):
            kind, h = heads[hi]
            stt = HS[hi]
            qb_, qbt = stt["qb"]
            kb_, kbt = stt["kb"]
            vm, vmt = stt["vm"]
            ydst = stt["ydst"]
            blks = []
            for c in range(NCH):
                qs = slice(c * 512, (c + 1) * 512)
                ops_, opt = o_rot.next()
                dps, dpt = d_rot.next()
                nkt = 4 * c + 4
                for kt in range(nkt):
                    ks = slice(kt * 128, (kt + 1) * 128)
                    sp_, spt = sb_rot.next()
                    p_, ptk, _ = pT.next()
                    diag = kt >= 4 * c
                    di = kt - 4 * c

                    def A(c=c, qs=qs, kt=kt, ks=ks, sp_=sp_, spt=spt, p_=p_, ptk=ptk, diag=diag, di=di):
                        extra = []
                        if kind == "mla":
                            q2_, q2t = stt["q2"]
                            extra.append((lambda e: e.matmul(sp_[:], lhsT=k2[:, ks], rhs=q2_[:, qs], start=False,
                                                             stop=True), [t_k2, q2t]))
                        elif kind == "fox":
                            for i3 in range(3):
                                extra.append((lambda e, i3=i3: e.matmul(sp_[:], lhsT=oh[h][0:H, :], rhs=cs3[i3][:, qs],
                                                                        start=False, stop=(i3 == 2)), [T, TF]))
                        else:
                            sT, sTt = stt["sT"]
                            n = kt // 2
                            extra.append((lambda e: e.matmul(sp_[:], lhsT=ohm[n][0:65, :], rhs=sT[0:65, qs],
                                                             start=False, stop=True), [T, sTt]))
                        P.op("pe", lambda e: e.matmul(sp_[:], lhsT=kb_[:, ks], rhs=qb_[:, qs], start=True,
                                                      stop=(len(extra) == 0)), reads=[kbt, qbt], writes=[spt])
                        for fn, rd in extra:
                            P.op("pe", fn, reads=rd, writes=[spt], partial=True)
                        src, srct = sp_, spt
                        if diag:
                            t_, tt_, _ = tmp.next()
                            P.op("dve", lambda e: e.tensor_tensor(out=t_[:], in0=sp_[:], in1=negm[di][:], op=ALU.add),
                                 reads=[spt, T], writes=[tt_])
                            src, srct = t_, tt_
                        if kind == "fox":
                            P.op("act", lambda e: e.activation(out=p_[:], in_=src[:], func=AF.Exp,
                                                               bias=cs_tm[:, kt, h:h + 1]),
                                 reads=[srct, TF], writes=[ptk])
                        elif kind == "moba":
                            mb, mbt = stt["mb"]
                            P.op("act", lambda e: e.activation(out=p_[:], in_=src[:], func=AF.Exp, bias=mb[:, kt:kt + 1]),
                                 reads=[srct, mbt], writes=[ptk])
                        else:
                            P.op("act", lambda e: e.activation(out=p_[:], in_=src[:], func=AF.Exp), reads=[srct],
                                 writes=[ptk])

                    def B(c=c, qs=qs, kt=kt, p_=p_, ptk=ptk, nkt=nkt, ops_=ops_, opt=opt, dps=dps, dpt=dpt):
                        P.op("pe", lambda e: e.matmul(ops_[:], lhsT=vm[:, kt, :], rhs=p_[:], start=(kt == 0),
                                                      stop=(kt == nkt - 1)),
                             reads=[vmt, ptk], writes=[opt], partial=(kt > 0))
                        P.op("pe", lambda e: e.matmul(dps[:], lhsT=k.ones_bf[:], rhs=p_[:], start=(kt == 0),
                                                      stop=(kt == nkt - 1)),
                             reads=[ptk, k.t_const], writes=[dpt], partial=(kt > 0))
                        if kt == nkt - 1:
                            rd, rdt, _ = rden.next()
                            y_, yt, yk = yst.next()
                            ld_, ldt, _ = lnd.next()
                            P.op("act", lambda e: e.activation(out=ld_[:], in_=dps[:], func=AF.Ln), reads=[dpt],
                                 writes=[ldt])
                            P.op("act", lambda e: e.activation(out=rd[:], in_=ld_[:], func=AF.Exp, scale=-1.0),
                                 reads=[ldt], writes=[rdt])
                            P.op("dve", lambda e: e.tensor_tensor(out=y_[:], in0=ops_[:], in1=rd[:], op=ALU.mult),
                                 reads=[opt, rdt], writes=[yt])
                            P.op("sp", lambda e: e.dma_start(out=ydst[:, qs], in_=y_[:]), reads=[yt], writes=[k.t_ycat],
                                 dma_sem=yk, partial=True)

                    blks.append((A, B))
            return blks

        LA = 2
        PE1_AT = 10
        PE2_AT = sum(4 * c + 4 for c in range(NCH - 1))
        pending = []
        fox_prep()
        prep_dma(0)
        prep_pe1(0)
        prep_pe2(0)
        for hi in range(NHD):
            if hi + 1 < NHD:
                prep_dma(hi + 1)
            blks = make_blocks(hi)
            for bi, (A, B) in enumerate(blks):
                if bi == PE1_AT and hi + 1 < NHD:
                    prep_pe1(hi + 1)
                if bi == PE2_AT and hi + 1 < NHD:
                    if heads[hi + 1][0] == "fox" and heads[hi][0] != "fox":
                        fox_prep_pe()
                    prep_pe2(hi + 1)
                A()
                pending.append(B)
                if len(pending) > LA:
                    pending.pop(0)()
        while pending:
            pending.pop(0)()


def phase_gmlp(k, l):
    cfg, P, I = k.cfg, k.P, k.I
    S, H, GW, NCH, NT = cfg.S, cfg.H, cfg.GW, cfg.NCH, cfg.NT
    st, sb = new_phase(k)
    with st:
        lg, lgt = load_vec_fm(k, sb, "lng", I["sgu_ln_g"][l].rearrange("h d -> (h d)"), H)
        lb, lbt = load_vec_fm(k, sb, "lnb", I["sgu_ln_b"][l].rearrange("h d -> (h d)"), H)
        T = Tok("gm")
        io = sb("io", [128, 128])
        m01 = sb("m01", [128, 128])
        P.op("pool", lambda e: e.iota(io[:], pattern=[[1, 128]], base=0, channel_multiplier=-1,
                                      allow_small_or_imprecise_dtypes=True), writes=[T])
        P.op("dve", lambda e: e.tensor_single_scalar(out=m01[:], in_=io[:], scalar=0.0, op=ALU.is_ge), reads=[T],
             writes=[T])
        uf = mkrot(sb, "uf", 3, [128, S], F32, key="hq")
        vf = mkrot(sb, "vf", 3, [128, S], F32, key="hk")
        ws = mkrot(sb, "ws", 3, [128, 128], F32, key="hv")
        bsr = mkrot(sb, "bsr", 3, [1, 128], F32, key="hqb")
        wmT = mkrot(sb, "wmT", 3, [128, 128], BF16)
        bhi = mkrot(sb, "bhi", 3, [1, 128], BF16)
        blo = mkrot(sb, "blo", 3, [1, 128], BF16)
        bfl = mkrot(sb, "bfl", 3, [1, 128], F32)
        vb = mkrot(sb, "vb", 4, [128, 512], BF16)
        sqb = mkrot(sb, "sqb", 4, [128, 512], BF16)
        mean = mkrot(sb, "mean", 4, [128, 512], F32)
        m2 = mkrot(sb, "m2", 4, [128, 512], F32)
        rstd = mkrot(sb, "rstd", 4, [128, 512], F32)
        vn = mkrot(sb, "vn", 8, [128, 512], F32)
        vtm = mkrot(sb, "vtm", 4, [128, 4, 128], BF16)
        yst = mkrot(sb, "yst", 3, [128, 512], F32, key="st")
        b_mean = Rot([(k.ps[0], k.tps[0]), (k.ps[1], k.tps[1])])
        b_msq = Rot([(k.ps[2], k.tps[2]), (k.ps[3], k.tps[3])])
        b_tr = Rot([(k.ps[4], k.tps[4]), (k.ps[5], k.tps[5])])
        b_out = Rot([(k.ps[6], k.tps[6]), (k.ps[7], k.tps[7])])
        HSt = {}

        def loads(g):
            stt = {}
            HSt[g] = stt
            u_, ut, uk = uf.next()
            v_, vt, vk = vf.next()
            w_, wt_, wk = ws.next()
            b_, bt_, bk = bsr.next()
            stt.update(u=(u_, ut), v=(v_, vt), w=(w_, wt_), b=(b_, bt_))
            P.op("sp", lambda e: e.dma_start(out=u_[:], in_=k.projT[cfg.oBu + g * 128:cfg.oBu + (g + 1) * 128, :]),
                 reads=[k.t_proj], writes=[ut], dma_sem=uk)
            P.op("sp", lambda e: e.dma_start(out=v_[:], in_=k.projT[cfg.oBv + g * 128:cfg.oBv + (g + 1) * 128, :]),
                 reads=[k.t_proj], writes=[vt], dma_sem=vk)
            P.op("sp", lambda e: e.dma_start(out=w_[:], in_=I["sgu_w"][l, g]), writes=[wt_], dma_sem=wk)
            P.op("sp", lambda e: e.dma_start(out=b_[:], in_=I["sgu_b"][l, g].rearrange("(o t) -> o t", o=1)),
                 writes=[bt_], dma_sem=bk)
            bh, bht, _ = bhi.next()
            bl, blt, _ = blo.next()
            bf_, bft, _ = bfl.next()
            stt.update(bh=(bh, bht), bl=(bl, blt))
            P.op("dve", lambda e: e.tensor_copy(out=bh[:], in_=b_[:]), reads=[bt_], writes=[bht])
            P.op("dve", lambda e: e.tensor_copy(out=bf_[:], in_=bh[:]), reads=[bht], writes=[bft])
            P.op("dve", lambda e: e.tensor_tensor(out=bf_[:], in0=b_[:], in1=bf_[:], op=ALU.subtract), reads=[bt_, bft],
                 writes=[bft])
            P.op("dve", lambda e: e.tensor_copy(out=bl[:], in_=bf_[:]), reads=[bft], writes=[blt])

        def front(g):
            stt = HSt[g]
            v_, vt = stt["v"]
            w_, wt_ = stt["w"]
            wm, wmt, _ = wmT.next()
            stt["wm"] = (wm, wmt)
            trp, trt = b_tr.next()
            P.op("pe", lambda e: e.transpose(out=trp[:, 0:128], in_=w_[:], identity=k.ident[:]),
                 reads=[wt_, k.t_const], writes=[trt])
            P.op("dve", lambda e: e.tensor_tensor(out=wm[:], in0=trp[:, 0:128], in1=m01[:], op=ALU.mult),
                 reads=[trt, T], writes=[wmt])
            cst = [dict() for _ in range(NCH)]
            stt["c"] = cst
            for c in range(NCH):
                cs = slice(c * 512, (c + 1) * 512)
                vb_, vbt, _ = vb.next()
                sq_, sqt, _ = sqb.next()
                cst[c].update(vb=(vb_, vbt), sq=(sq_, sqt))
                P.op("act", lambda e, vb_=vb_, cs=cs: e.activation(out=vb_[:], in_=v_[:, cs], func=AF.Copy), reads=[vt],
                     writes=[vbt])
                P.op("act", lambda e, sq_=sq_, cs=cs: e.activation(out=sq_[:], in_=v_[:, cs], func=AF.Square), reads=[vt],
                     writes=[sqt])
            for c in range(NCH):
                vb_, vbt = cst[c]["vb"]
                sq_, sqt = cst[c]["sq"]
                pm, pmt = b_mean.next()
                pq, pqt = b_msq.next()
                P.op("pe", lambda e, pm=pm, vb_=vb_: e.matmul(pm[:], lhsT=k.ones_bf[:], rhs=vb_[:], start=True, stop=True),
                     reads=[vbt, k.t_const], writes=[pmt])
                P.op("pe", lambda e, pq=pq, sq_=sq_: e.matmul(pq[:], lhsT=k.ones_bf[:], rhs=sq_[:], start=True, stop=True),
                     reads=[sqt, k.t_const], writes=[pqt])
                mn, mnt, _ = mean.next()
                mm, mmt, _ = m2.next()
                cst[c].update(mn=(mn, mnt), mm=(mm, mmt))
                P.op("act", lambda e, mn=mn, pm=pm: e.activation(out=mn[:], in_=pm[:], func=AF.Copy, scale=1.0 / 128.0),
                     reads=[pmt], writes=[mnt])
                P.op("dve", lambda e, mm=mm, mn=mn: e.tensor_tensor(out=mm[:], in0=mn[:], in1=mn[:], op=ALU.mult),
                     reads=[mnt], writes=[mmt])
                P.op("dve", lambda e, mm=mm, pq=pq: e.scalar_tensor_tensor(out=mm[:], in0=pq[:], scalar=1.0 / 128.0,
                                                                           in1=mm[:], op0=ALU.mult, op1=ALU.subtract),
                     reads=[pqt, mmt], writes=[mmt])

        def front2(g):
            stt = HSt[g]
            v_, vt = stt["v"]
            cst = stt["c"]
            for c in range(NCH):
                mm, mmt = cst[c]["mm"]
                rs, rst, _ = rstd.next()
                cst[c]["rs"] = (rs, rst)
                P.op("act", lambda e, rs=rs, mm=mm: e.activation(out=rs[:], in_=mm[:], func=AF.Ln, bias=EPS), reads=[mmt],
                     writes=[rst])
            for c in range(NCH):
                rs, rst = cst[c]["rs"]
                P.op("act", lambda e, rs=rs: e.activation(out=rs[:], in_=rs[:], func=AF.Exp, scale=-0.5), reads=[rst],
                     writes=[rst])
            for c in range(NCH):
                cs = slice(c * 512, (c + 1) * 512)
                mn, mnt = cst[c]["mn"]
                rs, rst = cst[c]["rs"]
                vn_, vnt, _ = vn.next()
                cst[c]["vn"] = (vn_, vnt)
                P.op("dve", lambda e, vn_=vn_, cs=cs, mn=mn: e.tensor_tensor(out=vn_[:], in0=v_[:, cs], in1=mn[:],
                                                                             op=ALU.subtract),
                     reads=[vt, mnt], writes=[vnt])
                P.op("dve", lambda e, vn_=vn_, rs=rs: e.tensor_tensor(out=vn_[:], in0=vn_[:], in1=rs[:], op=ALU.mult),
                     reads=[vnt, rst], writes=[vnt])
                P.op("dve", lambda e, vn_=vn_: e.tensor_scalar(out=vn_[:], in0=vn_[:], scalar1=lg[:, g:g + 1],
                                                               scalar2=lb[:, g:g + 1], op0=ALU.mult, op1=ALU.add),
                     reads=[vnt, lgt, lbt], writes=[vnt])

        def back(g):
            stt = HSt[g]
            u_, ut = stt["u"]
            bh, bht = stt["bh"]
            bl, blt = stt["bl"]
            wm, wmt = stt["wm"]
            cst = stt["c"]
            for c in range(NCH):
                vn_, vnt = cst[c]["vn"]
                trp, trt = b_tr.next()
                for i in range(4):
                    P.op("pe", lambda e, trp=trp, vn_=vn_, i=i: e.transpose(out=trp[:, i * 128:(i + 1) * 128],
                                                                           in_=vn_[:, i * 128:(i + 1) * 128],
                                                                           identity=k.ident[:]),
                         reads=[vnt, k.t_const], writes=[trt], partial=(i > 0))
                vm, vmt, _ = vtm.next()
                cst[c]["vm"] = (vm, vmt)
                P.op("act", lambda e, vm=vm, trp=trp: e.activation(out=vm[:].rearrange("p a b -> p (a b)"), in_=trp[:],
                                                                  func=AF.Copy), reads=[trt], writes=[vmt])

        def back2(g):
            stt = HSt[g]
            u_, ut = stt["u"]
            bh, bht = stt["bh"]
            bl, blt = stt["bl"]
            wm, wmt = stt["wm"]
            cst = stt["c"]
            for c in range(NCH):
                cs = slice(c * 512, (c + 1) * 512)
                vm, vmt = cst[c]["vm"]
                po, pot = b_out.next()
                for i in range(4):
                    P.op("pe", lambda e, po=po, vm=vm, i=i: e.matmul(po[:, i * 128:(i + 1) * 128], lhsT=vm[:, i, :],
                                                                    rhs=wm[:], start=True, stop=False),
                         reads=[vmt, wmt], writes=[pot], partial=(i > 0))
                    P.op("pe", lambda e, po=po, i=i: e.matmul(po[:, i * 128:(i + 1) * 128], lhsT=k.ones_bf[0:1, :],
                                                             rhs=bh[0:1, :], start=False, stop=False),
                         reads=[bht, k.t_const], writes=[pot], partial=True)
                    P.op("pe", lambda e, po=po, i=i: e.matmul(po[:, i * 128:(i + 1) * 128], lhsT=k.ones_bf[0:1, :],
                                                             rhs=bl[0:1, :], start=False, stop=True),
                         reads=[blt, k.t_const], writes=[pot], partial=True)
                y_, yt, yk = yst.next()
                P.op("dve", lambda e, y_=y_, po=po, cs=cs: e.tensor_tensor(out=y_[:], in0=po[:], in1=u_[:, cs], op=ALU.mult),
                     reads=[pot, ut], writes=[yt])
                P.op("sp", lambda e, y_=y_, cs=cs: e.dma_start(out=k.ycatT[GW + g * 128:GW + (g + 1) * 128, cs], in_=y_[:]),
                     reads=[yt], writes=[k.t_ycat], dma_sem=yk, partial=True)

        loads(0)
        if H > 1:
            loads(1)
        front(0)
        front2(0)
        for g in range(H):
            if g + 2 < H:
                loads(g + 2)
            if g + 1 < H:
                front(g + 1)
            back(g)
            if g + 1 < H:
                front2(g + 1)
            back2(g)


def phase_wo(k, l):
    cfg, P, I = k.cfg, k.P, k.I
    S, D, H, GW, NCH, KC = cfg.S, cfg.D, cfg.H, cfg.GW, cfg.NCH, cfg.KC
    KM = cfg.DMIX // 128
    st, sb = new_phase(k)
    with st:
        ynT = sb("ynT", [128, KM, S], BF16)
        t_yn = Tok("ynT")
        gg, ggt = load_vec_fm(k, sb, "gg", I["group_norm_g"][l].rearrange("a b -> (a b)"), KM)
        for gi in range(4):
            with ExitStack() as st2:
                def sb2(name, shape, dt=F32, st2=st2, gi=gi):
                    return st2.enter_context(k.nc.sbuf_tensor("gn_%d_%d_%s" % (l, gi, name), list(shape), dt))
                rms_norm_fm(k, sb2, lambda g0, n, c, gi=gi: (k.ycatT[(gi * H + g0) * 128:(gi * H + g0 + n) * 128,
                                                                    c * 512:(c + 1) * 512].rearrange(
                    "(g p) t -> p g t", p=128), [k.t_ycat]), H, GW,
                            gg[:, gi * H:(gi + 1) * H], ggt,
                            lambda kc, c, gi=gi: (ynT[:, gi * H + kc, c * 512:(c + 1) * 512], t_yn), NCH, tagp="g%d" % gi)
                P.barrier()
        wrot = mkrot(sb, "w", 3, [128, KM, 128], BF16)
        banks = Rot([(k.ps[i], k.tps[i]) for i in range(4)])
        ssq = SumSq(k, sb, lambda c: (k.ps[4 + c], k.tps[4 + c]), KC)
        ev = make_resid_evac(k, sb, lambda c: c * 512, ssq)
        tiles = [([(j * 128, 128)], 128) for j in range(KC)]
        linear(k, I["w_o"][l], KM, tiles, lambda kc, c: (ynT[:, kc, c * 512:(c + 1) * 512], t_yn), NCH, ev, wrot, banks)
        ssq.flush()


def phase_ffn_up(k, l):
    cfg, P, I = k.cfg, k.P, k.I
    S, D, NCH, KC, NF = cfg.S, cfg.D, cfg.NCH, cfg.KC, cfg.NF
    st, sb = new_phase(k)
    with st:
        hT = sb("h2T", [128, KC, S], BF16)
        t_h = Tok("h2T")
        gT, gtok = load_vec_fm(k, sb, "g2", I["norm_ffn_g"][l], KC)
        with ExitStack() as st2:
            def sb2(name, shape, dt=F32):
                return st2.enter_context(k.nc.sbuf_tensor("n2_%d_%s" % (l, name), list(shape), dt))
            rms_norm_fm(k, sb2, lambda g0, n, c: (k.xT[g0 * 128:(g0 + n) * 128, c * 512:(c + 1) * 512].rearrange("(g p) t -> p g t", p=128), [k.t_xT[g0 + i] for i in range(n)]), KC, D,
                        gT, gtok, lambda kc, c: (hT[:, kc, c * 512:(c + 1) * 512], t_h), NCH, rstd_pre=True)
            P.barrier()
        cw = []
        for j in range(3):
            cw.append(load_vec_fm(k, sb, "cw%d" % j, I["conv_w"][l, j], NF))
        cb, cbt = load_vec_fm(k, sb, "cb", I["conv_b"][l], NF)
        wrot = mkrot(sb, "w", 4, [128, KC, 128], BF16)
        bg = Rot([(k.ps[i], k.tps[i]) for i in range(0, 3)])
        bv = Rot([(k.ps[i], k.tps[i]) for i in range(3, 6)])
        gbuf = mkrot(sb, "gbuf", 2, [128, S + 2], F32)
        for gb, gbt, _ in gbuf.items:
            P.op("dve", lambda e, gb=gb: e.memset(gb[:, 0:2], 0.0), writes=[gbt])
        t1 = mkrot(sb, "t1", 2, [128, 512], F32)
        ast = mkrot(sb, "ast", 3, [128, 512], BF16, key="st")
        wvg = I["w_gate"][l].rearrange("(kc p) n -> p kc n", p=128)
        wvv = I["w_val"][l].rearrange("(kc p) n -> p kc n", p=128)
        for f in range(NF):
            wg, wgt, wgk = wrot.next()
            wv_, wvt, wvk = wrot.next()
            fs = slice(f * 128, (f + 1) * 128)
            P.op("pool", lambda e, wg=wg, fs=fs: e.dma_start(out=wg[:], in_=wvg[:, :, fs]), writes=[wgt], dma_sem=wgk)
            P.op("pool", lambda e, wv_=wv_, fs=fs: e.dma_start(out=wv_[:], in_=wvv[:, :, fs]), writes=[wvt], dma_sem=wvk)
            gb, gbt, _ = gbuf.next()
            for c in range(NCH):
                cs = slice(c * 512, (c + 1) * 512)
                pg, pgt = bg.next()
                pv, pvt = bv.next()
                for kc in range(KC):
                    P.op("pe", lambda e, pg=pg, wg=wg, kc=kc, cs=cs: e.matmul(pg[:], lhsT=wg[:, kc, :], rhs=hT[:, kc, cs],
                                                                             start=(kc == 0), stop=(kc == KC - 1)),
                         reads=[wgt, t_h], writes=[pgt])
                for kc in range(KC):
                    P.op("pe", lambda e, pv=pv, wv_=wv_, kc=kc, cs=cs: e.matmul(pv[:], lhsT=wv_[:, kc, :], rhs=hT[:, kc, cs],
                                                                               start=(kc == 0), stop=(kc == KC - 1)),
                         reads=[wvt, t_h], writes=[pvt])
                c0 = c * 512
                P.op("act", lambda e, gb=gb, pg=pg, c0=c0: e.activation(out=gb[:, 2 + c0:2 + c0 + 512], in_=pg[:],
                                                                       func=AF.Copy), reads=[pgt], writes=[gbt], partial=True)
                t_, tt_, _ = t1.next()
                P.op("dve", lambda e, t_=t_, gb=gb, c0=c0, f=f: e.tensor_scalar(
                    out=t_[:], in0=gb[:, 2 + c0:2 + c0 + 512], scalar1=cw[2][0][:, f:f + 1], scalar2=cb[:, f:f + 1],
                    op0=ALU.mult, op1=ALU.add), reads=[gbt, cw[2][1], cbt], writes=[tt_])
                P.op("dve", lambda e, t_=t_, gb=gb, c0=c0, f=f: e.scalar_tensor_tensor(
                    out=t_[:], in0=gb[:, 1 + c0:1 + c0 + 512], scalar=cw[1][0][:, f:f + 1], in1=t_[:], op0=ALU.mult,
                    op1=ALU.add), reads=[gbt, cw[1][1], tt_], writes=[tt_])
                P.op("dve", lambda e, t_=t_, gb=gb, c0=c0, f=f: e.scalar_tensor_tensor(
                    out=t_[:], in0=gb[:, c0:c0 + 512], scalar=cw[0][0][:, f:f + 1], in1=t_[:], op0=ALU.mult,
                    op1=ALU.add), reads=[gbt, cw[0][1], tt_], writes=[tt_])
                P.op("act", lambda e, t_=t_: e.activation(out=t_[:], in_=t_[:], func=AF.Silu), reads=[tt_], writes=[tt_])
                a_, at_, ak = ast.next()
                P.op("dve", lambda e, a_=a_, t_=t_, pv=pv: e.tensor_tensor(out=a_[:], in0=pv[:], in1=t_[:], op=ALU.mult),
                     reads=[pvt, tt_], writes=[at_])
                P.op("sp", lambda e, a_=a_, fs=fs, cs=cs: e.dma_start(out=k.aT[fs, cs], in_=a_[:]), reads=[at_],
                     writes=[k.t_aT], dma_sem=ak, partial=True)


def phase_ffn_down(k, l):
    cfg, P, I = k.cfg, k.P, k.I
    S, D, NCH, KC, NF = cfg.S, cfg.D, cfg.NCH, cfg.KC, cfg.NF
    st, sb = new_phase(k)
    with st:
        aTc = sb("aTc", [128, NF, 512], BF16)
        nsp = 6
        per = (NF + nsp - 1) // nsp
        t_ap = [Tok("aTc%d" % i) for i in range(nsp)]
        wrot = mkrot(sb, "w", 3, [128, NF, 128], BF16)
        banks = Rot([(k.ps[i], k.tps[i]) for i in range(6)])
        tiles = [([(j * 128, 128)], 128) for j in range(KC)]
        cur_c = [0]
        ssq = SumSq(k, sb, lambda c: (k.ps[6 + c % 2], k.tps[6 + c % 2]), KC)
        ev = make_resid_evac(k, sb, lambda cc: cur_c[0] * 512, ssq)
        for c in range(NCH):
            src = k.aT[:, c * 512:(c + 1) * 512].rearrange("(f p) t -> p f t", p=128)
            for i in range(nsp):
                f0, f1 = i * per, min(NF, (i + 1) * per)
                if f0 >= f1:
                    continue
                P.op("sp" if i % 2 == 0 else "pool",
                     lambda e, f0=f0, f1=f1, src=src: e.dma_start(out=aTc[:, f0:f1, :], in_=src[:, f0:f1, :]),
                     reads=[k.t_aT], writes=[t_ap[i]], dma_sem="aTc%d" % i)
            cur_c[0] = c
            linear(k, None, NF, tiles, lambda kc, cc: (aTc[:, kc, :], t_ap[kc // per]), 1, ev, wrot, banks,
                   wsrc=lambda j: k.wdb[j].rearrange("p (kc n) -> p kc n", n=128), wq="act", wreads=[k.t_wdb])
            ssq.flush()


def make_resid_evac_c(k, sb, c):
    if not hasattr(k, "_dn_rot") or k._dn_phase != k.cnt:
        k._dn_phase = k.cnt
        k._dn_rot = (mkrot(sb, "xr", 3, [128, 512], F32), mkrot(sb, "xo", 3, [128, 512], F32))
    xr, xo = k._dn_rot
    P = k.P

    def ev(j, cc, ps, pt, M):
        dst = k.xT[j * 128:(j + 1) * 128, c * 512:(c + 1) * 512]
        r, rt, rk = xr.next()
        o, ot, ok = xo.next()
        P.op("sp", lambda e: e.dma_start(out=r[:], in_=dst), reads=[k.t_xT[j]], writes=[rt], dma_sem=rk)
        P.op("dve", lambda e: e.tensor_tensor(out=o[:], in0=ps[:], in1=r[:], op=ALU.add), reads=[pt, rt], writes=[ot])
        P.op("sp", lambda e: e.dma_start(out=dst, in_=o[:]), reads=[ot], writes=[k.t_xT[j]], dma_sem=ok, partial=True)

    return ev


def phase_final(k):
    cfg, P, I = k.cfg, k.P, k.I
    S, D, NCH, KC = cfg.S, cfg.D, cfg.NCH, cfg.KC
    st, sb = new_phase(k)
    with st:
        gT, gtok = load_vec_fm(k, sb, "gf", I["final_norm_g"], KC)
        G = min(4, KC)
        xr = mkrot(sb, "nx", 3, [128, G, 512], F32)
        sq = mkrot(sb, "nsq", 3, [128, 512], BF16)
        rs = mkrot(sb, "nrs", 2, [128, 512], F32)
        yn = mkrot(sb, "yn", 3, [128, 512], F32)
        ost = mkrot(sb, "ost", 4, [128, 4, 128], F32, key="st")
        sbank = Rot([(k.ps[6], k.tps[6]), (k.ps[7], k.tps[7])])
        tbank = Rot([(k.ps[i], k.tps[i]) for i in range(6)])
        groups = [(g0, min(G, KC - g0)) for g0 in range(0, KC, G)]

        def src_of(g0, n, c):
            return k.xT[g0 * 128:(g0 + n) * 128, c * 512:(c + 1) * 512].rearrange("(g p) t -> p g t", p=128)

        cnt = 0
        for c in range(NCH):
            r, rt = k.rk[:, c, :], k.t_rk
            for (g0, n) in groups:
                x, xt, xk = xr.next()
                src = src_of(g0, n, c)
                P.op("sp", lambda e, x=x, src=src, n=n: e.dma_start(out=x[:, 0:n, :], in_=src),
                     reads=[k.t_xT[g0 + i] for i in range(n)], writes=[xt], dma_sem=xk)
                for i in range(n):
                    kc = g0 + i
                    y, yt, _ = yn.next()
                    P.op("dve", lambda e, x=x, y=y, kc=kc, r=r, i=i: e.scalar_tensor_tensor(
                        out=y[:], in0=x[:, i, :], scalar=gT[:, kc:kc + 1], in1=r, op0=ALU.mult, op1=ALU.mult),
                         reads=[xt, rt, gtok], writes=[yt])
                    tp, tpt = tbank.next()
                    for q in range(4):
                        P.op("pe", lambda e, tp=tp, y=y, q=q: e.transpose(out=tp[:, q * 128:(q + 1) * 128],
                                                                         in_=y[:, q * 128:(q + 1) * 128],
                                                                         identity=k.ident[:]),
                             reads=[yt, k.t_const], writes=[tpt], partial=(q > 0))
                    o, ot, ok = ost.next()
                    cnt += 1
                    if cnt % 3 == 0:
                        P.op("dve", lambda e, o=o, tp=tp: e.tensor_copy(out=o[:].rearrange("p a b -> p (a b)"), in_=tp[:]),
                             reads=[tpt], writes=[ot])
                    else:
                        P.op("act", lambda e, o=o, tp=tp: e.activation(out=o[:].rearrange("p a b -> p (a b)"), in_=tp[:],
                                                                      func=AF.Copy), reads=[tpt], writes=[ot])
                    dst = k.out[c * 512:(c + 1) * 512, kc * 128:(kc + 1) * 128].rearrange("(i p) f -> p i f", p=128)
                    P.op("act", lambda e, dst=dst, o=o: e.dma_start(out=dst, in_=o[:]), reads=[ot], writes=[k.t_out],
                         dma_sem=ok, partial=True)


_NC_CACHE = {}


def kernel(**inputs):
    cfg = Cfg()
    if "nc" not in _NC_CACHE:
        _NC_CACHE["nc"] = build_program(cfg)
    nc = _NC_CACHE["nc"]
    x = np.ascontiguousarray(inputs["x"], dtype=np.float32)
    B = x.shape[0]
    shared = {n: np.ascontiguousarray(inputs[n], dtype=np.float32) for n in inputs if n != "x"}
    in_maps = []
    for b in range(B):
        m = dict(shared)
        m["x"] = x[b]
        in_maps.append(m)
    res = run_bass_kernel_spmd(nc, in_maps, core_ids=list(range(B)))
    return np.stack([r["out"] for r in res.results], axis=0).astype(np.float32)
```

```python
import math
import numpy as np
from contextlib import ExitStack
import concourse.bass as bass
import concourse.mybir as mybir
from concourse.bass_utils import run_bass_kernel_spmd

F32 = mybir.dt.float32
BF16 = mybir.dt.bfloat16
I32 = mybir.dt.int32
AF = mybir.ActivationFunctionType
ALU = mybir.AluOpType
AX = mybir.AxisListType

COMPUTE = ("pe", "act", "dve", "pool")
ALL_ENG = ("pe", "act", "dve", "pool", "sp")
EPS = 1e-6
NEG = -30000.0


class Tok:
    __slots__ = ("name", "writers", "readers")

    def __init__(self, name=""):
        self.name = name
        self.writers = {}
        self.readers = {}


class Op:
    __slots__ = ("eng", "fn", "deps", "marked", "milestone", "dma_sem", "dma_val", "idx", "key")


class Prog:
    def __init__(self, nc):
        self.nc = nc
        self.ops = {e: [] for e in ALL_ENG}
        self.dma_sem_count = {}
        self.last = {}
        self.barrier_deps = {e: {} for e in ALL_ENG}
        self.n = 0

    def op(self, eng, fn, reads=(), writes=(), dma_sem=None, partial=False):
        o = Op()
        o.eng, o.fn, o.marked, o.milestone = eng, fn, False, None
        o.dma_sem, o.dma_val = dma_sem, None
        o.idx = self.n
        self.n += 1
        o.key = ("dma", dma_sem) if dma_sem is not None else eng
        deps = {}

        def add(d):
            for kk, dd in d.items():
                cur = deps.get(kk)
                if cur is None or cur.idx < dd.idx:
                    deps[kk] = dd

        if self.barrier_deps[eng]:
            add(self.barrier_deps[eng])
            self.barrier_deps[eng] = {}
        for r in reads:
            add(r.writers)
        for w in writes:
            if not partial:
                add(w.writers)
            add(w.readers)
        for r in reads:
            r.readers[o.key] = o
        for w in writes:
            if partial:
                w.writers[o.key] = o
            else:
                w.writers = {o.key: o}
            w.readers = {}
        if dma_sem is not None:
            v = self.dma_sem_count.get(dma_sem, 0) + 16
            self.dma_sem_count[dma_sem] = v
            o.dma_val = v
        fd = []
        for kk, d in deps.items():
            if d is o:
                continue
            if d.dma_sem is None:
                if d.eng == eng and eng == "pe":
                    continue
                d.marked = True
            fd.append(d)
        o.deps = fd
        self.ops[eng].append(o)
        self.last[o.key] = o
        return o

    def barrier(self):
        for e in ALL_ENG:
            self.barrier_deps[e] = dict(self.last)

    def emit(self):
        nc = self.nc
        with ExitStack() as st:
            esem = {e: st.enter_context(nc.semaphore("s_" + e)) for e in COMPUTE}
            dsem = {k: st.enter_context(nc.semaphore("d_%s" % (k,))) for k in self.dma_sem_count}
            for e in COMPUTE:
                c = 0
                for o in self.ops[e]:
                    if o.dma_sem is None and o.marked:
                        c += 1
                        o.milestone = c
                assert c < 60000, (e, c)
            for k_, v_ in self.dma_sem_count.items():
                assert v_ < 60000, (k_, v_)
            block = st.enter_context(nc.Block())

            def gen(ename):
                def body(eng):
                    seen = {}
                    for o in self.ops[ename]:
                        need = {}
                        for d in o.deps:
                            if d.dma_sem is not None:
                                s, v = dsem[d.dma_sem], d.dma_val
                            else:
                                s, v = esem[d.eng], d.milestone
                            key = id(s)
                            if seen.get(key, 0) >= v:
                                continue
                            if key not in need or need[key][1] < v:
                                need[key] = (s, v)
                        for key, (s, v) in need.items():
                            eng.wait_ge(s, v)
                            seen[key] = v
                        inst = o.fn(eng)
                        if o.dma_sem is not None:
                            inst.then_inc(dsem[o.dma_sem], 16)
                        elif o.marked:
                            inst.then_inc(esem[ename], 1)
                    if ename == "sp":
                        for k, v in self.dma_sem_count.items():
                            eng.wait_ge(dsem[k], v)
                        for e2 in COMPUTE:
                            m = 0
                            for o in self.ops[e2]:
                                if o.milestone:
                                    m = o.milestone
                            if m:
                                eng.wait_ge(esem[e2], m)
                return body

            block.tensor(gen("pe"))
            block.scalar(gen("act"))
            block.vector(gen("dve"))
            block.gpsimd(gen("pool"))
            block.sync(gen("sp"))


class Rot:
    def __init__(self, items):
        self.items = items
        self.i = 0

    def next(self):
        it = self.items[self.i % len(self.items)]
        self.i += 1
        return it


class Cfg:
    def __init__(self, S=2048, D=4096, H=8, QL=768, KVL=256, DFF=11008, L=2, theta=10000.0):
        self.S, self.D, self.H, self.QL, self.KVL, self.DFF, self.L, self.theta = S, D, H, QL, KVL, DFF, L, theta
        self.GW = H * 128
        self.DMIX = 4 * self.GW
        self.KC = D // 128
        self.NCH = S // 512
        self.NT = S // 128
        self.NF = DFF // 128
        GW = self.GW
        self.oAq, self.oAk, self.oAv, self.oBu, self.oBv = 0, GW, 2 * GW, 3 * GW, 4 * GW
        self.oCq = 5 * GW
        self.oCkv = self.oCq + QL
        self.oCkr = self.oCkv + KVL
        self.oDq = self.oCkr + 64
        self.oDk = self.oDq + GW
        self.oDv = self.oDk + GW
        self.oDf = self.oDv + GW
        self.INC = self.oDf + H
        self.NBLK = S // 256


class K:
    pass


def build_program(cfg, dbg=False):
    nc = bass.Bass("TRN2", target_bir_lowering=False)
    k = K()
    k.nc, k.cfg, k.P = nc, cfg, Prog(nc)
    P = k.P
    S, D, H, GW, L = cfg.S, cfg.D, cfg.H, cfg.GW, cfg.L
    KC, NCH, NT, NF = cfg.KC, cfg.NCH, cfg.NT, cfg.NF

    def din(name, shape):
        return nc.dram_tensor(name, list(shape), F32, kind="ExternalInput").ap()

    I = {}
    I["x"] = din("x", [S, D])
    I["norm_mix_g"] = din("norm_mix_g", [L, D])
    I["w_in"] = din("w_in", [L, D, cfg.INC])
    I["sgu_ln_g"] = din("sgu_ln_g", [L, H, 128])
    I["sgu_ln_b"] = din("sgu_ln_b", [L, H, 128])
    I["sgu_w"] = din("sgu_w", [L, H, 128, 128])
    I["sgu_b"] = din("sgu_b", [L, H, 128])
    I["mla_q_norm_g"] = din("mla_q_norm_g", [L, cfg.QL])
    I["mla_kv_norm_g"] = din("mla_kv_norm_g", [L, cfg.KVL])
    I["mla_w_uq"] = din("mla_w_uq", [L, cfg.QL, H * 192])
    I["mla_w_ukv"] = din("mla_w_ukv", [L, cfg.KVL, H * 256])
    I["fox_b_f"] = din("fox_b_f", [L, H])
    I["group_norm_g"] = din("group_norm_g", [L, 4, GW])
    I["w_o"] = din("w_o", [L, cfg.DMIX, D])
    I["norm_ffn_g"] = din("norm_ffn_g", [L, D])
    I["w_gate"] = din("w_gate", [L, D, cfg.DFF])
    I["w_val"] = din("w_val", [L, D, cfg.DFF])
    I["conv_w"] = din("conv_w", [L, 3, cfg.DFF])
    I["conv_b"] = din("conv_b", [L, cfg.DFF])
    I["w_down"] = din("w_down", [L, cfg.DFF, D])
    I["final_norm_g"] = din("final_norm_g", [D])
    k.I = I
    out = nc.dram_tensor("out", [S, D], F32, kind="ExternalOutput").ap()
    k.out = out
    skind = "ExternalOutput" if dbg else "Internal"

    def scratch(name, shape, dt=F32):
        return nc.dram_tensor(name, list(shape), dt, kind=skind).ap()

    k.xT = scratch("xT", [D, S])
    k.projT = scratch("projT", [cfg.INC, S])
    k.krT = scratch("krT", [64, S])
    k.qmT = scratch("qmT", [H * 192, S])
    k.kvT = scratch("kvT", [H * 256, S])
    k.ycatT = scratch("ycatT", [cfg.DMIX, S])
    k.aT = scratch("aT", [cfg.DFF, S], BF16)
    k.cosT = scratch("cosT", [64, S])
    k.sinT = scratch("sinT", [64, S])
    k.t_xT = [Tok("xT%d" % i) for i in range(KC)]
    k.t_proj = Tok("proj")
    k.t_kr = Tok("kr")
    k.t_qm = Tok("qm")
    k.t_kv = Tok("kv")
    k.t_ycat = Tok("ycat")
    k.t_aT = Tok("aT")
    k.t_rope = Tok("rope")
    k.t_out = Tok("out")
    k.wdb = scratch("wdb", [KC, 128, NF * 128], BF16)
    k.t_wdb = Tok("wdb")

    with ExitStack() as gst:
        k.ps = [gst.enter_context(nc.psum_tensor("ps%d" % i, [128, 512], F32)) for i in range(8)]
        k.tps = [Tok("ps%d" % i) for i in range(8)]
        k.ident = gst.enter_context(nc.sbuf_tensor("ident", [128, 128], F32))
        k.ones_bf = gst.enter_context(nc.sbuf_tensor("ones_bf", [128, 128], BF16))
        k.ones_f = gst.enter_context(nc.sbuf_tensor("ones_f", [128, 128], F32))
        k.t_const = Tok("const")
        k.rk = gst.enter_context(nc.sbuf_tensor("rstd_keep", [128, NCH, 512], F32))
        k.t_rk = Tok("rk")
        with ExitStack() as st:
            io = st.enter_context(nc.sbuf_tensor("io_tmp", [128, 128], F32))
            P.op("pool", lambda e: e.iota(io[:], pattern=[[1, 128]], base=0, channel_multiplier=-1,
                                          allow_small_or_imprecise_dtypes=True), writes=[k.t_const])
            P.op("dve", lambda e: e.tensor_single_scalar(out=k.ident[:], in_=io[:], scalar=0.0, op=ALU.is_equal),
                 reads=[k.t_const], writes=[k.t_const])
            P.op("dve", lambda e: e.memset(k.ones_f[:], 1.0), writes=[k.t_const], partial=True)
            P.op("dve", lambda e: e.memset(k.ones_bf[:], 1.0), writes=[k.t_const], partial=True)
            P.barrier()
        phase_rope_tables(k)
        phase_transpose_in(k)
        for l in range(L):
            phase_norm_inproj(k, l)
            phase_mla_proj(k, l)
            phase_attention(k, l)
            phase_gmlp(k, l)
            phase_wo(k, l)
            phase_ffn_up(k, l)
            phase_ffn_down(k, l)
        phase_final(k)
        P.emit()
    return nc


def new_phase(k):
    k.P.barrier()
    st = ExitStack()
    k.cnt = getattr(k, "cnt", 0) + 1
    pfx = "p%d_" % k.cnt

    def sb(name, shape, dt=F32):
        return st.enter_context(k.nc.sbuf_tensor(pfx + name, list(shape), dt))

    return st, sb


def mkrot(sb, name, n, shape, dt=F32, key=None):
    items = []
    for i in range(n):
        t = sb("%s%d" % (name, i), shape, dt)
        items.append((t, Tok("%s%d" % (name, i)), "%s%d" % (key or name, i)))
    return Rot(items)


def load_vec_fm(k, sb, name, vec_ap, n, rows=128):
    P = k.P
    tmp = sb(name + "_t", [n, rows], F32)
    dst = sb(name, [rows, n], F32)
    ttok, dtok = Tok(), Tok()
    if not hasattr(k, "t_vser"):
        k.t_vser = Tok("vser")
    P.op("sp", lambda e: e.dma_start(out=tmp[:], in_=vec_ap.rearrange("(n p) -> n p", p=rows)),
         writes=[ttok, k.t_vser], dma_sem="vec")
    ps, pt = k.ps[7], k.tps[7]
    P.op("pe", lambda e: e.transpose(out=ps[0:rows, 0:n], in_=tmp[0:n, 0:rows], identity=k.ident[0:n, 0:n]),
         reads=[ttok, k.t_const], writes=[pt])
    P.op("dve", lambda e: e.tensor_copy(out=dst[:], in_=ps[0:rows, 0:n]), reads=[pt], writes=[dtok])
    return dst, dtok


def rms_norm_fm(k, sb, src_fn, nK, nfeat, gT, gtok, dst_fn, nch, tagp="", rstd_pre=False):
    P = k.P
    P.barrier()
    G = min(4, nK)
    xr = mkrot(sb, tagp + "nx", 3, [128, G, 512], F32, key="nx")
    sq = mkrot(sb, tagp + "nsq", 3, [128, 512], BF16)
    rs = mkrot(sb, tagp + "nrs", 2, [128, 512], F32)
    banks = Rot([(k.ps[6], k.tps[6]), (k.ps[7], k.tps[7])])
    groups = [(g0, min(G, nK - g0)) for g0 in range(0, nK, G)]
    nq_ = [0]
    for c in range(nch):
        ps, pt = banks.next()
        for (g0, n) in ([] if rstd_pre else groups):
            x, xt, xk = xr.next()
            src, stoks = src_fn(g0, n, c)
            nq_[0] += 1
            P.op("sp" if nq_[0] % 2 else "pool", lambda e, x=x, src=src, n=n: e.dma_start(out=x[:, 0:n, :], in_=src),
                 reads=stoks, writes=[xt], dma_sem=xk)
            for i in range(n):
                kc = g0 + i
                s, sqt, _ = sq.next()
                P.op("act", lambda e, s=s, x=x, i=i: e.activation(out=s[:], in_=x[:, i, :], func=AF.Square), reads=[xt],
                     writes=[sqt])
                P.op("pe", lambda e, ps=ps, s=s, kc=kc: e.matmul(ps[:], lhsT=k.ones_bf[:], rhs=s[:], start=(kc == 0),
                                                                stop=(kc == nK - 1)),
                     reads=[sqt, k.t_const], writes=[pt])
        if rstd_pre:
            r, rt = k.rk[:, c, :], k.t_rk
        else:
            r_, rt, _ = rs.next()
            r = r_[:]
            P.op("act", lambda e, r=r, ps=ps: e.activation(out=r, in_=ps[:], func=AF.Sqrt, scale=1.0 / nfeat, bias=EPS),
                 reads=[pt], writes=[rt])
            P.op("dve", lambda e, r=r: e.reciprocal(out=r, in_=r), reads=[rt], writes=[rt])
        for (g0, n) in groups:
            x, xt, xk = xr.next()
            src, stoks = src_fn(g0, n, c)
            nq_[0] += 1
            P.op("sp" if nq_[0] % 2 else "pool", lambda e, x=x, src=src, n=n: e.dma_start(out=x[:, 0:n, :], in_=src),
                 reads=stoks, writes=[xt], dma_sem=xk)
            for i in range(n):
                kc = g0 + i
                dst, dtok = dst_fn(kc, c)
                P.op("dve", lambda e, x=x, dst=dst, kc=kc, r=r, i=i: e.scalar_tensor_tensor(
                    out=dst, in0=x[:, i, :], scalar=gT[:, kc:kc + 1], in1=r, op0=ALU.mult, op1=ALU.mult),
                     reads=[xt, rt, gtok], writes=[dtok], partial=True)


def linear(k, w2d, nK, col_tiles, act_fn, nch, evac, wrot, banks, wsrc=None, wq="pool", wreads=()):
    P = k.P
    wv = w2d.rearrange("(kc p) n -> p kc n", p=128) if w2d is not None else None
    for j, (pieces, M) in enumerate(col_tiles):
        wt, wtok, wkey = wrot.next()
        off = 0
        if wsrc is not None:
            src = wsrc(j)
            P.op(wq, lambda e, wt=wt, src=src: e.dma_start(out=wt[:, 0:nK, :], in_=src), reads=list(wreads),
                 writes=[wtok], dma_sem=wkey)
            pieces = []
        for (c0, n) in pieces:
            P.op("pool", lambda e, wt=wt, off=off, c0=c0, n=n: e.dma_start(out=wt[:, 0:nK, off:off + n],
                                                                          in_=wv[:, :, c0:c0 + n]),
                 writes=[wtok], dma_sem=wkey, partial=(off > 0))
            off += n
        for c in range(nch):
            ps, pt = banks.next()
            for kc in range(nK):
                a, atok = act_fn(kc, c)
                P.op("pe", lambda e, ps=ps, wt=wt, kc=kc, a=a, M=M: e.matmul(ps[0:M, :], lhsT=wt[:, kc, 0:M], rhs=a,
                                                                            start=(kc == 0), stop=(kc == nK - 1)),
                     reads=[wtok, atok], writes=[pt])
            evac(j, c, ps, pt, M)


class SumSq:
    def __init__(self, k, sb, acc_of_c, nj):
        self.k, self.acc_of_c, self.nj = k, acc_of_c, nj
        self.sq = mkrot(sb, "ssq", 4, [128, 512], BF16)
        self.pending = []
        self.fin = []

    def add(self, j, c, xo, xot):
        k, P = self.k, self.k.P
        s, st_, _ = self.sq.next()
        P.op("dve", lambda e: e.tensor_tensor(out=s[:], in0=xo[:], in1=xo[:], op=ALU.mult), reads=[xot], writes=[st_])
        self.pending.append((j, c, s, st_))
        if len(self.pending) > 2:
            self.flush(1)

    def flush(self, n=None):
        k, P = self.k, self.k.P
        D = k.cfg.D
        n = len(self.pending) if n is None else n
        for _ in range(n):
            j, c, s, st_ = self.pending.pop(0)
            acc, acct = self.acc_of_c(c)
            P.op("pe", lambda e, acc=acc, s=s, j=j: e.matmul(acc[:], lhsT=k.ones_bf[:], rhs=s[:], start=(j == 0),
                                                            stop=(j == self.nj - 1)),
                 reads=[st_, k.t_const], writes=[acct], partial=(j > 0))
            if j == self.nj - 1:
                P.op("dve", lambda e, acc=acc, c=c: e.tensor_scalar(out=k.rk[:, c, :], in0=acc[:], scalar1=1.0 / D,
                                                                    scalar2=EPS, op0=ALU.mult, op1=ALU.add),
                     reads=[acct], writes=[k.t_rk], partial=True)
                self.fin.append(c)


def ssq_finish(k, ssq, sqrt_eng="act"):
    P = k.P
    for c in ssq.fin:
        P.op(sqrt_eng, lambda e, c=c: e.activation(out=k.rk[:, c, :], in_=k.rk[:, c, :], func=AF.Sqrt), reads=[k.t_rk],
             writes=[k.t_rk], partial=True)
        P.op("dve", lambda e, c=c: e.reciprocal(out=k.rk[:, c, :], in_=k.rk[:, c, :]), reads=[k.t_rk],
             writes=[k.t_rk], partial=True)
    ssq.fin = []


def make_resid_evac(k, sb, coff_fn, ssq=None):
    P = k.P
    xr = mkrot(sb, "xr", 3, [128, 512], F32)
    xo = mkrot(sb, "xo", 4, [128, 512], F32)

    def ev(j, c, ps, pt, M):
        c0 = coff_fn(c)
        dst = k.xT[j * 128:(j + 1) * 128, c0:c0 + 512]
        r, rt, rk = xr.next()
        o, ot, ok = xo.next()
        P.op("sp", lambda e: e.dma_start(out=r[:], in_=dst), reads=[k.t_xT[j]], writes=[rt], dma_sem=rk)
        P.op("dve", lambda e: e.tensor_tensor(out=o[:], in0=ps[:], in1=r[:], op=ALU.add), reads=[pt, rt], writes=[ot])
        P.op("sp", lambda e: e.dma_start(out=dst, in_=o[:]), reads=[ot], writes=[k.t_xT[j]], dma_sem=ok, partial=True)
        if ssq is not None:
            ssq.add(j, coff_fn(c) // 512, o, ot)

    return ev


def phase_rope_tables(k):
    cfg, P = k.cfg, k.P
    S = cfg.S
    st, sb = new_phase(k)
    with st:
        pidx = sb("pidx", [64, 1])
        invf = sb("invf", [64, 1])
        pos = sb("pos", [64, S])
        ang = sb("ang", [64, S])
        kk = sb("kk", [64, S])
        ki = sb("ki", [64, S], I32)
        res = sb("res", [64, S])
        T = Tok("rt")
        P.op("pool", lambda e: e.iota(pidx[:], pattern=[[0, 1]], base=0, channel_multiplier=1,
                                      allow_small_or_imprecise_dtypes=True), writes=[T])
        P.op("pool", lambda e: e.iota(pos[:], pattern=[[1, S]], base=0, channel_multiplier=0,
                                      allow_small_or_imprecise_dtypes=True), reads=[T], writes=[T])
        P.op("act", lambda e: e.activation(out=invf[:], in_=pidx[:], func=AF.Exp, scale=-math.log(cfg.theta) / 32.0),
             reads=[T], writes=[T])
        P.op("dve", lambda e: e.tensor_scalar_mul(out=invf[32:64, :], in0=invf[32:64, :], scalar1=float(cfg.theta)),
             reads=[T], writes=[T])
        twopi = 2.0 * math.pi
        for which, shift, dstT in (("sin", 0.0, k.sinT), ("cos", math.pi / 2.0, k.cosT)):
            P.op("dve", lambda e, shift=shift: e.tensor_scalar(out=ang[:], in0=pos[:], scalar1=invf[:, 0:1], scalar2=shift,
                                                              op0=ALU.mult, op1=ALU.add), reads=[T], writes=[T])
            P.op("dve", lambda e: e.tensor_scalar_mul(out=kk[:], in0=ang[:], scalar1=1.0 / twopi), reads=[T], writes=[T])
            P.op("dve", lambda e: e.tensor_copy(out=ki[:], in_=kk[:]), reads=[T], writes=[T])
            P.op("dve", lambda e: e.tensor_copy(out=kk[:], in_=ki[:]), reads=[T], writes=[T])
            P.op("dve", lambda e: e.scalar_tensor_tensor(out=ang[:], in0=kk[:], scalar=-twopi, in1=ang[:], op0=ALU.mult,
                                                         op1=ALU.add), reads=[T], writes=[T])
            P.op("dve", lambda e: e.tensor_scalar(out=ang[:], in0=ang[:], scalar1=math.pi, scalar2=-math.pi, op0=ALU.min,
                                                  op1=ALU.max), reads=[T], writes=[T])
            P.op("act", lambda e: e.activation(out=res[:], in_=ang[:], func=AF.Sin), reads=[T], writes=[T])
            if which == "sin":
                P.op("dve", lambda e: e.tensor_scalar_mul(out=res[0:32, :], in0=res[0:32, :], scalar1=-1.0), reads=[T],
                     writes=[T])
            P.op("sp", lambda e, dstT=dstT: e.dma_start(out=dstT, in_=res[:]), reads=[T], writes=[k.t_rope, T],
                 dma_sem="st0", partial=True)


def phase_transpose_in(k):
    cfg, P = k.cfg, k.P
    S, D, KC, NT = cfg.S, cfg.D, cfg.KC, cfg.NT
    st, sb = new_phase(k)
    with st:
        xin = mkrot(sb, "xin", 2, [128, D], F32)
        stg = mkrot(sb, "stg", 3, [128, 4, 128], F32)
        banks = Rot([(k.ps[i], k.tps[i]) for i in range(6)])
        tx = Tok("x")
        n = 0
        for tt in range(NT):
            x, xt, xk = xin.next()
            P.op("sp", lambda e, x=x, tt=tt: e.dma_start(out=x[:], in_=k.I["x"][tt * 128:(tt + 1) * 128, :]), reads=[tx],
                 writes=[xt], dma_sem=xk)
            for g in range(KC // 4):
                ps, pt = banks.next()
                for i in range(4):
                    kc = g * 4 + i
                    P.op("pe", lambda e, ps=ps, x=x, i=i, kc=kc: e.transpose(out=ps[:, i * 128:(i + 1) * 128],
                                                                            in_=x[:, kc * 128:(kc + 1) * 128],
                                                                            identity=k.ident[:]),
                         reads=[xt, k.t_const], writes=[pt], partial=(i > 0))
                s, stok, sk = stg.next()
                n += 1
                if n % 2:
                    P.op("act", lambda e, s=s, ps=ps: e.activation(out=s[:].rearrange("p a b -> p (a b)"), in_=ps[:],
                                                                  func=AF.Copy), reads=[pt], writes=[stok])
                else:
                    P.op("dve", lambda e, s=s, ps=ps: e.tensor_copy(out=s[:].rearrange("p a b -> p (a b)"), in_=ps[:]),
                         reads=[pt], writes=[stok])
                dst = k.xT[g * 512:(g + 1) * 512, tt * 128:(tt + 1) * 128].rearrange("(i p) t -> p i t", p=128)
                P.op("act", lambda e, dst=dst, s=s: e.dma_start(out=dst, in_=s[:]), reads=[stok],
                     writes=[k.t_xT[g * 4 + i] for i in range(4)], dma_sem=sk, partial=True)


def phase_norm_inproj(k, l):
    cfg, P, I = k.cfg, k.P, k.I
    S, D, H, GW, KC, NCH = cfg.S, cfg.D, cfg.H, cfg.GW, cfg.KC, cfg.NCH
    st, sb = new_phase(k)
    with st:
        hT = sb("hT", [128, KC, S], BF16)
        t_h = Tok("hT")
        gT, gtok = load_vec_fm(k, sb, "g1", I["norm_mix_g"][l], KC)
        with ExitStack() as st2:
            def sb2(name, shape, dt=F32):
                return st2.enter_context(k.nc.sbuf_tensor("n1_%d_%s" % (l, name), list(shape), dt))
            rms_norm_fm(k, sb2, lambda g0, n, c: (k.xT[g0 * 128:(g0 + n) * 128, c * 512:(c + 1) * 512].rearrange("(g p) t -> p g t", p=128), [k.t_xT[g0 + i] for i in range(n)]), KC, D,
                        gT, gtok, lambda kc, c: (hT[:, kc, c * 512:(c + 1) * 512], t_h), NCH, rstd_pre=(l > 0))
            P.barrier()
        cosS = sb("cos", [64, S])
        sinS = sb("sin", [64, S])
        t_cs = Tok("cs")
        P.op("sp", lambda e: e.dma_start(out=cosS[:], in_=k.cosT), reads=[k.t_rope], writes=[t_cs], dma_sem="ld0")
        P.op("sp", lambda e: e.dma_start(out=sinS[:], in_=k.sinT), reads=[k.t_rope], writes=[t_cs], dma_sem="ld1",
             partial=True)
        krraw = sb("krraw", [64, S])
        t_krraw = Tok("krraw")
        wrot = mkrot(sb, "w", 3, [128, KC, 128], BF16)
        banks = Rot([(k.ps[i], k.tps[i]) for i in range(6)])
        stg = mkrot(sb, "stg", 3, [128, 512], F32, key="st")
        tiles = []
        kinds = []
        sc_q = 128.0 ** -0.5
        for c0 in range(0, cfg.oCkr, 128):
            tiles.append(([(c0, 128)], 128))
            if c0 < GW:
                kinds.append(("scale", sc_q, c0))
            elif cfg.oBu <= c0 < cfg.oCq:
                kinds.append(("gelu", 1.0, c0))
            else:
                kinds.append(("scale", 1.0, c0))
        tiles.append(([(cfg.oCkr, 64)], 64))
        kinds.append(("kr", 1.0, 0))
        tiles.append(([(cfg.oCkr + 32, 32), (cfg.oCkr, 32)], 64))
        kinds.append(("krsw", 1.0, 0))
        for c0 in range(cfg.oDq, cfg.oDf, 128):
            tiles.append(([(c0, 128)], 128))
            kinds.append(("scale", sc_q if c0 < cfg.oDk else 1.0, c0))
        tiles.append(([(cfg.oDf, H)], H))
        kinds.append(("scale", 1.0, cfg.oDf))
        cnt = [0]

        def evac(j, c, ps, pt, M):
            kind, sc, r0 = kinds[j]
            cs = slice(c * 512, (c + 1) * 512)
            if kind == "kr":
                P.op("act", lambda e: e.activation(out=krraw[:, cs], in_=ps[0:64, :], func=AF.Copy), reads=[pt],
                     writes=[t_krraw], partial=True)
                return
            s, stok, sk = stg.next()
            if kind == "krsw":
                P.op("dve", lambda e: e.tensor_tensor(out=s[0:64, :], in0=ps[0:64, :], in1=sinS[:, cs], op=ALU.mult),
                     reads=[pt, t_cs], writes=[stok])
                P.op("dve", lambda e: e.tensor_tensor(out=krraw[:, cs], in0=krraw[:, cs], in1=cosS[:, cs], op=ALU.mult),
                     reads=[t_krraw, t_cs], writes=[t_krraw], partial=True)
                P.op("dve", lambda e: e.tensor_tensor(out=s[0:64, :], in0=s[0:64, :], in1=krraw[:, cs], op=ALU.add),
                     reads=[t_krraw, stok], writes=[stok])
                P.op("sp", lambda e: e.dma_start(out=k.krT[:, cs], in_=s[0:64, :]), reads=[stok], writes=[k.t_kr],
                     dma_sem=sk, partial=True)
                return
            if kind == "gelu":
                P.op("act", lambda e: e.activation(out=s[0:M, :], in_=ps[0:M, :], func=AF.Gelu_apprx_tanh), reads=[pt],
                     writes=[stok])
            else:
                cnt[0] += 1
                if cnt[0] % 2:
                    P.op("act", lambda e: e.activation(out=s[0:M, :], in_=ps[0:M, :], func=AF.Copy, scale=float(sc)),
                         reads=[pt], writes=[stok])
                else:
                    P.op("dve", lambda e: e.tensor_scalar_mul(out=s[0:M, :], in0=ps[0:M, :], scalar1=float(sc)),
                         reads=[pt], writes=[stok])
            P.op("sp", lambda e: e.dma_start(out=k.projT[r0:r0 + M, cs], in_=s[0:M, :]), reads=[stok], writes=[k.t_proj],
                 dma_sem=sk, partial=True)

        linear(k, I["w_in"][l], KC, tiles, lambda kc, c: (hT[:, kc, c * 512:(c + 1) * 512], t_h), NCH, evac, wrot, banks)


def phase_mla_proj(k, l):
    cfg, P, I = k.cfg, k.P, k.I
    S, H, NCH = cfg.S, cfg.H, cfg.NCH
    nq, nkv = cfg.QL // 128, cfg.KVL // 128
    st, sb = new_phase(k)
    with st:
        cqn = sb("cqn", [128, nq, S], BF16)
        ckvn = sb("ckvn", [128, nkv, S], BF16)
        t_cqn, t_ckvn = Tok(), Tok()
        gq, gqt = load_vec_fm(k, sb, "gq", I["mla_q_norm_g"][l], nq)
        gkv, gkvt = load_vec_fm(k, sb, "gkv", I["mla_kv_norm_g"][l], nkv)
        rms_norm_fm(k, sb, lambda g0, n, c: (k.projT[cfg.oCq + g0 * 128:cfg.oCq + (g0 + n) * 128, c * 512:(c + 1) * 512].rearrange(
            "(g p) t -> p g t", p=128), [k.t_proj]), nq, cfg.QL, gq, gqt,
                    lambda kc, c: (cqn[:, kc, c * 512:(c + 1) * 512], t_cqn), NCH, tagp="q")
        rms_norm_fm(k, sb, lambda g0, n, c: (k.projT[cfg.oCkv + g0 * 128:cfg.oCkv + (g0 + n) * 128, c * 512:(c + 1) * 512].rearrange(
            "(g p) t -> p g t", p=128), [k.t_proj]), nkv, cfg.KVL, gkv, gkvt,
                    lambda kc, c: (ckvn[:, kc, c * 512:(c + 1) * 512], t_ckvn), NCH, tagp="kv")
        cosS = sb("cos", [64, S])
        sinS = sb("sin", [64, S])
        t_cs = Tok("cs")
        P.op("sp", lambda e: e.dma_start(out=cosS[:], in_=k.cosT), reads=[k.t_rope], writes=[t_cs], dma_sem="ld0")
        P.op("sp", lambda e: e.dma_start(out=sinS[:], in_=k.sinT), reads=[k.t_rope], writes=[t_cs], dma_sem="ld1",
             partial=True)
        qraw = sb("qraw", [64, S])
        t_qraw = Tok()
        wq = mkrot(sb, "wq", 3, [128, nq, 128], BF16, key="w")
        wkv = mkrot(sb, "wkv", 3, [128, nkv, 128], BF16, key="w")
        banks = Rot([(k.ps[i], k.tps[i]) for i in range(6)])
        stg = mkrot(sb, "stg", 3, [128, 512], F32, key="st")
        sc = 192.0 ** -0.5
        tiles, kinds = [], []
        for h in range(H):
            tiles.append(([(h * 192, 128)], 128)); kinds.append(("nope", h))
            tiles.append(([(h * 192 + 128, 64)], 64)); kinds.append(("r", h))
            tiles.append(([(h * 192 + 160, 32), (h * 192 + 128, 32)], 64)); kinds.append(("rsw", h))

        def evac_q(j, c, ps, pt, M):
            kind, h = kinds[j]
            cs = slice(c * 512, (c + 1) * 512)
            if kind == "r":
                P.op("act", lambda e: e.activation(out=qraw[:, cs], in_=ps[0:64, :], func=AF.Copy), reads=[pt],
                     writes=[t_qraw], partial=True)
                return
            s, stok, sk = stg.next()
            if kind == "rsw":
                P.op("dve", lambda e: e.tensor_tensor(out=s[0:64, :], in0=ps[0:64, :], in1=sinS[:, cs], op=ALU.mult),
                     reads=[pt, t_cs], writes=[stok])
                P.op("dve", lambda e: e.tensor_tensor(out=qraw[:, cs], in0=qraw[:, cs], in1=cosS[:, cs], op=ALU.mult),
                     reads=[t_qraw, t_cs], writes=[t_qraw], partial=True)
                P.op("dve", lambda e: e.scalar_tensor_tensor(out=s[0:64, :], in0=s[0:64, :], scalar=1.0, in1=qraw[:, cs],
                                                             op0=ALU.mult, op1=ALU.add),
                     reads=[t_qraw, stok], writes=[stok])
                P.op("act", lambda e: e.activation(out=s[0:64, :], in_=s[0:64, :], func=AF.Copy, scale=sc), reads=[stok],
                     writes=[stok])
                P.op("sp", lambda e: e.dma_start(out=k.qmT[h * 192 + 128:h * 192 + 192, cs], in_=s[0:64, :]),
                     reads=[stok], writes=[k.t_qm], dma_sem=sk, partial=True)
                return
            P.op("act", lambda e: e.activation(out=s[:], in_=ps[:], func=AF.Copy, scale=sc), reads=[pt], writes=[stok])
            P.op("sp", lambda e: e.dma_start(out=k.qmT[h * 192:h * 192 + 128, cs], in_=s[:]), reads=[stok],
                 writes=[k.t_qm], dma_sem=sk, partial=True)

        linear(k, I["mla_w_uq"][l], nq, tiles, lambda kc, c: (cqn[:, kc, c * 512:(c + 1) * 512], t_cqn), NCH, evac_q, wq,
               banks)
        tiles2 = [([(j * 128, 128)], 128) for j in range(2 * H)]

        def evac_kv(j, c, ps, pt, M):
            cs = slice(c * 512, (c + 1) * 512)
            s, stok, sk = stg.next()
            P.op("dve", lambda e: e.tensor_copy(out=s[:], in_=ps[:]), reads=[pt], writes=[stok])
            P.op("sp", lambda e: e.dma_start(out=k.kvT[j * 128:(j + 1) * 128, cs], in_=s[:]), reads=[stok],
                 writes=[k.t_kv], dma_sem=sk, partial=True)

        linear(k, I["mla_w_ukv"][l], nkv, tiles2, lambda kc, c: (ckvn[:, kc, c * 512:(c + 1) * 512], t_ckvn), NCH, evac_kv,
               wkv, banks)


def phase_attention(k, l):
    cfg, P, I = k.cfg, k.P, k.I
    S, H, GW, NCH, NT, NBLK = cfg.S, cfg.H, cfg.GW, cfg.NCH, cfg.NT, cfg.NBLK
    NOH = max(NBLK, H)
    st, sb = new_phase(k)
    with st:
        T = Tok("acst")
        iot = sb("iot", [128, 512])
        negm = [sb("negm%d" % i, [128, 512]) for i in range(4)]
        pio = sb("pio", [NOH, 128])
        oh = [sb("oh%d" % n, [NOH, 128], BF16) for n in range(NOH)]
        ohm = [sb("ohm%d" % n, [65, 128], BF16) for n in range(NBLK)]
        iot16 = sb("iot16", [128, NT])
        arows = sb("arows", [65, S])
        P.op("pool", lambda e: e.iota(iot[:], pattern=[[1, 512]], base=0, channel_multiplier=-1,
                                      allow_small_or_imprecise_dtypes=True), writes=[T])
        P.op("pool", lambda e: e.iota(pio[:], pattern=[[0, 128]], base=0, channel_multiplier=1,
                                      allow_small_or_imprecise_dtypes=True), reads=[T], writes=[T])
        P.op("pool", lambda e: e.iota(iot16[:], pattern=[[128, NT]], base=0, channel_multiplier=1,
                                      allow_small_or_imprecise_dtypes=True), reads=[T], writes=[T])
        P.op("pool", lambda e: e.iota(arows[32:33, :].rearrange("p (a b) -> p a b", b=128), pattern=[[128, NT], [0, 128]],
                                      base=0, channel_multiplier=0, allow_small_or_imprecise_dtypes=True),
             reads=[T], writes=[T])
        P.op("pool", lambda e: e.iota(arows[64:65, :].rearrange("p (a b) -> p a b", b=128), pattern=[[0, NT], [1, 128]],
                                      base=0, channel_multiplier=0, allow_small_or_imprecise_dtypes=True),
             reads=[T], writes=[T])
        for i in range(4):
            P.op("dve", lambda e, i=i: e.tensor_scalar(out=negm[i][:], in0=iot[:], scalar1=float(128 * i), scalar2=NEG,
                                                      op0=ALU.is_lt, op1=ALU.mult), reads=[T], writes=[T])
        for n in range(NOH):
            P.op("dve", lambda e, n=n: e.tensor_single_scalar(out=oh[n][:], in_=pio[:], scalar=float(n), op=ALU.is_equal),
                 reads=[T], writes=[T])
        for n in range(NBLK):
            P.op("dve", lambda e, n=n: e.memset(ohm[n][:], 0.0), reads=[T], writes=[T])
            P.op("dve", lambda e, n=n: e.tensor_copy(out=ohm[n][0:NBLK, :], in_=oh[n][0:NBLK, :]), reads=[T], writes=[T])
            P.op("dve", lambda e, n=n: e.memset(ohm[n][32:33, :], 1.0), reads=[T], writes=[T])
            P.op("dve", lambda e, n=n: e.memset(ohm[n][64:65, :], 1.0), reads=[T], writes=[T])

        vio = sb("vio", [128, NT, 8])
        vmask = sb("vmask", [128, NT, 8])
        nfill = sb("nfill", [128, NT, 8])
        P.op("pool", lambda e: e.iota(vio[:], pattern=[[1, NT], [-2, 8]], base=0, channel_multiplier=0,
                                      allow_small_or_imprecise_dtypes=True), reads=[T], writes=[T])
        P.op("dve", lambda e: e.tensor_single_scalar(out=vmask[:], in_=vio[:], scalar=2.0, op=ALU.is_ge), reads=[T],
             writes=[T])
        P.op("dve", lambda e: e.tensor_scalar(out=nfill[:], in0=vio[:], scalar1=2.0, scalar2=-1e30, op0=ALU.is_lt,
                                              op1=ALU.mult), reads=[T], writes=[T])
        fA = sb("fA", [H, S])
        fB = sb("fB", [H, S])
        bfv = sb("bfv", [H, 1])
        cs3 = [sb("cs3_%d" % i, [H, S], BF16) for i in range(3)]
        csr = sb("csr", [H, S])
        cs_tm = sb("cs_tm", [128, NT, H])
        TF = Tok("fox")
        fox_state = {}

        def fox_prep():
            P.op("sp", lambda e: e.dma_start(out=fA[:], in_=k.projT[cfg.oDf:cfg.oDf + H, :]), reads=[k.t_proj], writes=[TF],
                 dma_sem="ld0")
            P.op("sp", lambda e: e.dma_start(out=bfv[:], in_=I["fox_b_f"][l].rearrange("(h o) -> h o", o=1)), writes=[TF],
                 dma_sem="ld1", partial=True)
            P.op("act", lambda e: e.activation(out=bfv[:], in_=bfv[:], func=AF.Copy, scale=-1.0), reads=[TF], writes=[TF])
            P.op("act", lambda e: e.activation(out=fB[:], in_=fA[:], func=AF.Exp, bias=bfv[:, 0:1], scale=-1.0), reads=[TF],
                 writes=[TF])
            P.op("act", lambda e: e.activation(out=fA[:], in_=fB[:], func=AF.Ln, bias=1.0), reads=[TF], writes=[TF])
            a, b = fA, fB
            d = 1
            while d < S:
                P.op("pool", lambda e, a=a, b=b, d=d: e.tensor_tensor(out=b[:, d:S], in0=a[:, d:S], in1=a[:, 0:S - d], op=ALU.add),
                     reads=[TF], writes=[TF])
                P.op("pool", lambda e, a=a, b=b, d=d: e.tensor_copy(out=b[:, 0:d], in_=a[:, 0:d]), reads=[TF], writes=[TF])
                a, b = b, a
                d *= 2
            cs = a
            oth = b
            P.op("act", lambda e: e.activation(out=oth[:], in_=cs[:], func=AF.Copy, scale=-1.0), reads=[TF], writes=[TF])
            for i in range(3):
                P.op("pool", lambda e, i=i: e.tensor_copy(out=cs3[i][:], in_=oth[:]), reads=[TF], writes=[TF])
                if i < 2:
                    P.op("pool", lambda e, i=i: e.tensor_copy(out=csr[:], in_=cs3[i][:]), reads=[TF], writes=[TF])
                    P.op("pool", lambda e: e.tensor_tensor(out=oth[:], in0=oth[:], in1=csr[:], op=ALU.subtract), reads=[TF],
                         writes=[TF])
            fox_state['cs'] = cs

        def fox_prep_pe():
            cs = fox_state['cs']
            for tt in range(NT):
                P.op("pe", lambda e, tt=tt: e.transpose(out=k.ps[7][:, tt * H:(tt + 1) * H],
                                                       in_=cs[0:H, tt * 128:(tt + 1) * 128],
                                                       identity=k.ident[0:H, 0:H]), reads=[TF, k.t_const],
                     writes=[k.tps[7]], partial=(tt > 0))
            P.op("dve", lambda e: e.tensor_copy(out=cs_tm[:].rearrange("p a b -> p (a b)"), in_=k.ps[7][:, 0:NT * H]),
                 reads=[k.tps[7]], writes=[TF], partial=True)

        qf = mkrot(sb, "qf", 2, [128, S], F32, key="hq")
        kf = mkrot(sb, "kf", 2, [128, S], F32, key="hk")
        vf = mkrot(sb, "vf", 2, [128, S], F32, key="hv")
        qb = mkrot(sb, "qb", 2, [128, S], BF16, key="hqb")
        kb = mkrot(sb, "kb", 2, [128, S], BF16, key="hkb")
        q2 = mkrot(sb, "q2", 2, [64, S], BF16, key="hq2")
        q2f = mkrot(sb, "q2f", 2, [64, S], F32, key="hq2f")
        vtm = mkrot(sb, "vtm", 2, [128, NT, 128], BF16)
        selbT = mkrot(sb, "selbT", 2, [65, S], BF16)
        mbias = mkrot(sb, "mbias", 2, [128, NT], F32)
        k2 = sb("k2", [64, S], BF16)
        t_k2 = Tok("k2")
        kmean = sb("kmean", [128, 8])
        g8 = sb("g8", [128, NT, 8])
        cmp4 = sb("cmp4", [128, NT, 8, 8])
        cnt8 = sb("cnt8", [128, NT, 8])
        selb = sb("selb", [128, NT, 8])
        TG = Tok("gate")
        lnd = mkrot(sb, "lnd", 2, [128, 512], F32)
        pT = mkrot(sb, "pT", 3, [128, 512], BF16)
        tmp = mkrot(sb, "tmp", 3, [128, 512], F32)
        rden = mkrot(sb, "rden", 2, [128, 512], F32)
        yst = mkrot(sb, "yst", 2, [128, 512], F32, key="st")
        sb_rot = Rot([(k.ps[i], k.tps[i]) for i in range(3)])
        o_rot = Rot([(k.ps[3], k.tps[3]), (k.ps[4], k.tps[4])])
        d_rot = Rot([(k.ps[5], k.tps[5]), (k.ps[6], k.tps[6])])
        misc, tmisc = k.ps[7], k.tps[7]

        P.op("pool", lambda e: e.dma_start(out=k2[:], in_=k.krT), reads=[k.t_kr], writes=[t_k2], dma_sem="k2")

        heads = []
        for h in range(H):
            heads.append(("moba", h))
        for h in range(H):
            heads.append(("mla", h))
        for h in range(H):
            heads.append(("fox", h))
        NHD = len(heads)
        HS = {}
        KCd = cfg.KC
        pc_per = (KCd + NHD - 1) // NHD
        wdv = I["w_down"][l].rearrange("(kc p) n -> p kc n", p=128)

        def prep_dma(hi):
            kind, h = heads[hi]
            stt = {}
            HS[hi] = stt
            if kind == "moba":
                qsrc = k.projT[cfg.oAq + h * 128:cfg.oAq + (h + 1) * 128, :]
                ksrc = k.projT[cfg.oAk + h * 128:cfg.oAk + (h + 1) * 128, :]
                vsrc = k.projT[cfg.oAv + h * 128:cfg.oAv + (h + 1) * 128, :]
                rtoks = [k.t_proj]
                stt["ydst"] = k.ycatT[h * 128:(h + 1) * 128, :]
                stt["slope"] = 2.0 ** (-8.0 * (h + 1) / H)
            elif kind == "mla":
                qsrc = k.qmT[h * 192:h * 192 + 128, :]
                ksrc = k.kvT[h * 256:h * 256 + 128, :]
                vsrc = k.kvT[h * 256 + 128:h * 256 + 256, :]
                rtoks = [k.t_qm, k.t_kv]
                stt["ydst"] = k.ycatT[2 * GW + h * 128:2 * GW + (h + 1) * 128, :]
            else:
                qsrc = k.projT[cfg.oDq + h * 128:cfg.oDq + (h + 1) * 128, :]
                ksrc = k.projT[cfg.oDk + h * 128:cfg.oDk + (h + 1) * 128, :]
                vsrc = k.projT[cfg.oDv + h * 128:cfg.oDv + (h + 1) * 128, :]
                rtoks = [k.t_proj]
                stt["ydst"] = k.ycatT[3 * GW + h * 128:3 * GW + (h + 1) * 128, :]
            v_, vt, vk = vf.next()
            stt["v"] = (v_, vt)
            P.op("sp", lambda e: e.dma_start(out=v_[:], in_=vsrc), reads=rtoks, writes=[vt], dma_sem=vk)
            qb_, qbt, qbk = qb.next()
            kb_, kbt, kbk = kb.next()
            stt["qb"] = (qb_, qbt)
            stt["kb"] = (kb_, kbt)
            q_, qt, qk = qf.next()
            k_, kt_, kk = kf.next()
            stt["qf"] = (q_, qt)
            stt["kf"] = (k_, kt_)
            P.op("sp", lambda e: e.dma_start(out=q_[:], in_=qsrc), reads=rtoks, writes=[qt], dma_sem=qk)
            P.op("sp", lambda e: e.dma_start(out=k_[:], in_=ksrc), reads=rtoks, writes=[kt_], dma_sem=kk)
            P.op("act", lambda e: e.activation(out=qb_[:], in_=q_[:], func=AF.Copy), reads=[qt], writes=[qbt])
            P.op("dve", lambda e: e.tensor_copy(out=kb_[:], in_=k_[:]), reads=[kt_], writes=[kbt])
            if kind == "mla":
                q2_, q2t, q2k = q2.next()
                q2f_, q2ft, q2fk = q2f.next()
                stt["q2"] = (q2_, q2t)
                P.op("sp", lambda e: e.dma_start(out=q2f_[:], in_=k.qmT[h * 192 + 128:h * 192 + 192, :]),
                     reads=[k.t_qm], writes=[q2ft], dma_sem=q2fk)
                P.op("dve", lambda e: e.tensor_copy(out=q2_[:], in_=q2f_[:]), reads=[q2ft], writes=[q2t])
            for j in range(hi * pc_per, min(KCd, (hi + 1) * pc_per)):
                P.op("pool", lambda e, j=j: e.dma_start(out=k.wdb[j].rearrange("p (kc n) -> p kc n", n=128),
                                                        in_=wdv[:, :, j * 128:(j + 1) * 128]),
                     writes=[k.t_wdb], dma_sem="pc%d" % (j % 2), partial=True)

        def prep_pe1(hi):
            kind, h = heads[hi]
            stt = HS[hi]
            if kind != "moba":
                return
            q_, qt = stt["qf"]
            k_, kt_ = stt["kf"]
            sT, sTt, _ = selbT.next()
            stt["sT"] = (sT, sTt)
            P.op("dve", lambda e: e.memset(kmean[:], 0.0), reads=[TG], writes=[TG])
            P.op("dve", lambda e: e.reduce_sum(out=kmean[:, 0:NBLK], in_=k_[:].rearrange("p (n b) -> p n b", b=256),
                                               axis=AX.X), reads=[kt_, TG], writes=[TG])
            P.op("dve", lambda e: e.memset(sT[:], 0.0), writes=[sTt])
            slope_ = stt["slope"]
            P.op("dve", lambda e: e.tensor_scalar_mul(out=sT[32:33, :], in0=arows[32:33, :], scalar1=-slope_),
                 reads=[T, sTt], writes=[sTt], partial=True)
            P.op("dve", lambda e: e.tensor_scalar_mul(out=sT[64:65, :], in0=arows[64:65, :], scalar1=-slope_),
                 reads=[T, sTt], writes=[sTt], partial=True)
            mb, mbt, _ = mbias.next()
            stt["mb"] = (mb, mbt)
            P.op("dve", lambda e: e.tensor_scalar_mul(out=mb[:], in0=iot16[:], scalar1=slope_), reads=[T], writes=[mbt])
            for tt in range(NT):
                P.op("pe", lambda e, tt=tt: e.matmul(misc[:, tt * 8:(tt + 1) * 8], lhsT=q_[:, tt * 128:(tt + 1) * 128],
                                                    rhs=kmean[:], start=True, stop=True),
                     reads=[qt, TG], writes=[tmisc], partial=(tt > 0))
            g8f = g8[:].rearrange("p a b -> p (a b)")
            P.op("dve", lambda e: e.tensor_tensor(out=g8f, in0=misc[:, 0:NT * 8], in1=vmask[:].rearrange("p a b -> p (a b)"),
                                                  op=ALU.mult), reads=[tmisc, T, TG], writes=[TG])
            P.op("dve", lambda e: e.tensor_tensor(out=g8f, in0=g8f, in1=nfill[:].rearrange("p a b -> p (a b)"), op=ALU.add),
                 reads=[TG, T], writes=[TG])
            P.op("dve", lambda e: e.tensor_tensor(out=cmp4[:], in0=g8[:].unsqueeze(2).to_broadcast([128, NT, 8, 8]),
                                                  in1=g8[:].unsqueeze(3).to_broadcast([128, NT, 8, 8]), op=ALU.is_gt),
                 reads=[TG], writes=[TG])
            P.op("dve", lambda e: e.reduce_sum(out=cnt8[:], in_=cmp4[:], axis=AX.X), reads=[TG], writes=[TG])
            P.op("dve", lambda e: e.tensor_scalar(out=selb[:], in0=cnt8[:], scalar1=3.0, scalar2=NEG, op0=ALU.is_ge,
                                                  op1=ALU.mult), reads=[TG], writes=[TG])
            P.op("dve", lambda e: e.tensor_tensor(out=selb[:], in0=selb[:], in1=vmask[:], op=ALU.mult), reads=[TG, T],
                 writes=[TG])

        def prep_pe2(hi):
            kind, h = heads[hi]
            stt = HS[hi]
            v_, vt = stt["v"]
            if kind == "moba":
                sT, sTt = stt["sT"]
                for g in range(NT // 4):
                    for i in range(4):
                        tt = g * 4 + i
                        P.op("pe", lambda e, tt=tt, i=i: e.transpose(out=misc[0:8, i * 128:(i + 1) * 128],
                                                                    in_=selb[:, tt, :], identity=k.ident[:]),
                             reads=[TG, k.t_const], writes=[tmisc], partial=(i > 0))
                    P.op("dve", lambda e, g=g: e.tensor_copy(out=sT[0:NBLK, g * 512:(g + 1) * 512], in_=misc[0:NBLK, :]),
                         reads=[tmisc, sTt], writes=[sTt], partial=True)
            vm, vmt, _ = vtm.next()
            stt["vm"] = (vm, vmt)
            for g in range(NT // 4):
                for i in range(4):
                    tt = g * 4 + i
                    P.op("pe", lambda e, tt=tt, i=i: e.transpose(out=misc[:, i * 128:(i + 1) * 128],
                                                                in_=v_[:, tt * 128:(tt + 1) * 128], identity=k.ident[:]),
                         reads=[vt, k.t_const], writes=[tmisc], partial=(i > 0))
                P.op("act", lambda e, g=g: e.activation(out=vm[:, g * 4:(g + 1) * 4, :].rearrange("p a b -> p (a b)"),
                                                        in_=misc[:], func=AF.Copy),
                     reads=[tmisc], writes=[vmt], partial=True)

        def make_blocks(hi):
            kind, h = heads[hi]
            stt = HS[hi]
            qb_, qbt = stt["qb"]
            kb_, kbt = stt["kb"]
            vm, vmt = stt["vm"]
            ydst = stt["ydst"]
            blks = []
            for c in range(NCH):
                qs = slice(c * 512, (c + 1) * 512)
                ops_, opt = o_rot.next()
                dps, dpt = d_rot.next()
                nkt = 4 * c + 4
                for kt in range(nkt):
                    ks = slice(kt * 128, (kt + 1) * 128)
                    sp_, spt = sb_rot.next()
                    p_, ptk, _ = pT.next()
                    diag = kt >= 4 * c
                    di = kt - 4 * c

                    def A(c=c, qs=qs, kt=kt, ks=ks, sp_=sp_, spt=spt, p_=p_, ptk=ptk, diag=diag, di=di):
                        extra = []
                        if kind == "mla":
                            q2_, q2t = stt["q2"]
                            extra.append((lambda e: e.matmul(sp_[:], lhsT=k2[:, ks], rhs=q2_[:, qs], start=False,
                                                             stop=True), [t_k2, q2t]))
                        elif kind == "fox":
                            for i3 in range(3):
                                extra.append((lambda e, i3=i3: e.matmul(sp_[:], lhsT=oh[h][0:H, :], rhs=cs3[i3][:, qs],
                                                                        start=False, stop=(i3 == 2)), [T, TF]))
                        else:
                            sT, sTt = stt["sT"]
                            n = kt // 2
                            extra.append((lambda e: e.matmul(sp_[:], lhsT=ohm[n][0:65, :], rhs=sT[0:65, qs],
                                                             start=False, stop=True), [T, sTt]))
                        P.op("pe", lambda e: e.matmul(sp_[:], lhsT=kb_[:, ks], rhs=qb_[:, qs], start=True,
                                                      stop=(len(extra) == 0)), reads=[kbt, qbt], writes=[spt])
                        for fn, rd in extra:
                            P.op("pe", fn, reads=rd, writes=[spt], partial=True)
                        src, srct = sp_, spt
                        if diag:
                            t_, tt_, _ = tmp.next()
                            P.op("dve", lambda e: e.tensor_tensor(out=t_[:], in0=sp_[:], in1=negm[di][:], op=ALU.add),
                                 reads=[spt, T], writes=[tt_])
                            src, srct = t_, tt_
                        if kind == "fox":
                            P.op("act", lambda e: e.activation(out=p_[:], in_=src[:], func=AF.Exp,
                                                               bias=cs_tm[:, kt, h:h + 1]),
                                 reads=[srct, TF], writes=[ptk])
                        elif kind == "moba":
                            mb, mbt = stt["mb"]
                            P.op("act", lambda e: e.activation(out=p_[:], in_=src[:], func=AF.Exp, bias=mb[:, kt:kt + 1]),
                                 reads=[srct, mbt], writes=[ptk])
                        else:
                            P.op("act", lambda e: e.activation(out=p_[:], in_=src[:], func=AF.Exp), reads=[srct],
                                 writes=[ptk])

                    def B(c=c, qs=qs, kt=kt, p_=p_, ptk=ptk, nkt=nkt, ops_=ops_, opt=opt, dps=dps, dpt=dpt):
                        P.op("pe", lambda e: e.matmul(ops_[:], lhsT=vm[:, kt, :], rhs=p_[:], start=(kt == 0),
                                                      stop=(kt == nkt - 1)),
                             reads=[vmt, ptk], writes=[opt], partial=(kt > 0))
                        P.op("pe", lambda e: e.matmul(dps[:], lhsT=k.ones_bf[:], rhs=p_[:], start=(kt == 0),
                                                      stop=(kt == nkt - 1)),
                             reads=[ptk, k.t_const], writes=[dpt], partial=(kt > 0))
                        if kt == nkt - 1:
                            rd, rdt, _ = rden.next()
                            y_, yt, yk = yst.next()
                            ld_, ldt, _ = lnd.next()
                            P.op("act", lambda e: e.activation(out=ld_[:], in_=dps[:], func=AF.Ln), reads=[dpt],
                                 writes=[ldt])
                            P.op("act", lambda e: e.activation(out=rd[:], in_=ld_[:], func=AF.Exp, scale=-1.0),
                                 reads=[ldt], writes=[rdt])
                            P.op("dve", lambda e: e.tensor_tensor(out=y_[:], in0=ops_[:], in1=rd[:], op=ALU.mult),
                                 reads=[opt, rdt], writes=[yt])
                            P.op("sp", lambda e: e.dma_start(out=ydst[:, qs], in_=y_[:]), reads=[yt], writes=[k.t_ycat],
                                 dma_sem=yk, partial=True)

                    blks.append((A, B))
            return blks

        LA = 2
        PE1_AT = 10
        PE2_AT = sum(4 * c + 4 for c in range(NCH - 1))
        pending = []
        fox_prep()
        prep_dma(0)
        prep_pe1(0)
        prep_pe2(0)
        for hi in range(NHD):
            if hi + 1 < NHD:
                prep_dma(hi + 1)
            blks = make_blocks(hi)
            for bi, (A, B) in enumerate(blks):
                if bi == PE1_AT and hi + 1 < NHD:
                    prep_pe1(hi + 1)
                if bi == PE2_AT and hi + 1 < NHD:
                    if heads[hi + 1][0] == "fox" and heads[hi][0] != "fox":
                        fox_prep_pe()
                    prep_pe2(hi + 1)
                A()
                pending.append(B)
                if len(pending) > LA:
                    pending.pop(0)()
        while pending:
            pending.pop(0)()


def phase_gmlp(k, l):
    cfg, P, I = k.cfg, k.P, k.I
    S, H, GW, NCH, NT = cfg.S, cfg.H, cfg.GW, cfg.NCH, cfg.NT
    st, sb = new_phase(k)
    with st:
        lg, lgt = load_vec_fm(k, sb, "lng", I["sgu_ln_g"][l].rearrange("h d -> (h d)"), H)
        lb, lbt = load_vec_fm(k, sb, "lnb", I["sgu_ln_b"][l].rearrange("h d -> (h d)"), H)
        T = Tok("gm")
        io = sb("io", [128, 128])
        m01 = sb("m01", [128, 128])
        P.op("pool", lambda e: e.iota(io[:], pattern=[[1, 128]], base=0, channel_multiplier=-1,
                                      allow_small_or_imprecise_dtypes=True), writes=[T])
        P.op("dve", lambda e: e.tensor_single_scalar(out=m01[:], in_=io[:], scalar=0.0, op=ALU.is_ge), reads=[T],
             writes=[T])
        uf = mkrot(sb, "uf", 3, [128, S], F32, key="hq")
        vf = mkrot(sb, "vf", 3, [128, S], F32, key="hk")
        ws = mkrot(sb, "ws", 3, [128, 128], F32, key="hv")
        bsr = mkrot(sb, "bsr", 3, [1, 128], F32, key="hqb")
        wmT = mkrot(sb, "wmT", 3, [128, 128], BF16)
        bhi = mkrot(sb, "bhi", 3, [1, 128], BF16)
        blo = mkrot(sb, "blo", 3, [1, 128], BF16)
        bfl = mkrot(sb, "bfl", 3, [1, 128], F32)
        vb = mkrot(sb, "vb", 4, [128, 512], BF16)
        sqb = mkrot(sb, "sqb", 4, [128, 512], BF16)
        mean = mkrot(sb, "mean", 4, [128, 512], F32)
        m2 = mkrot(sb, "m2", 4, [128, 512], F32)
        rstd = mkrot(sb, "rstd", 4, [128, 512], F32)
        vn = mkrot(sb, "vn", 8, [128, 512], F32)
        vtm = mkrot(sb, "vtm", 4, [128, 4, 128], BF16)
        yst = mkrot(sb, "yst", 3, [128, 512], F32, key="st")
        b_mean = Rot([(k.ps[0], k.tps[0]), (k.ps[1], k.tps[1])])
        b_msq = Rot([(k.ps[2], k.tps[2]), (k.ps[3], k.tps[3])])
        b_tr = Rot([(k.ps[4], k.tps[4]), (k.ps[5], k.tps[5])])
        b_out = Rot([(k.ps[6], k.tps[6]), (k.ps[7], k.tps[7])])
        HSt = {}

        def loads(g):
            stt = {}
            HSt[g] = stt
            u_, ut, uk = uf.next()
            v_, vt, vk = vf.next()
            w_, wt_, wk = ws.next()
            b_, bt_, bk = bsr.next()
            stt.update(u=(u_, ut), v=(v_, vt), w=(w_, wt_), b=(b_, bt_))
            P.op("sp", lambda e: e.dma_start(out=u_[:], in_=k.projT[cfg.oBu + g * 128:cfg.oBu + (g + 1) * 128, :]),
                 reads=[k.t_proj], writes=[ut], dma_sem=uk)
            P.op("sp", lambda e: e.dma_start(out=v_[:], in_=k.projT[cfg.oBv + g * 128:cfg.oBv + (g + 1) * 128, :]),
                 reads=[k.t_proj], writes=[vt], dma_sem=vk)
            P.op("sp", lambda e: e.dma_start(out=w_[:], in_=I["sgu_w"][l, g]), writes=[wt_], dma_sem=wk)
            P.op("sp", lambda e: e.dma_start(out=b_[:], in_=I["sgu_b"][l, g].rearrange("(o t) -> o t", o=1)),
                 writes=[bt_], dma_sem=bk)
            bh, bht, _ = bhi.next()
            bl, blt, _ = blo.next()
            bf_, bft, _ = bfl.next()
            stt.update(bh=(bh, bht), bl=(bl, blt))
            P.op("dve", lambda e: e.tensor_copy(out=bh[:], in_=b_[:]), reads=[bt_], writes=[bht])
            P.op("dve", lambda e: e.tensor_copy(out=bf_[:], in_=bh[:]), reads=[bht], writes=[bft])
            P.op("dve", lambda e: e.tensor_tensor(out=bf_[:], in0=b_[:], in1=bf_[:], op=ALU.subtract), reads=[bt_, bft],
                 writes=[bft])
            P.op("dve", lambda e: e.tensor_copy(out=bl[:], in_=bf_[:]), reads=[bft], writes=[blt])

        def front(g):
            stt = HSt[g]
            v_, vt = stt["v"]
            w_, wt_ = stt["w"]
            wm, wmt, _ = wmT.next()
            stt["wm"] = (wm, wmt)
            trp, trt = b_tr.next()
            P.op("pe", lambda e: e.transpose(out=trp[:, 0:128], in_=w_[:], identity=k.ident[:]),
                 reads=[wt_, k.t_const], writes=[trt])
            P.op("dve", lambda e: e.tensor_tensor(out=wm[:], in0=trp[:, 0:128], in1=m01[:], op=ALU.mult),
                 reads=[trt, T], writes=[wmt])
            cst = [dict() for _ in range(NCH)]
            stt["c"] = cst
            for c in range(NCH):
                cs = slice(c * 512, (c + 1) * 512)
                vb_, vbt, _ = vb.next()
                sq_, sqt, _ = sqb.next()
                cst[c].update(vb=(vb_, vbt), sq=(sq_, sqt))
                P.op("act", lambda e, vb_=vb_, cs=cs: e.activation(out=vb_[:], in_=v_[:, cs], func=AF.Copy), reads=[vt],
                     writes=[vbt])
                P.op("act", lambda e, sq_=sq_, cs=cs: e.activation(out=sq_[:], in_=v_[:, cs], func=AF.Square), reads=[vt],
                     writes=[sqt])
            for c in range(NCH):
                vb_, vbt = cst[c]["vb"]
                sq_, sqt = cst[c]["sq"]
                pm, pmt = b_mean.next()
                pq, pqt = b_msq.next()
                P.op("pe", lambda e, pm=pm, vb_=vb_: e.matmul(pm[:], lhsT=k.ones_bf[:], rhs=vb_[:], start=True, stop=True),
                     reads=[vbt, k.t_const], writes=[pmt])
                P.op("pe", lambda e, pq=pq, sq_=sq_: e.matmul(pq[:], lhsT=k.ones_bf[:], rhs=sq_[:], start=True, stop=True),
                     reads=[sqt, k.t_const], writes=[pqt])
                mn, mnt, _ = mean.next()
                mm, mmt, _ = m2.next()
                cst[c].update(mn=(mn, mnt), mm=(mm, mmt))
                P.op("act", lambda e, mn=mn, pm=pm: e.activation(out=mn[:], in_=pm[:], func=AF.Copy, scale=1.0 / 128.0),
                     reads=[pmt], writes=[mnt])
                P.op("dve", lambda e, mm=mm, mn=mn: e.tensor_tensor(out=mm[:], in0=mn[:], in1=mn[:], op=ALU.mult),
                     reads=[mnt], writes=[mmt])
                P.op("dve", lambda e, mm=mm, pq=pq: e.scalar_tensor_tensor(out=mm[:], in0=pq[:], scalar=1.0 / 128.0,
                                                                           in1=mm[:], op0=ALU.mult, op1=ALU.subtract),
                     reads=[pqt, mmt], writes=[mmt])

        def front2(g):
            stt = HSt[g]
            v_, vt = stt["v"]
            cst = stt["c"]
            for c in range(NCH):
                mm, mmt = cst[c]["mm"]
                rs, rst, _ = rstd.next()
                cst[c]["rs"] = (rs, rst)
                P.op("act", lambda e, rs=rs, mm=mm: e.activation(out=rs[:], in_=mm[:], func=AF.Ln, bias=EPS), reads=[mmt],
                     writes=[rst])
            for c in range(NCH):
                rs, rst = cst[c]["rs"]
                P.op("act", lambda e, rs=rs: e.activation(out=rs[:], in_=rs[:], func=AF.Exp, scale=-0.5), reads=[rst],
                     writes=[rst])
            for c in range(NCH):
                cs = slice(c * 512, (c + 1) * 512)
                mn, mnt = cst[c]["mn"]
                rs, rst = cst[c]["rs"]
                vn_, vnt, _ = vn.next()
                cst[c]["vn"] = (vn_, vnt)
                P.op("dve", lambda e, vn_=vn_, cs=cs, mn=mn: e.tensor_tensor(out=vn_[:], in0=v_[:, cs], in1=mn[:],
                                                                             op=ALU.subtract),
                     reads=[vt, mnt], writes=[vnt])
                P.op("dve", lambda e, vn_=vn_, rs=rs: e.tensor_tensor(out=vn_[:], in0=vn_[:], in1=rs[:], op=ALU.mult),
                     reads=[vnt, rst], writes=[vnt])
                P.op("dve", lambda e, vn_=vn_: e.tensor_scalar(out=vn_[:], in0=vn_[:], scalar1=lg[:, g:g + 1],
                                                               scalar2=lb[:, g:g + 1], op0=ALU.mult, op1=ALU.add),
                     reads=[vnt, lgt, lbt], writes=[vnt])

        def back(g):
            stt = HSt[g]
            u_, ut = stt["u"]
            bh, bht = stt["bh"]
            bl, blt = stt["bl"]
            wm, wmt = stt["wm"]
            cst = stt["c"]
            for c in range(NCH):
                vn_, vnt = cst[c]["vn"]
                trp, trt = b_tr.next()
                for i in range(4):
                    P.op("pe", lambda e, trp=trp, vn_=vn_, i=i: e.transpose(out=trp[:, i * 128:(i + 1) * 128],
                                                                           in_=vn_[:, i * 128:(i + 1) * 128],
                                                                           identity=k.ident[:]),
                         reads=[vnt, k.t_const], writes=[trt], partial=(i > 0))
                vm, vmt, _ = vtm.next()
                cst[c]["vm"] = (vm, vmt)
                P.op("act", lambda e, vm=vm, trp=trp: e.activation(out=vm[:].rearrange("p a b -> p (a b)"), in_=trp[:],
                                                                  func=AF.Copy), reads=[trt], writes=[vmt])

        def back2(g):
            stt = HSt[g]
            u_, ut = stt["u"]
            bh, bht = stt["bh"]
            bl, blt = stt["bl"]
            wm, wmt = stt["wm"]
            cst = stt["c"]
            for c in range(NCH):
                cs = slice(c * 512, (c + 1) * 512)
                vm, vmt = cst[c]["vm"]
                po, pot = b_out.next()
                for i in range(4):
                    P.op("pe", lambda e, po=po, vm=vm, i=i: e.matmul(po[:, i * 128:(i + 1) * 128], lhsT=vm[:, i, :],
                                                                    rhs=wm[:], start=True, stop=False),
                         reads=[vmt, wmt], writes=[pot], partial=(i > 0))
                    P.op("pe", lambda e, po=po, i=i: e.matmul(po[:, i * 128:(i + 1) * 128], lhsT=k.ones_bf[0:1, :],
                                                             rhs=bh[0:1, :], start=False, stop=False),
                         reads=[bht, k.t_const], writes=[pot], partial=True)
                    P.op("pe", lambda e, po=po, i=i: e.matmul(po[:, i * 128:(i + 1) * 128], lhsT=k.ones_bf[0:1, :],
                                                             rhs=bl[0:1, :], start=False, stop=True),
                         reads=[blt, k.t_const], writes=[pot], partial=True)
                y_, yt, yk = yst.next()
                P.op("dve", lambda e, y_=y_, po=po, cs=cs: e.tensor_tensor(out=y_[:], in0=po[:], in1=u_[:, cs], op=ALU.mult),
                     reads=[pot, ut], writes=[yt])
                P.op("sp", lambda e, y_=y_, cs=cs: e.dma_start(out=k.ycatT[GW + g * 128:GW + (g + 1) * 128, cs], in_=y_[:]),
                     reads=[yt], writes=[k.t_ycat], dma_sem=yk, partial=True)

        loads(0)
        if H > 1:
            loads(1)
        front(0)
        front2(0)
        for g in range(H):
            if g + 2 < H:
                loads(g + 2)
            if g + 1 < H:
                front(g + 1)
            back(g)
            if g + 1 < H:
                front2(g + 1)
            back2(g)


def phase_wo(k, l):
    cfg, P, I = k.cfg, k.P, k.I
    S, D, H, GW, NCH, KC = cfg.S, cfg.D, cfg.H, cfg.GW, cfg.NCH, cfg.KC
    KM = cfg.DMIX // 128
    st, sb = new_phase(k)
    with st:
        ynT = sb("ynT", [128, KM, S], BF16)
        t_yn = Tok("ynT")
        gg, ggt = load_vec_fm(k, sb, "gg", I["group_norm_g"][l].rearrange("a b -> (a b)"), KM)
        for gi in range(4):
            with ExitStack() as st2:
                def sb2(name, shape, dt=F32, st2=st2, gi=gi):
                    return st2.enter_context(k.nc.sbuf_tensor("gn_%d_%d_%s" % (l, gi, name), list(shape), dt))
                rms_norm_fm(k, sb2, lambda g0, n, c, gi=gi: (k.ycatT[(gi * H + g0) * 128:(gi * H + g0 + n) * 128,
                                                                    c * 512:(c + 1) * 512].rearrange(
                    "(g p) t -> p g t", p=128), [k.t_ycat]), H, GW,
                            gg[:, gi * H:(gi + 1) * H], ggt,
                            lambda kc, c, gi=gi: (ynT[:, gi * H + kc, c * 512:(c + 1) * 512], t_yn), NCH, tagp="g%d" % gi)
                P.barrier()
        wrot = mkrot(sb, "w", 3, [128, KM, 128], BF16)
        banks = Rot([(k.ps[i], k.tps[i]) for i in range(4)])
        ssq = SumSq(k, sb, lambda c: (k.ps[4 + c], k.tps[4 + c]), KC)
        ev = make_resid_evac(k, sb, lambda c: c * 512, ssq)
        tiles = [([(j * 128, 128)], 128) for j in range(KC)]
        linear(k, I["w_o"][l], KM, tiles, lambda kc, c: (ynT[:, kc, c * 512:(c + 1) * 512], t_yn), NCH, ev, wrot, banks)
        ssq.flush()
        ssq_finish(k, ssq)


def phase_ffn_up(k, l):
    cfg, P, I = k.cfg, k.P, k.I
    S, D, NCH, KC, NF = cfg.S, cfg.D, cfg.NCH, cfg.KC, cfg.NF
    st, sb = new_phase(k)
    with st:
        hT = sb("h2T", [128, KC, S], BF16)
        t_h = Tok("h2T")
        gT, gtok = load_vec_fm(k, sb, "g2", I["norm_ffn_g"][l], KC)
        with ExitStack() as st2:
            def sb2(name, shape, dt=F32):
                return st2.enter_context(k.nc.sbuf_tensor("n2_%d_%s" % (l, name), list(shape), dt))
            rms_norm_fm(k, sb2, lambda g0, n, c: (k.xT[g0 * 128:(g0 + n) * 128, c * 512:(c + 1) * 512].rearrange("(g p) t -> p g t", p=128), [k.t_xT[g0 + i] for i in range(n)]), KC, D,
                        gT, gtok, lambda kc, c: (hT[:, kc, c * 512:(c + 1) * 512], t_h), NCH, rstd_pre=True)
            P.barrier()
        cw = []
        for j in range(3):
            cw.append(load_vec_fm(k, sb, "cw%d" % j, I["conv_w"][l, j], NF))
        cb, cbt = load_vec_fm(k, sb, "cb", I["conv_b"][l], NF)
        wrot = mkrot(sb, "w", 4, [128, KC, 128], BF16)
        bg = Rot([(k.ps[i], k.tps[i]) for i in range(0, 3)])
        bv = Rot([(k.ps[i], k.tps[i]) for i in range(3, 6)])
        gbuf = mkrot(sb, "gbuf", 2, [128, S + 2], F32)
        for gb, gbt, _ in gbuf.items:
            P.op("dve", lambda e, gb=gb: e.memset(gb[:, 0:2], 0.0), writes=[gbt])
        t1 = mkrot(sb, "t1", 2, [128, 512], F32)
        ast = mkrot(sb, "ast", 3, [128, 512], BF16, key="st")
        wvg = I["w_gate"][l].rearrange("(kc p) n -> p kc n", p=128)
        wvv = I["w_val"][l].rearrange("(kc p) n -> p kc n", p=128)
        for f in range(NF):
            wg, wgt, wgk = wrot.next()
            wv_, wvt, wvk = wrot.next()
            fs = slice(f * 128, (f + 1) * 128)
            P.op("pool", lambda e, wg=wg, fs=fs: e.dma_start(out=wg[:], in_=wvg[:, :, fs]), writes=[wgt], dma_sem=wgk)
            P.op("pool", lambda e, wv_=wv_, fs=fs: e.dma_start(out=wv_[:], in_=wvv[:, :, fs]), writes=[wvt], dma_sem=wvk)
            gb, gbt, _ = gbuf.next()
            for c in range(NCH):
                cs = slice(c * 512, (c + 1) * 512)
                pg, pgt = bg.next()
                pv, pvt = bv.next()
                for kc in range(KC):
                    P.op("pe", lambda e, pg=pg, wg=wg, kc=kc, cs=cs: e.matmul(pg[:], lhsT=wg[:, kc, :], rhs=hT[:, kc, cs],
                                                                             start=(kc == 0), stop=(kc == KC - 1)),
                         reads=[wgt, t_h], writes=[pgt])
                for kc in range(KC):
                    P.op("pe", lambda e, pv=pv, wv_=wv_, kc=kc, cs=cs: e.matmul(pv[:], lhsT=wv_[:, kc, :], rhs=hT[:, kc, cs],
                                                                               start=(kc == 0), stop=(kc == KC - 1)),
                         reads=[wvt, t_h], writes=[pvt])
                c0 = c * 512
                P.op("act", lambda e, gb=gb, pg=pg, c0=c0: e.activation(out=gb[:, 2 + c0:2 + c0 + 512], in_=pg[:],
                                                                       func=AF.Copy), reads=[pgt], writes=[gbt], partial=True)
                t_, tt_, _ = t1.next()
                P.op("dve", lambda e, t_=t_, gb=gb, c0=c0, f=f: e.tensor_scalar(
                    out=t_[:], in0=gb[:, 2 + c0:2 + c0 + 512], scalar1=cw[2][0][:, f:f + 1], scalar2=cb[:, f:f + 1],
                    op0=ALU.mult, op1=ALU.add), reads=[gbt, cw[2][1], cbt], writes=[tt_])
                P.op("dve", lambda e, t_=t_, gb=gb, c0=c0, f=f: e.scalar_tensor_tensor(
                    out=t_[:], in0=gb[:, 1 + c0:1 + c0 + 512], scalar=cw[1][0][:, f:f + 1], in1=t_[:], op0=ALU.mult,
                    op1=ALU.add), reads=[gbt, cw[1][1], tt_], writes=[tt_])
                P.op("dve", lambda e, t_=t_, gb=gb, c0=c0, f=f: e.scalar_tensor_tensor(
                    out=t_[:], in0=gb[:, c0:c0 + 512], scalar=cw[0][0][:, f:f + 1], in1=t_[:], op0=ALU.mult,
                    op1=ALU.add), reads=[gbt, cw[0][1], tt_], writes=[tt_])
                P.op("act", lambda e, t_=t_: e.activation(out=t_[:], in_=t_[:], func=AF.Silu), reads=[tt_], writes=[tt_])
                a_, at_, ak = ast.next()
                P.op("dve", lambda e, a_=a_, t_=t_, pv=pv: e.tensor_tensor(out=a_[:], in0=pv[:], in1=t_[:], op=ALU.mult),
                     reads=[pvt, tt_], writes=[at_])
                P.op("sp", lambda e, a_=a_, fs=fs, cs=cs: e.dma_start(out=k.aT[fs, cs], in_=a_[:]), reads=[at_],
                     writes=[k.t_aT], dma_sem=ak, partial=True)


def phase_ffn_down(k, l):
    cfg, P, I = k.cfg, k.P, k.I
    S, D, NCH, KC, NF = cfg.S, cfg.D, cfg.NCH, cfg.KC, cfg.NF
    st, sb = new_phase(k)
    with st:
        aTc = sb("aTc", [128, NF, 512], BF16)
        nsp = 6
        per = (NF + nsp - 1) // nsp
        t_ap = [Tok("aTc%d" % i) for i in range(nsp)]
        wrot = mkrot(sb, "w", 3, [128, NF, 128], BF16)
        banks = Rot([(k.ps[i], k.tps[i]) for i in range(6)])
        tiles = [([(j * 128, 128)], 128) for j in range(KC)]
        cur_c = [0]
        ssq = SumSq(k, sb, lambda c: (k.ps[6 + c % 2], k.tps[6 + c % 2]), KC)
        ev = make_resid_evac(k, sb, lambda cc: cur_c[0] * 512, ssq)
        for c in range(NCH):
            src = k.aT[:, c * 512:(c + 1) * 512].rearrange("(f p) t -> p f t", p=128)
            for i in range(nsp):
                f0, f1 = i * per, min(NF, (i + 1) * per)
                if f0 >= f1:
                    continue
                P.op("sp" if i % 2 == 0 else "pool",
                     lambda e, f0=f0, f1=f1, src=src: e.dma_start(out=aTc[:, f0:f1, :], in_=src[:, f0:f1, :]),
                     reads=[k.t_aT], writes=[t_ap[i]], dma_sem="aTc%d" % i)
            cur_c[0] = c
            linear(k, None, NF, tiles, lambda kc, cc: (aTc[:, kc, :], t_ap[kc // per]), 1, ev, wrot, banks,
                   wsrc=lambda j: k.wdb[j].rearrange("p (kc n) -> p kc n", n=128), wq="act", wreads=[k.t_wdb])
            ssq.flush()
        ssq_finish(k, ssq)


def make_resid_evac_c(k, sb, c):
    if not hasattr(k, "_dn_rot") or k._dn_phase != k.cnt:
        k._dn_phase = k.cnt
        k._dn_rot = (mkrot(sb, "xr", 3, [128, 512], F32), mkrot(sb, "xo", 3, [128, 512], F32))
    xr, xo = k._dn_rot
    P = k.P

    def ev(j, cc, ps, pt, M):
        dst = k.xT[j * 128:(j + 1) * 128, c * 512:(c + 1) * 512]
        r, rt, rk = xr.next()
        o, ot, ok = xo.next()
        P.op("sp", lambda e: e.dma_start(out=r[:], in_=dst), reads=[k.t_xT[j]], writes=[rt], dma_sem=rk)
        P.op("dve", lambda e: e.tensor_tensor(out=o[:], in0=ps[:], in1=r[:], op=ALU.add), reads=[pt, rt], writes=[ot])
        P.op("sp", lambda e: e.dma_start(out=dst, in_=o[:]), reads=[ot], writes=[k.t_xT[j]], dma_sem=ok, partial=True)

    return ev


def phase_final(k):
    cfg, P, I = k.cfg, k.P, k.I
    S, D, NCH, KC = cfg.S, cfg.D, cfg.NCH, cfg.KC
    st, sb = new_phase(k)
    with st:
        gT, gtok = load_vec_fm(k, sb, "gf", I["final_norm_g"], KC)
        G = min(4, KC)
        xr = mkrot(sb, "nx", 3, [128, G, 512], F32)
        sq = mkrot(sb, "nsq", 3, [128, 512], BF16)
        rs = mkrot(sb, "nrs", 2, [128, 512], F32)
        yn = mkrot(sb, "yn", 3, [128, 512], F32)
        ost = mkrot(sb, "ost", 4, [128, 4, 128], F32, key="st")
        sbank = Rot([(k.ps[6], k.tps[6]), (k.ps[7], k.tps[7])])
        tbank = Rot([(k.ps[i], k.tps[i]) for i in range(6)])
        groups = [(g0, min(G, KC - g0)) for g0 in range(0, KC, G)]

        def src_of(g0, n, c):
            return k.xT[g0 * 128:(g0 + n) * 128, c * 512:(c + 1) * 512].rearrange("(g p) t -> p g t", p=128)

        cnt = 0
        for c in range(NCH):
            r, rt = k.rk[:, c, :], k.t_rk
            for (g0, n) in groups:
                x, xt, xk = xr.next()
                src = src_of(g0, n, c)
                P.op("sp", lambda e, x=x, src=src, n=n: e.dma_start(out=x[:, 0:n, :], in_=src),
                     reads=[k.t_xT[g0 + i] for i in range(n)], writes=[xt], dma_sem=xk)
                for i in range(n):
                    kc = g0 + i
                    y, yt, _ = yn.next()
                    P.op("dve", lambda e, x=x, y=y, kc=kc, r=r, i=i: e.scalar_tensor_tensor(
                        out=y[:], in0=x[:, i, :], scalar=gT[:, kc:kc + 1], in1=r, op0=ALU.mult, op1=ALU.mult),
                         reads=[xt, rt, gtok], writes=[yt])
                    tp, tpt = tbank.next()
                    for q in range(4):
                        P.op("pe", lambda e, tp=tp, y=y, q=q: e.transpose(out=tp[:, q * 128:(q + 1) * 128],
                                                                         in_=y[:, q * 128:(q + 1) * 128],
                                                                         identity=k.ident[:]),
                             reads=[yt, k.t_const], writes=[tpt], partial=(q > 0))
                    o, ot, ok = ost.next()
                    cnt += 1
                    if cnt % 3 == 0:
                        P.op("dve", lambda e, o=o, tp=tp: e.tensor_copy(out=o[:].rearrange("p a b -> p (a b)"), in_=tp[:]),
                             reads=[tpt], writes=[ot])
                    else:
                        P.op("act", lambda e, o=o, tp=tp: e.activation(out=o[:].rearrange("p a b -> p (a b)"), in_=tp[:],
                                                                      func=AF.Copy), reads=[tpt], writes=[ot])
                    dst = k.out[c * 512:(c + 1) * 512, kc * 128:(kc + 1) * 128].rearrange("(i p) f -> p i f", p=128)
                    P.op("act", lambda e, dst=dst, o=o: e.dma_start(out=dst, in_=o[:]), reads=[ot], writes=[k.t_out],
                         dma_sem=ok, partial=True)


_NC_CACHE = {}


def kernel(**inputs):
    cfg = Cfg()
    if "nc" not in _NC_CACHE:
        _NC_CACHE["nc"] = build_program(cfg)
    nc = _NC_CACHE["nc"]
    x = np.ascontiguousarray(inputs["x"], dtype=np.float32)
    B = x.shape[0]
    shared = {n: np.ascontiguousarray(inputs[n], dtype=np.float32) for n in inputs if n != "x"}
    in_maps = []
    for b in range(B):
        m = dict(shared)
        m["x"] = x[b]
        in_maps.append(m)
    res = run_bass_kernel_spmd(nc, in_maps, core_ids=list(range(B)))
    return np.stack([r["out"] for r in res.results], axis=0).astype(np.float32)
```

```python
import math
import numpy as np
from contextlib import ExitStack
import concourse.bass as bass
import concourse.mybir as mybir
from concourse.bass_utils import run_bass_kernel_spmd

F32 = mybir.dt.float32
BF16 = mybir.dt.bfloat16
I32 = mybir.dt.int32
AF = mybir.ActivationFunctionType
ALU = mybir.AluOpType
AX = mybir.AxisListType

COMPUTE = ("pe", "act", "dve", "pool")
ALL_ENG = ("pe", "act", "dve", "pool", "sp")
EPS = 1e-6
NEG = -30000.0


class Tok:
    __slots__ = ("name", "writers", "readers")

    def __init__(self, name=""):
        self.name = name
        self.writers = {}
        self.readers = {}


class Op:
    __slots__ = ("eng", "fn", "deps", "marked", "milestone", "dma_sem", "dma_val", "idx", "key")


class Prog:
    def __init__(self, nc):
        self.nc = nc
        self.ops = {e: [] for e in ALL_ENG}
        self.dma_sem_count = {}
        self.last = {}
        self.barrier_deps = {e: {} for e in ALL_ENG}
        self.n = 0

    def op(self, eng, fn, reads=(), writes=(), dma_sem=None, partial=False):
        o = Op()
        o.eng, o.fn, o.marked, o.milestone = eng, fn, False, None
        o.dma_sem, o.dma_val = dma_sem, None
        o.idx = self.n
        self.n += 1
        o.key = ("dma", dma_sem) if dma_sem is not None else eng
        deps = {}

        def add(d):
            for kk, dd in d.items():
                cur = deps.get(kk)
                if cur is None or cur.idx < dd.idx:
                    deps[kk] = dd

        if self.barrier_deps[eng]:
            add(self.barrier_deps[eng])
            self.barrier_deps[eng] = {}
        for r in reads:
            add(r.writers)
        for w in writes:
            if not partial:
                add(w.writers)
            add(w.readers)
        for r in reads:
            r.readers[o.key] = o
        for w in writes:
            if partial:
                w.writers[o.key] = o
            else:
                w.writers = {o.key: o}
            w.readers = {}
        if dma_sem is not None:
            v = self.dma_sem_count.get(dma_sem, 0) + 16
            self.dma_sem_count[dma_sem] = v
            o.dma_val = v
        fd = []
        for kk, d in deps.items():
            if d is o:
                continue
            if d.dma_sem is None:
                if d.eng == eng and eng == "pe":
                    continue
                d.marked = True
            fd.append(d)
        o.deps = fd
        self.ops[eng].append(o)
        self.last[o.key] = o
        return o

    def barrier(self):
        for e in ALL_ENG:
            self.barrier_deps[e] = dict(self.last)

    def emit(self):
        nc = self.nc
        with ExitStack() as st:
            esem = {e: st.enter_context(nc.semaphore("s_" + e)) for e in COMPUTE}
            dsem = {k: st.enter_context(nc.semaphore("d_%s" % (k,))) for k in self.dma_sem_count}
            for e in COMPUTE:
                c = 0
                for o in self.ops[e]:
                    if o.dma_sem is None and o.marked:
                        c += 1
                        o.milestone = c
                assert c < 60000, (e, c)
            for k_, v_ in self.dma_sem_count.items():
                assert v_ < 60000, (k_, v_)
            block = st.enter_context(nc.Block())

            def gen(ename):
                def body(eng):
                    seen = {}
                    for o in self.ops[ename]:
                        need = {}
                        for d in o.deps:
                            if d.dma_sem is not None:
                                s, v = dsem[d.dma_sem], d.dma_val
                            else:
                                s, v = esem[d.eng], d.milestone
                            key = id(s)
                            if seen.get(key, 0) >= v:
                                continue
                            if key not in need or need[key][1] < v:
                                need[key] = (s, v)
                        for key, (s, v) in need.items():
                            eng.wait_ge(s, v)
                            seen[key] = v
                        inst = o.fn(eng)
                        if o.dma_sem is not None:
                            inst.then_inc(dsem[o.dma_sem], 16)
                        elif o.marked:
                            inst.then_inc(esem[ename], 1)
                    if ename == "sp":
                        for k, v in self.dma_sem_count.items():
                            eng.wait_ge(dsem[k], v)
                        for e2 in COMPUTE:
                            m = 0
                            for o in self.ops[e2]:
                                if o.milestone:
                                    m = o.milestone
                            if m:
                                eng.wait_ge(esem[e2], m)
                return body

            block.tensor(gen("pe"))
            block.scalar(gen("act"))
            block.vector(gen("dve"))
            block.gpsimd(gen("pool"))
            block.sync(gen("sp"))


class Rot:
    def __init__(self, items):
        self.items = items
        self.i = 0

    def next(self):
        it = self.items[self.i % len(self.items)]
        self.i += 1
        return it


class Cfg:
    def __init__(self, S=2048, D=4096, H=8, QL=768, KVL=256, DFF=11008, L=2, theta=10000.0):
        self.S, self.D, self.H, self.QL, self.KVL, self.DFF, self.L, self.theta = S, D, H, QL, KVL, DFF, L, theta
        self.GW = H * 128
        self.DMIX = 4 * self.GW
        self.KC = D // 128
        self.NCH = S // 512
        self.NT = S // 128
        self.NF = DFF // 128
        GW = self.GW
        self.oAq, self.oAk, self.oAv, self.oBu, self.oBv = 0, GW, 2 * GW, 3 * GW, 4 * GW
        self.oCq = 5 * GW
        self.oCkv = self.oCq + QL
        self.oCkr = self.oCkv + KVL
        self.oDq = self.oCkr + 64
        self.oDk = self.oDq + GW
        self.oDv = self.oDk + GW
        self.oDf = self.oDv + GW
        self.INC = self.oDf + H
        self.NBLK = S // 256


class K:
    pass


def build_program(cfg, dbg=False):
    nc = bass.Bass("TRN2", target_bir_lowering=False)
    k = K()
    k.nc, k.cfg, k.P = nc, cfg, Prog(nc)
    P = k.P
    S, D, H, GW, L = cfg.S, cfg.D, cfg.H, cfg.GW, cfg.L
    KC, NCH, NT, NF = cfg.KC, cfg.NCH, cfg.NT, cfg.NF

    def din(name, shape):
        return nc.dram_tensor(name, list(shape), F32, kind="ExternalInput").ap()

    I = {}
    I["x"] = din("x", [S, D])
    I["norm_mix_g"] = din("norm_mix_g", [L, D])
    I["w_in"] = din("w_in", [L, D, cfg.INC])
    I["sgu_ln_g"] = din("sgu_ln_g", [L, H, 128])
    I["sgu_ln_b"] = din("sgu_ln_b", [L, H, 128])
    I["sgu_w"] = din("sgu_w", [L, H, 128, 128])
    I["sgu_b"] = din("sgu_b", [L, H, 128])
    I["mla_q_norm_g"] = din("mla_q_norm_g", [L, cfg.QL])
    I["mla_kv_norm_g"] = din("mla_kv_norm_g", [L, cfg.KVL])
    I["mla_w_uq"] = din("mla_w_uq", [L, cfg.QL, H * 192])
    I["mla_w_ukv"] = din("mla_w_ukv", [L, cfg.KVL, H * 256])
    I["fox_b_f"] = din("fox_b_f", [L, H])
    I["group_norm_g"] = din("group_norm_g", [L, 4, GW])
    I["w_o"] = din("w_o", [L, cfg.DMIX, D])
    I["norm_ffn_g"] = din("norm_ffn_g", [L, D])
    I["w_gate"] = din("w_gate", [L, D, cfg.DFF])
    I["w_val"] = din("w_val", [L, D, cfg.DFF])
    I["conv_w"] = din("conv_w", [L, 3, cfg.DFF])
    I["conv_b"] = din("conv_b", [L, cfg.DFF])
    I["w_down"] = din("w_down", [L, cfg.DFF, D])
    I["final_norm_g"] = din("final_norm_g", [D])
    k.I = I
    out = nc.dram_tensor("out", [S, D], F32, kind="ExternalOutput").ap()
    k.out = out
    skind = "ExternalOutput" if dbg else "Internal"

    def scratch(name, shape, dt=F32):
        return nc.dram_tensor(name, list(shape), dt, kind=skind).ap()

    k.xT = scratch("xT", [D, S])
    k.projT = scratch("projT", [cfg.INC, S])
    k.krT = scratch("krT", [64, S])
    k.qmT = scratch("qmT", [H * 192, S])
    k.kvT = scratch("kvT", [H * 256, S])
    k.ycatT = scratch("ycatT", [cfg.DMIX, S])
    k.aT = scratch("aT", [cfg.DFF, S], BF16)
    k.cosT = scratch("cosT", [64, S])
    k.sinT = scratch("sinT", [64, S])
    k.t_xT = [Tok("xT%d" % i) for i in range(KC)]
    k.t_proj = Tok("proj")
    k.t_kr = Tok("kr")
    k.t_qm = Tok("qm")
    k.t_kv = Tok("kv")
    k.t_ycat = Tok("ycat")
    k.t_aT = Tok("aT")
    k.t_rope = Tok("rope")
    k.t_out = Tok("out")
    k.wdb = scratch("wdb", [KC, 128, NF * 128], BF16)
    k.t_wdb = Tok("wdb")

    with ExitStack() as gst:
        k.ps = [gst.enter_context(nc.psum_tensor("ps%d" % i, [128, 512], F32)) for i in range(8)]
        k.tps = [Tok("ps%d" % i) for i in range(8)]
        k.ident = gst.enter_context(nc.sbuf_tensor("ident", [128, 128], F32))
        k.ones_bf = gst.enter_context(nc.sbuf_tensor("ones_bf", [128, 128], BF16))
        k.ones_f = gst.enter_context(nc.sbuf_tensor("ones_f", [128, 128], F32))
        k.t_const = Tok("const")
        k.rk = gst.enter_context(nc.sbuf_tensor("rstd_keep", [128, NCH, 512], F32))
        k.t_rk = Tok("rk")
        with ExitStack() as st:
            io = st.enter_context(nc.sbuf_tensor("io_tmp", [128, 128], F32))
            P.op("pool", lambda e: e.iota(io[:], pattern=[[1, 128]], base=0, channel_multiplier=-1,
                                          allow_small_or_imprecise_dtypes=True), writes=[k.t_const])
            P.op("dve", lambda e: e.tensor_single_scalar(out=k.ident[:], in_=io[:], scalar=0.0, op=ALU.is_equal),
                 reads=[k.t_const], writes=[k.t_const])
            P.op("dve", lambda e: e.memset(k.ones_f[:], 1.0), writes=[k.t_const], partial=True)
            P.op("dve", lambda e: e.memset(k.ones_bf[:], 1.0), writes=[k.t_const], partial=True)
            P.barrier()
        phase_rope_tables(k)
        phase_transpose_in(k)
        for l in range(L):
            phase_norm_inproj(k, l)
            phase_mla_proj(k, l)
            phase_attention(k, l)
            phase_gmlp(k, l)
            phase_wo(k, l)
            phase_ffn_up(k, l)
            phase_ffn_down(k, l)
        phase_final(k)
        P.emit()
    return nc


def new_phase(k):
    k.P.barrier()
    st = ExitStack()
    k.cnt = getattr(k, "cnt", 0) + 1
    pfx = "p%d_" % k.cnt

    def sb(name, shape, dt=F32):
        return st.enter_context(k.nc.sbuf_tensor(pfx + name, list(shape), dt))

    return st, sb


def mkrot(sb, name, n, shape, dt=F32, key=None):
    items = []
    for i in range(n):
        t = sb("%s%d" % (name, i), shape, dt)
        items.append((t, Tok("%s%d" % (name, i)), "%s%d" % (key or name, i)))
    return Rot(items)


def load_vec_fm(k, sb, name, vec_ap, n, rows=128):
    P = k.P
    tmp = sb(name + "_t", [n, rows], F32)
    dst = sb(name, [rows, n], F32)
    ttok, dtok = Tok(), Tok()
    if not hasattr(k, "t_vser"):
        k.t_vser = Tok("vser")
    P.op("sp", lambda e: e.dma_start(out=tmp[:], in_=vec_ap.rearrange("(n p) -> n p", p=rows)),
         writes=[ttok, k.t_vser], dma_sem="vec")
    ps, pt = k.ps[7], k.tps[7]
    P.op("pe", lambda e: e.transpose(out=ps[0:rows, 0:n], in_=tmp[0:n, 0:rows], identity=k.ident[0:n, 0:n]),
         reads=[ttok, k.t_const], writes=[pt])
    P.op("dve", lambda e: e.tensor_copy(out=dst[:], in_=ps[0:rows, 0:n]), reads=[pt], writes=[dtok])
    return dst, dtok


def rms_norm_fm(k, sb, src_fn, nK, nfeat, gT, gtok, dst_fn, nch, tagp="", rstd_pre=False):
    P = k.P
    P.barrier()
    G = min(4, nK)
    xr = mkrot(sb, tagp + "nx", 3, [128, G, 512], F32, key="nx")
    sq = mkrot(sb, tagp + "nsq", 3, [128, 512], BF16)
    rs = mkrot(sb, tagp + "nrs", 2, [128, 512], F32)
    banks = Rot([(k.ps[6], k.tps[6]), (k.ps[7], k.tps[7])])
    groups = [(g0, min(G, nK - g0)) for g0 in range(0, nK, G)]
    nq_ = [0]
    for c in range(nch):
        ps, pt = banks.next()
        for (g0, n) in ([] if rstd_pre else groups):
            x, xt, xk = xr.next()
            src, stoks = src_fn(g0, n, c)
            nq_[0] += 1
            P.op("sp" if nq_[0] % 2 else "pool", lambda e, x=x, src=src, n=n: e.dma_start(out=x[:, 0:n, :], in_=src),
                 reads=stoks, writes=[xt], dma_sem=xk)
            for i in range(n):
                kc = g0 + i
                s, sqt, _ = sq.next()
                P.op("act", lambda e, s=s, x=x, i=i: e.activation(out=s[:], in_=x[:, i, :], func=AF.Square), reads=[xt],
                     writes=[sqt])
                P.op("pe", lambda e, ps=ps, s=s, kc=kc: e.matmul(ps[:], lhsT=k.ones_bf[:], rhs=s[:], start=(kc == 0),
                                                                stop=(kc == nK - 1)),
                     reads=[sqt, k.t_const], writes=[pt])
        if rstd_pre:
            r, rt = k.rk[:, c, :], k.t_rk
        else:
            r_, rt, _ = rs.next()
            r = r_[:]
            P.op("act", lambda e, r=r, ps=ps: e.activation(out=r, in_=ps[:], func=AF.Sqrt, scale=1.0 / nfeat, bias=EPS),
                 reads=[pt], writes=[rt])
            P.op("dve", lambda e, r=r: e.reciprocal(out=r, in_=r), reads=[rt], writes=[rt])
        for (g0, n) in groups:
            x, xt, xk = xr.next()
            src, stoks = src_fn(g0, n, c)
            nq_[0] += 1
            P.op("sp" if nq_[0] % 2 else "pool", lambda e, x=x, src=src, n=n: e.dma_start(out=x[:, 0:n, :], in_=src),
                 reads=stoks, writes=[xt], dma_sem=xk)
            for i in range(n):
                kc = g0 + i
                dst, dtok = dst_fn(kc, c)
                P.op("dve", lambda e, x=x, dst=dst, kc=kc, r=r, i=i: e.scalar_tensor_tensor(
                    out=dst, in0=x[:, i, :], scalar=gT[:, kc:kc + 1], in1=r, op0=ALU.mult, op1=ALU.mult),
                     reads=[xt, rt, gtok], writes=[dtok], partial=True)


def linear(k, w2d, nK, col_tiles, act_fn, nch, evac, wrot, banks, wsrc=None, wq="pool", wreads=()):
    P = k.P
    wv = w2d.rearrange("(kc p) n -> p kc n", p=128) if w2d is not None else None
    for j, (pieces, M) in enumerate(col_tiles):
        wt, wtok, wkey = wrot.next()
        off = 0
        if wsrc is not None:
            src = wsrc(j)
            P.op(wq, lambda e, wt=wt, src=src: e.dma_start(out=wt[:, 0:nK, :], in_=src), reads=list(wreads),
                 writes=[wtok], dma_sem=wkey)
            pieces = []
        for (c0, n) in pieces:
            P.op("pool", lambda e, wt=wt, off=off, c0=c0, n=n: e.dma_start(out=wt[:, 0:nK, off:off + n],
                                                                          in_=wv[:, :, c0:c0 + n]),
                 writes=[wtok], dma_sem=wkey, partial=(off > 0))
            off += n
        for c in range(nch):
            ps, pt = banks.next()
            for kc in range(nK):
                a, atok = act_fn(kc, c)
                P.op("pe", lambda e, ps=ps, wt=wt, kc=kc, a=a, M=M: e.matmul(ps[0:M, :], lhsT=wt[:, kc, 0:M], rhs=a,
                                                                            start=(kc == 0), stop=(kc == nK - 1)),
                     reads=[wtok, atok], writes=[pt])
            evac(j, c, ps, pt, M)


class SumSq:
    def __init__(self, k, sb, acc_of_c, nj):
        self.k, self.acc_of_c, self.nj = k, acc_of_c, nj
        self.sq = mkrot(sb, "ssq", 4, [128, 512], BF16)
        self.pending = []
        self.fin = []

    def add(self, j, c, xo, xot):
        k, P = self.k, self.k.P
        s, st_, _ = self.sq.next()
        P.op("dve", lambda e: e.tensor_tensor(out=s[:], in0=xo[:], in1=xo[:], op=ALU.mult), reads=[xot], writes=[st_])
        self.pending.append((j, c, s, st_))
        if len(self.pending) > 2:
            self.flush(1)

    def flush(self, n=None):
        k, P = self.k, self.k.P
        D = k.cfg.D
        n = len(self.pending) if n is None else n
        for _ in range(n):
            j, c, s, st_ = self.pending.pop(0)
            acc, acct = self.acc_of_c(c)
            P.op("pe", lambda e, acc=acc, s=s, j=j: e.matmul(acc[:], lhsT=k.ones_bf[:], rhs=s[:], start=(j == 0),
                                                            stop=(j == self.nj - 1)),
                 reads=[st_, k.t_const], writes=[acct], partial=(j > 0))
            if j == self.nj - 1:
                P.op("dve", lambda e, acc=acc, c=c: e.tensor_scalar(out=k.rk[:, c, :], in0=acc[:], scalar1=1.0 / D,
                                                                    scalar2=EPS, op0=ALU.mult, op1=ALU.add),
                     reads=[acct], writes=[k.t_rk], partial=True)
                self.fin.append(c)


def ssq_finish(k, ssq, sqrt_eng="act"):
    P = k.P
    for c in ssq.fin:
        P.op(sqrt_eng, lambda e, c=c: e.activation(out=k.rk[:, c, :], in_=k.rk[:, c, :], func=AF.Sqrt), reads=[k.t_rk],
             writes=[k.t_rk], partial=True)
        P.op("dve", lambda e, c=c: e.reciprocal(out=k.rk[:, c, :], in_=k.rk[:, c, :]), reads=[k.t_rk],
             writes=[k.t_rk], partial=True)
    ssq.fin = []


def make_resid_evac(k, sb, coff_fn, ssq=None):
    P = k.P
    xr = mkrot(sb, "xr", 3, [128, 512], F32)
    xo = mkrot(sb, "xo", 4, [128, 512], F32)

    def ev(j, c, ps, pt, M):
        c0 = coff_fn(c)
        dst = k.xT[j * 128:(j + 1) * 128, c0:c0 + 512]
        r, rt, rk = xr.next()
        o, ot, ok = xo.next()
        P.op("sp", lambda e: e.dma_start(out=r[:], in_=dst), reads=[k.t_xT[j]], writes=[rt], dma_sem=rk)
        P.op("dve", lambda e: e.tensor_tensor(out=o[:], in0=ps[:], in1=r[:], op=ALU.add), reads=[pt, rt], writes=[ot])
        P.op("sp", lambda e: e.dma_start(out=dst, in_=o[:]), reads=[ot], writes=[k.t_xT[j]], dma_sem=ok, partial=True)
        if ssq is not None:
            ssq.add(j, coff_fn(c) // 512, o, ot)

    return ev


def phase_rope_tables(k):
    cfg, P = k.cfg, k.P
    S = cfg.S
    st, sb = new_phase(k)
    with st:
        pidx = sb("pidx", [64, 1])
        invf = sb("invf", [64, 1])
        pos = sb("pos", [64, S])
        ang = sb("ang", [64, S])
        kk = sb("kk", [64, S])
        ki = sb("ki", [64, S], I32)
        res = sb("res", [64, S])
        T = Tok("rt")
        P.op("pool", lambda e: e.iota(pidx[:], pattern=[[0, 1]], base=0, channel_multiplier=1,
                                      allow_small_or_imprecise_dtypes=True), writes=[T])
        P.op("pool", lambda e: e.iota(pos[:], pattern=[[1, S]], base=0, channel_multiplier=0,
                                      allow_small_or_imprecise_dtypes=True), reads=[T], writes=[T])
        P.op("act", lambda e: e.activation(out=invf[:], in_=pidx[:], func=AF.Exp, scale=-math.log(cfg.theta) / 32.0),
             reads=[T], writes=[T])
        P.op("dve", lambda e: e.tensor_scalar_mul(out=invf[32:64, :], in0=invf[32:64, :], scalar1=float(cfg.theta)),
             reads=[T], writes=[T])
        twopi = 2.0 * math.pi
        for which, shift, dstT in (("sin", 0.0, k.sinT), ("cos", math.pi / 2.0, k.cosT)):
            P.op("dve", lambda e, shift=shift: e.tensor_scalar(out=ang[:], in0=pos[:], scalar1=invf[:, 0:1], scalar2=shift,
                                                              op0=ALU.mult, op1=ALU.add), reads=[T], writes=[T])
            P.op("dve", lambda e: e.tensor_scalar_mul(out=kk[:], in0=ang[:], scalar1=1.0 / twopi), reads=[T], writes=[T])
            P.op("dve", lambda e: e.tensor_copy(out=ki[:], in_=kk[:]), reads=[T], writes=[T])
            P.op("dve", lambda e: e.tensor_copy(out=kk[:], in_=ki[:]), reads=[T], writes=[T])
            P.op("dve", lambda e: e.scalar_tensor_tensor(out=ang[:], in0=kk[:], scalar=-twopi, in1=ang[:], op0=ALU.mult,
                                                         op1=ALU.add), reads=[T], writes=[T])
            P.op("dve", lambda e: e.tensor_scalar(out=ang[:], in0=ang[:], scalar1=math.pi, scalar2=-math.pi, op0=ALU.min,
                                                  op1=ALU.max), reads=[T], writes=[T])
            P.op("act", lambda e: e.activation(out=res[:], in_=ang[:], func=AF.Sin), reads=[T], writes=[T])
            if which == "sin":
                P.op("dve", lambda e: e.tensor_scalar_mul(out=res[0:32, :], in0=res[0:32, :], scalar1=-1.0), reads=[T],
                     writes=[T])
            P.op("sp", lambda e, dstT=dstT: e.dma_start(out=dstT, in_=res[:]), reads=[T], writes=[k.t_rope, T],
                 dma_sem="st0", partial=True)


def phase_transpose_in(k):
    cfg, P = k.cfg, k.P
    S, D, KC, NT = cfg.S, cfg.D, cfg.KC, cfg.NT
    st, sb = new_phase(k)
    with st:
        xin = mkrot(sb, "xin", 2, [128, D], F32)
        stg = mkrot(sb, "stg", 3, [128, 4, 128], F32)
        banks = Rot([(k.ps[i], k.tps[i]) for i in range(6)])
        tx = Tok("x")
        n = 0
        for tt in range(NT):
            x, xt, xk = xin.next()
            P.op("sp", lambda e, x=x, tt=tt: e.dma_start(out=x[:], in_=k.I["x"][tt * 128:(tt + 1) * 128, :]), reads=[tx],
                 writes=[xt], dma_sem=xk)
            for g in range(KC // 4):
                ps, pt = banks.next()
                for i in range(4):
                    kc = g * 4 + i
                    P.op("pe", lambda e, ps=ps, x=x, i=i, kc=kc: e.transpose(out=ps[:, i * 128:(i + 1) * 128],
                                                                            in_=x[:, kc * 128:(kc + 1) * 128],
                                                                            identity=k.ident[:]),
                         reads=[xt, k.t_const], writes=[pt], partial=(i > 0))
                s, stok, sk = stg.next()
                n += 1
                if n % 2:
                    P.op("act", lambda e, s=s, ps=ps: e.activation(out=s[:].rearrange("p a b -> p (a b)"), in_=ps[:],
                                                                  func=AF.Copy), reads=[pt], writes=[stok])
                else:
                    P.op("dve", lambda e, s=s, ps=ps: e.tensor_copy(out=s[:].rearrange("p a b -> p (a b)"), in_=ps[:]),
                         reads=[pt], writes=[stok])
                dst = k.xT[g * 512:(g + 1) * 512, tt * 128:(tt + 1) * 128].rearrange("(i p) t -> p i t", p=128)
                P.op("act", lambda e, dst=dst, s=s: e.dma_start(out=dst, in_=s[:]), reads=[stok],
                     writes=[k.t_xT[g * 4 + i] for i in range(4)], dma_sem=sk, partial=True)


def phase_norm_inproj(k, l):
    cfg, P, I = k.cfg, k.P, k.I
    S, D, H, GW, KC, NCH = cfg.S, cfg.D, cfg.H, cfg.GW, cfg.KC, cfg.NCH
    st, sb = new_phase(k)
    with st:
        hT = sb("hT", [128, KC, S], BF16)
        t_h = Tok("hT")
        gT, gtok = load_vec_fm(k, sb, "g1", I["norm_mix_g"][l], KC)
        with ExitStack() as st2:
            def sb2(name, shape, dt=F32):
                return st2.enter_context(k.nc.sbuf_tensor("n1_%d_%s" % (l, name), list(shape), dt))
            rms_norm_fm(k, sb2, lambda g0, n, c: (k.xT[g0 * 128:(g0 + n) * 128, c * 512:(c + 1) * 512].rearrange("(g p) t -> p g t", p=128), [k.t_xT[g0 + i] for i in range(n)]), KC, D,
                        gT, gtok, lambda kc, c: (hT[:, kc, c * 512:(c + 1) * 512], t_h), NCH, rstd_pre=(l > 0))
            P.barrier()
        cosS = sb("cos", [64, S])
        sinS = sb("sin", [64, S])
        t_cs = Tok("cs")
        P.op("sp", lambda e: e.dma_start(out=cosS[:], in_=k.cosT), reads=[k.t_rope], writes=[t_cs], dma_sem="ld0")
        P.op("sp", lambda e: e.dma_start(out=sinS[:], in_=k.sinT), reads=[k.t_rope], writes=[t_cs], dma_sem="ld1",
             partial=True)
        krraw = sb("krraw", [64, S])
        t_krraw = Tok("krraw")
        wrot = mkrot(sb, "w", 3, [128, KC, 128], BF16)
        banks = Rot([(k.ps[i], k.tps[i]) for i in range(6)])
        stg = mkrot(sb, "stg", 3, [128, 512], F32, key="st")
        tiles = []
        kinds = []
        sc_q = 128.0 ** -0.5
        for c0 in range(0, cfg.oCkr, 128):
            tiles.append(([(c0, 128)], 128))
            if c0 < GW:
                kinds.append(("scale", sc_q, c0))
            elif cfg.oBu <= c0 < cfg.oCq:
                kinds.append(("gelu", 1.0, c0))
            else:
                kinds.append(("scale", 1.0, c0))
        tiles.append(([(cfg.oCkr, 64)], 64))
        kinds.append(("kr", 1.0, 0))
        tiles.append(([(cfg.oCkr + 32, 32), (cfg.oCkr, 32)], 64))
        kinds.append(("krsw", 1.0, 0))
        for c0 in range(cfg.oDq, cfg.oDf, 128):
            tiles.append(([(c0, 128)], 128))
            kinds.append(("scale", sc_q if c0 < cfg.oDk else 1.0, c0))
        tiles.append(([(cfg.oDf, H)], H))
        kinds.append(("scale", 1.0, cfg.oDf))
        cnt = [0]

        def evac(j, c, ps, pt, M):
            kind, sc, r0 = kinds[j]
            cs = slice(c * 512, (c + 1) * 512)
            if kind == "kr":
                P.op("act", lambda e: e.activation(out=krraw[:, cs], in_=ps[0:64, :], func=AF.Copy), reads=[pt],
                     writes=[t_krraw], partial=True)
                return
            s, stok, sk = stg.next()
            if kind == "krsw":
                P.op("dve", lambda e: e.tensor_tensor(out=s[0:64, :], in0=ps[0:64, :], in1=sinS[:, cs], op=ALU.mult),
                     reads=[pt, t_cs], writes=[stok])
                P.op("dve", lambda e: e.tensor_tensor(out=krraw[:, cs], in0=krraw[:, cs], in1=cosS[:, cs], op=ALU.mult),
                     reads=[t_krraw, t_cs], writes=[t_krraw], partial=True)
                P.op("dve", lambda e: e.tensor_tensor(out=s[0:64, :], in0=s[0:64, :], in1=krraw[:, cs], op=ALU.add),
                     reads=[t_krraw, stok], writes=[stok])
                P.op("sp", lambda e: e.dma_start(out=k.krT[:, cs], in_=s[0:64, :]), reads=[stok], writes=[k.t_kr],
                     dma_sem=sk, partial=True)
                return
            if kind == "gelu":
                P.op("act", lambda e: e.activation(out=s[0:M, :], in_=ps[0:M, :], func=AF.Gelu_apprx_tanh), reads=[pt],
                     writes=[stok])
            else:
                cnt[0] += 1
                if cnt[0] % 2:
                    P.op("act", lambda e: e.activation(out=s[0:M, :], in_=ps[0:M, :], func=AF.Copy, scale=float(sc)),
                         reads=[pt], writes=[stok])
                else:
                    P.op("dve", lambda e: e.tensor_scalar_mul(out=s[0:M, :], in0=ps[0:M, :], scalar1=float(sc)),
                         reads=[pt], writes=[stok])
            P.op("sp", lambda e: e.dma_start(out=k.projT[r0:r0 + M, cs], in_=s[0:M, :]), reads=[stok], writes=[k.t_proj],
                 dma_sem=sk, partial=True)

        linear(k, I["w_in"][l], KC, tiles, lambda kc, c: (hT[:, kc, c * 512:(c + 1) * 512], t_h), NCH, evac, wrot, banks)


def phase_mla_proj(k, l):
    cfg, P, I = k.cfg, k.P, k.I
    S, H, NCH = cfg.S, cfg.H, cfg.NCH
    nq, nkv = cfg.QL // 128, cfg.KVL // 128
    st, sb = new_phase(k)
    with st:
        cqn = sb("cqn", [128, nq, S], BF16)
        ckvn = sb("ckvn", [128, nkv, S], BF16)
        t_cqn, t_ckvn = Tok(), Tok()
        gq, gqt = load_vec_fm(k, sb, "gq", I["mla_q_norm_g"][l], nq)
        gkv, gkvt = load_vec_fm(k, sb, "gkv", I["mla_kv_norm_g"][l], nkv)
        rms_norm_fm(k, sb, lambda g0, n, c: (k.projT[cfg.oCq + g0 * 128:cfg.oCq + (g0 + n) * 128, c * 512:(c + 1) * 512].rearrange(
            "(g p) t -> p g t", p=128), [k.t_proj]), nq, cfg.QL, gq, gqt,
                    lambda kc, c: (cqn[:, kc, c * 512:(c + 1) * 512], t_cqn), NCH, tagp="q")
        rms_norm_fm(k, sb, lambda g0, n, c: (k.projT[cfg.oCkv + g0 * 128:cfg.oCkv + (g0 + n) * 128, c * 512:(c + 1) * 512].rearrange(
            "(g p) t -> p g t", p=128), [k.t_proj]), nkv, cfg.KVL, gkv, gkvt,
                    lambda kc, c: (ckvn[:, kc, c * 512:(c + 1) * 512], t_ckvn), NCH, tagp="kv")
        cosS = sb("cos", [64, S])
        sinS = sb("sin", [64, S])
        t_cs = Tok("cs")
        P.op("sp", lambda e: e.dma_start(out=cosS[:], in_=k.cosT), reads=[k.t_rope], writes=[t_cs], dma_sem="ld0")
        P.op("sp", lambda e: e.dma_start(out=sinS[:], in_=k.sinT), reads=[k.t_rope], writes=[t_cs], dma_sem="ld1",
             partial=True)
        qraw = sb("qraw", [64, S])
        t_qraw = Tok()
        wq = mkrot(sb, "wq", 3, [128, nq, 128], BF16, key="w")
        wkv = mkrot(sb, "wkv", 3, [128, nkv, 128], BF16, key="w")
        banks = Rot([(k.ps[i], k.tps[i]) for i in range(6)])
        stg = mkrot(sb, "stg", 3, [128, 512], F32, key="st")
        sc = 192.0 ** -0.5
        tiles, kinds = [], []
        for h in range(H):
            tiles.append(([(h * 192, 128)], 128)); kinds.append(("nope", h))
            tiles.append(([(h * 192 + 128, 64)], 64)); kinds.append(("r", h))
            tiles.append(([(h * 192 + 160, 32), (h * 192 + 128, 32)], 64)); kinds.append(("rsw", h))

        def evac_q(j, c, ps, pt, M):
            kind, h = kinds[j]
            cs = slice(c * 512, (c + 1) * 512)
            if kind == "r":
                P.op("act", lambda e: e.activation(out=qraw[:, cs], in_=ps[0:64, :], func=AF.Copy), reads=[pt],
                     writes=[t_qraw], partial=True)
                return
            s, stok, sk = stg.next()
            if kind == "rsw":
                P.op("dve", lambda e: e.tensor_tensor(out=s[0:64, :], in0=ps[0:64, :], in1=sinS[:, cs], op=ALU.mult),
                     reads=[pt, t_cs], writes=[stok])
                P.op("dve", lambda e: e.tensor_tensor(out=qraw[:, cs], in0=qraw[:, cs], in1=cosS[:, cs], op=ALU.mult),
                     reads=[t_qraw, t_cs], writes=[t_qraw], partial=True)
                P.op("dve", lambda e: e.scalar_tensor_tensor(out=s[0:64, :], in0=s[0:64, :], scalar=1.0, in1=qraw[:, cs],
                                                             op0=ALU.mult, op1=ALU.add),
                     reads=[t_qraw, stok], writes=[stok])
                P.op("act", lambda e: e.activation(out=s[0:64, :], in_=s[0:64, :], func=AF.Copy, scale=sc), reads=[stok],
                     writes=[stok])
                P.op("sp", lambda e: e.dma_start(out=k.qmT[h * 192 + 128:h * 192 + 192, cs], in_=s[0:64, :]),
                     reads=[stok], writes=[k.t_qm], dma_sem=sk, partial=True)
                return
            P.op("act", lambda e: e.activation(out=s[:], in_=ps[:], func=AF.Copy, scale=sc), reads=[pt], writes=[stok])
            P.op("sp", lambda e: e.dma_start(out=k.qmT[h * 192:h * 192 + 128, cs], in_=s[:]), reads=[stok],
                 writes=[k.t_qm], dma_sem=sk, partial=True)

        linear(k, I["mla_w_uq"][l], nq, tiles, lambda kc, c: (cqn[:, kc, c * 512:(c + 1) * 512], t_cqn), NCH, evac_q, wq,
               banks)
        tiles2 = [([(j * 128, 128)], 128) for j in range(2 * H)]

        def evac_kv(j, c, ps, pt, M):
            cs = slice(c * 512, (c + 1) * 512)
            s, stok, sk = stg.next()
            P.op("dve", lambda e: e.tensor_copy(out=s[:], in_=ps[:]), reads=[pt], writes=[stok])
            P.op("sp", lambda e: e.dma_start(out=k.kvT[j * 128:(j + 1) * 128, cs], in_=s[:]), reads=[stok],
                 writes=[k.t_kv], dma_sem=sk, partial=True)

        linear(k, I["mla_w_ukv"][l], nkv, tiles2, lambda kc, c: (ckvn[:, kc, c * 512:(c + 1) * 512], t_ckvn), NCH, evac_kv,
               wkv, banks)


def phase_attention(k, l):
    cfg, P, I = k.cfg, k.P, k.I
    S, H, GW, NCH, NT, NBLK = cfg.S, cfg.H, cfg.GW, cfg.NCH, cfg.NT, cfg.NBLK
    NOH = max(NBLK, H)
    st, sb = new_phase(k)
    with st:
        T = Tok("acst")
        iot = sb("iot", [128, 512])
        negm = [sb("negm%d" % i, [128, 512]) for i in range(4)]
        pio = sb("pio", [NOH, 128])
        oh = [sb("oh%d" % n, [NOH, 128], BF16) for n in range(NOH)]
        ohm = [sb("ohm%d" % n, [65, 128], BF16) for n in range(NBLK)]
        iot16 = sb("iot16", [128, NT])
        arows = sb("arows", [65, S])
        P.op("pool", lambda e: e.iota(iot[:], pattern=[[1, 512]], base=0, channel_multiplier=-1,
                                      allow_small_or_imprecise_dtypes=True), writes=[T])
        P.op("pool", lambda e: e.iota(pio[:], pattern=[[0, 128]], base=0, channel_multiplier=1,
                                      allow_small_or_imprecise_dtypes=True), reads=[T], writes=[T])
        P.op("pool", lambda e: e.iota(iot16[:], pattern=[[128, NT]], base=0, channel_multiplier=1,
                                      allow_small_or_imprecise_dtypes=True), reads=[T], writes=[T])
        P.op("pool", lambda e: e.iota(arows[32:33, :].rearrange("p (a b) -> p a b", b=128), pattern=[[128, NT], [0, 128]],
                                      base=0, channel_multiplier=0, allow_small_or_imprecise_dtypes=True),
             reads=[T], writes=[T])
        P.op("pool", lambda e: e.iota(arows[64:65, :].rearrange("p (a b) -> p a b", b=128), pattern=[[0, NT], [1, 128]],
                                      base=0, channel_multiplier=0, allow_small_or_imprecise_dtypes=True),
             reads=[T], writes=[T])
        for i in range(4):
            P.op("dve", lambda e, i=i: e.tensor_scalar(out=negm[i][:], in0=iot[:], scalar1=float(128 * i), scalar2=NEG,
                                                      op0=ALU.is_lt, op1=ALU.mult), reads=[T], writes=[T])
        for n in range(NOH):
            P.op("dve", lambda e, n=n: e.tensor_single_scalar(out=oh[n][:], in_=pio[:], scalar=float(n), op=ALU.is_equal),
                 reads=[T], writes=[T])
        for n in range(NBLK):
            P.op("dve", lambda e, n=n: e.memset(ohm[n][:], 0.0), reads=[T], writes=[T])
            P.op("dve", lambda e, n=n: e.tensor_copy(out=ohm[n][0:NBLK, :], in_=oh[n][0:NBLK, :]), reads=[T], writes=[T])
            P.op("dve", lambda e, n=n: e.memset(ohm[n][32:33, :], 1.0), reads=[T], writes=[T])
            P.op("dve", lambda e, n=n: e.memset(ohm[n][64:65, :], 1.0), reads=[T], writes=[T])

        vio = sb("vio", [128, NT, 8])
        vmask = sb("vmask", [128, NT, 8])
        nfill = sb("nfill", [128, NT, 8])
        P.op("pool", lambda e: e.iota(vio[:], pattern=[[1, NT], [-2, 8]], base=0, channel_multiplier=0,
                                      allow_small_or_imprecise_dtypes=True), reads=[T], writes=[T])
        P.op("dve", lambda e: e.tensor_single_scalar(out=vmask[:], in_=vio[:], scalar=2.0, op=ALU.is_ge), reads=[T],
             writes=[T])
        P.op("dve", lambda e: e.tensor_scalar(out=nfill[:], in0=vio[:], scalar1=2.0, scalar2=-1e30, op0=ALU.is_lt,
                                              op1=ALU.mult), reads=[T], writes=[T])
        fA = sb("fA", [H, S])
        fB = sb("fB", [H, S])
        bfv = sb("bfv", [H, 1])
        cs3 = [sb("cs3_%d" % i, [H, S], BF16) for i in range(3)]
        csr = sb("csr", [H, S])
        cs_tm = sb("cs_tm", [128, NT, H])
        TF = Tok("fox")
        fox_state = {}

        def fox_prep():
            P.op("sp", lambda e: e.dma_start(out=fA[:], in_=k.projT[cfg.oDf:cfg.oDf + H, :]), reads=[k.t_proj], writes=[TF],
                 dma_sem="ld0")
            P.op("sp", lambda e: e.dma_start(out=bfv[:], in_=I["fox_b_f"][l].rearrange("(h o) -> h o", o=1)), writes=[TF],
                 dma_sem="ld1", partial=True)
            P.op("act", lambda e: e.activation(out=bfv[:], in_=bfv[:], func=AF.Copy, scale=-1.0), reads=[TF], writes=[TF])
            P.op("act", lambda e: e.activation(out=fB[:], in_=fA[:], func=AF.Exp, bias=bfv[:, 0:1], scale=-1.0), reads=[TF],
                 writes=[TF])
            P.op("act", lambda e: e.activation(out=fA[:], in_=fB[:], func=AF.Ln, bias=1.0), reads=[TF], writes=[TF])
            a, b = fA, fB
            d = 1
            while d < S:
                P.op("pool", lambda e, a=a, b=b, d=d: e.tensor_tensor(out=b[:, d:S], in0=a[:, d:S], in1=a[:, 0:S - d], op=ALU.add),
                     reads=[TF], writes=[TF])
                P.op("pool", lambda e, a=a, b=b, d=d: e.tensor_copy(out=b[:, 0:d], in_=a[:, 0:d]), reads=[TF], writes=[TF])
                a, b = b, a
                d *= 2
            cs = a
            oth = b
            P.op("act", lambda e: e.activation(out=oth[:], in_=cs[:], func=AF.Copy, scale=-1.0), reads=[TF], writes=[TF])
            for i in range(3):
                P.op("pool", lambda e, i=i: e.tensor_copy(out=cs3[i][:], in_=oth[:]), reads=[TF], writes=[TF])
                if i < 2:
                    P.op("pool", lambda e, i=i: e.tensor_copy(out=csr[:], in_=cs3[i][:]), reads=[TF], writes=[TF])
                    P.op("pool", lambda e: e.tensor_tensor(out=oth[:], in0=oth[:], in1=csr[:], op=ALU.subtract), reads=[TF],
                         writes=[TF])
            fox_state['cs'] = cs

        def fox_prep_pe():
            cs = fox_state['cs']
            for tt in range(NT):
                P.op("pe", lambda e, tt=tt: e.transpose(out=k.ps[7][:, tt * H:(tt + 1) * H],
                                                       in_=cs[0:H, tt * 128:(tt + 1) * 128],
                                                       identity=k.ident[0:H, 0:H]), reads=[TF, k.t_const],
                     writes=[k.tps[7]], partial=(tt > 0))
            P.op("dve", lambda e: e.tensor_copy(out=cs_tm[:].rearrange("p a b -> p (a b)"), in_=k.ps[7][:, 0:NT * H]),
                 reads=[k.tps[7]], writes=[TF], partial=True)

        qf = mkrot(sb, "qf", 2, [128, S], F32, key="hq")
        kf = mkrot(sb, "kf", 2, [128, S], F32, key="hk")
        vf = mkrot(sb, "vf", 2, [128, S], F32, key="hv")
        qb = mkrot(sb, "qb", 2, [128, S], BF16, key="hqb")
        kb = mkrot(sb, "kb", 2, [128, S], BF16, key="hkb")
        q2 = mkrot(sb, "q2", 2, [64, S], BF16, key="hq2")
        q2f = mkrot(sb, "q2f", 2, [64, S], F32, key="hq2f")
        vtm = mkrot(sb, "vtm", 2, [128, NT, 128], BF16)
        selbT = mkrot(sb, "selbT", 2, [65, S], BF16)
        mbias = mkrot(sb, "mbias", 2, [128, NT], F32)
        k2 = sb("k2", [64, S], BF16)
        t_k2 = Tok("k2")
        kmean = sb("kmean", [128, 8])
        g8 = sb("g8", [128, NT, 8])
        cmp4 = sb("cmp4", [128, NT, 8, 8])
        cnt8 = sb("cnt8", [128, NT, 8])
        selb = sb("selb", [128, NT, 8])
        TG = Tok("gate")
        lnd = mkrot(sb, "lnd", 2, [128, 512], F32)
        pT = mkrot(sb, "pT", 3, [128, 512], BF16)
        tmp = mkrot(sb, "tmp", 3, [128, 512], F32)
        rden = mkrot(sb, "rden", 2, [128, 512], F32)
        yst = mkrot(sb, "yst", 2, [128, 512], F32, key="st")
        sb_rot = Rot([(k.ps[i], k.tps[i]) for i in range(3)])
        o_rot = Rot([(k.ps[3], k.tps[3]), (k.ps[4], k.tps[4])])
        d_rot = Rot([(k.ps[5], k.tps[5]), (k.ps[6], k.tps[6])])
        misc, tmisc = k.ps[7], k.tps[7]

        P.op("pool", lambda e: e.dma_start(out=k2[:], in_=k.krT), reads=[k.t_kr], writes=[t_k2], dma_sem="k2")

        heads = []
        for h in range(H):
            heads.append(("moba", h))
        for h in range(H):
            heads.append(("mla", h))
        for h in range(H):
            heads.append(("fox", h))
        NHD = len(heads)
        HS = {}
        KCd = cfg.KC
        pc_per = (KCd + NHD - 1) // NHD
        wdv = I["w_down"][l].rearrange("(kc p) n -> p kc n", p=128)

        def prep_dma(hi):
            kind, h = heads[hi]
            stt = {}
            HS[hi] = stt
            if kind == "moba":
                qsrc = k.projT[cfg.oAq + h * 128:cfg.oAq + (h + 1) * 128, :]
                ksrc = k.projT[cfg.oAk + h * 128:cfg.oAk + (h + 1) * 128, :]
                vsrc = k.projT[cfg.oAv + h * 128:cfg.oAv + (h + 1) * 128, :]
                rtoks = [k.t_proj]
                stt["ydst"] = k.ycatT[h * 128:(h + 1) * 128, :]
                stt["slope"] = 2.0 ** (-8.0 * (h + 1) / H)
            elif kind == "mla":
                qsrc = k.qmT[h * 192:h * 192 + 128, :]
                ksrc = k.kvT[h * 256:h * 256 + 128, :]
                vsrc = k.kvT[h * 256 + 128:h * 256 + 256, :]
                rtoks = [k.t_qm, k.t_kv]
                stt["ydst"] = k.ycatT[2 * GW + h * 128:2 * GW + (h + 1) * 128, :]
            else:
                qsrc = k.projT[cfg.oDq + h * 128:cfg.oDq + (h + 1) * 128, :]
                ksrc = k.projT[cfg.oDk + h * 128:cfg.oDk + (h + 1) * 128, :]
                vsrc = k.projT[cfg.oDv + h * 128:cfg.oDv + (h + 1) * 128, :]
                rtoks = [k.t_proj]
                stt["ydst"] = k.ycatT[3 * GW + h * 128:3 * GW + (h + 1) * 128, :]
            v_, vt, vk = vf.next()
            stt["v"] = (v_, vt)
            P.op("sp", lambda e: e.dma_start(out=v_[:], in_=vsrc), reads=rtoks, writes=[vt], dma_sem=vk)
            qb_, qbt, qbk = qb.next()
            kb_, kbt, kbk = kb.next()
            stt["qb"] = (qb_, qbt)
            stt["kb"] = (kb_, kbt)
            q_, qt, qk = qf.next()
            k_, kt_, kk = kf.next()
            stt["qf"] = (q_, qt)
            stt["kf"] = (k_, kt_)
            P.op("sp", lambda e: e.dma_start(out=q_[:], in_=qsrc), reads=rtoks, writes=[qt], dma_sem=qk)
            P.op("sp", lambda e: e.dma_start(out=k_[:], in_=ksrc), reads=rtoks, writes=[kt_], dma_sem=kk)
            P.op("act", lambda e: e.activation(out=qb_[:], in_=q_[:], func=AF.Copy), reads=[qt], writes=[qbt])
            P.op("dve", lambda e: e.tensor_copy(out=kb_[:], in_=k_[:]), reads=[kt_], writes=[kbt])
            if kind == "mla":
                q2_, q2t, q2k = q2.next()
                q2f_, q2ft, q2fk = q2f.next()
                stt["q2"] = (q2_, q2t)
                P.op("sp", lambda e: e.dma_start(out=q2f_[:], in_=k.qmT[h * 192 + 128:h * 192 + 192, :]),
                     reads=[k.t_qm], writes=[q2ft], dma_sem=q2fk)
                P.op("dve", lambda e: e.tensor_copy(out=q2_[:], in_=q2f_[:]), reads=[q2ft], writes=[q2t])
            for j in range(hi * pc_per, min(KCd, (hi + 1) * pc_per)):
                P.op("pool", lambda e, j=j: e.dma_start(out=k.wdb[j].rearrange("p (kc n) -> p kc n", n=128),
                                                        in_=wdv[:, :, j * 128:(j + 1) * 128]),
                     writes=[k.t_wdb], dma_sem="pc%d" % (j % 2), partial=True)

        def prep_pe1(hi):
            kind, h = heads[hi]
            stt = HS[hi]
            if kind != "moba":
                return
            q_, qt = stt["qf"]
            k_, kt_ = stt["kf"]
            sT, sTt, _ = selbT.next()
            stt["sT"] = (sT, sTt)
            P.op("dve", lambda e: e.memset(kmean[:], 0.0), reads=[TG], writes=[TG])
            P.op("dve", lambda e: e.reduce_sum(out=kmean[:, 0:NBLK], in_=k_[:].rearrange("p (n b) -> p n b", b=256),
                                               axis=AX.X), reads=[kt_, TG], writes=[TG])
            P.op("dve", lambda e: e.memset(sT[:], 0.0), writes=[sTt])
            slope_ = stt["slope"]
            P.op("dve", lambda e: e.tensor_scalar_mul(out=sT[32:33, :], in0=arows[32:33, :], scalar1=-slope_),
                 reads=[T, sTt], writes=[sTt], partial=True)
            P.op("dve", lambda e: e.tensor_scalar_mul(out=sT[64:65, :], in0=arows[64:65, :], scalar1=-slope_),
                 reads=[T, sTt], writes=[sTt], partial=True)
            mb, mbt, _ = mbias.next()
            stt["mb"] = (mb, mbt)
            P.op("dve", lambda e: e.tensor_scalar_mul(out=mb[:], in0=iot16[:], scalar1=slope_), reads=[T], writes=[mbt])
            for tt in range(NT):
                P.op("pe", lambda e, tt=tt: e.matmul(misc[:, tt * 8:(tt + 1) * 8], lhsT=q_[:, tt * 128:(tt + 1) * 128],
                                                    rhs=kmean[:], start=True, stop=True),
                     reads=[qt, TG], writes=[tmisc], partial=(tt > 0))
            g8f = g8[:].rearrange("p a b -> p (a b)")
            P.op("dve", lambda e: e.tensor_tensor(out=g8f, in0=misc[:, 0:NT * 8], in1=vmask[:].rearrange("p a b -> p (a b)"),
                                                  op=ALU.mult), reads=[tmisc, T, TG], writes=[TG])
            P.op("dve", lambda e: e.tensor_tensor(out=g8f, in0=g8f, in1=nfill[:].rearrange("p a b -> p (a b)"), op=ALU.add),
                 reads=[TG, T], writes=[TG])
            P.op("dve", lambda e: e.tensor_tensor(out=cmp4[:], in0=g8[:].unsqueeze(2).to_broadcast([128, NT, 8, 8]),
                                                  in1=g8[:].unsqueeze(3).to_broadcast([128, NT, 8, 8]), op=ALU.is_gt),
                 reads=[TG], writes=[TG])
            P.op("dve", lambda e: e.reduce_sum(out=cnt8[:], in_=cmp4[:], axis=AX.X), reads=[TG], writes=[TG])
            P.op("dve", lambda e: e.tensor_scalar(out=selb[:], in0=cnt8[:], scalar1=3.0, scalar2=NEG, op0=ALU.is_ge,
                                                  op1=ALU.mult), reads=[TG], writes=[TG])
            P.op("dve", lambda e: e.tensor_tensor(out=selb[:], in0=selb[:], in1=vmask[:], op=ALU.mult), reads=[TG, T],
                 writes=[TG])

        def prep_pe2(hi):
            kind, h = heads[hi]
            stt = HS[hi]
            v_, vt = stt["v"]
            if kind == "moba":
                sT, sTt = stt["sT"]
                for g in range(NT // 4):
                    for i in range(4):
                        tt = g * 4 + i
                        P.op("pe", lambda e, tt=tt, i=i: e.transpose(out=misc[0:8, i * 128:(i + 1) * 128],
                                                                    in_=selb[:, tt, :], identity=k.ident[:]),
                             reads=[TG, k.t_const], writes=[tmisc], partial=(i > 0))
                    P.op("dve", lambda e, g=g: e.tensor_copy(out=sT[0:NBLK, g * 512:(g + 1) * 512], in_=misc[0:NBLK, :]),
                         reads=[tmisc, sTt], writes=[sTt], partial=True)
            vm, vmt, _ = vtm.next()
            stt["vm"] = (vm, vmt)
            for g in range(NT // 4):
                for i in range(4):
                    tt = g * 4 + i
                    P.op("pe", lambda e, tt=tt, i=i: e.transpose(out=misc[:, i * 128:(i + 1) * 128],
                                                                in_=v_[:, tt * 128:(tt + 1) * 128], identity=k.ident[:]),
                         reads=[vt, k.t_const], writes=[tmisc], partial=(i > 0))
                P.op("act", lambda e, g=g: e.activation(out=vm[:, g * 4:(g + 1) * 4, :].rearrange("p a b -> p (a b)"),
                                                        in_=misc[:], func=AF.Copy),
                     reads=[tmisc], writes=[vmt], partial=True)

        def make_blocks(hi):
            kind, h = heads[hi]
            stt = HS[hi]
            qb_, qbt = stt["qb"]
            kb_, kbt = stt["kb"]
            vm, vmt = stt["vm"]
            ydst = stt["ydst"]
            blks = []
            for c in range(NCH):
                qs = slice(c * 512, (c + 1) * 512)
                ops_, opt = o_rot.next()
                dps, dpt = d_rot.next()
                nkt = 4 * c + 4
                for kt in range(nkt):
                    ks = slice(kt * 128, (kt + 1) * 128)
                    sp_, spt = sb_rot.next()
                    p_, ptk, _ = pT.next()
                    diag = kt >= 4 * c
                    di = kt - 4 * c

                    n0 = 128 * di if diag else 0
                    qs2 = slice(c * 512 + n0, (c + 1) * 512)

                    def A(c=c, qs=qs2, kt=kt, ks=ks, sp_=sp_, spt=spt, p_=p_, ptk=ptk, diag=diag, di=di, n0=n0):
                        extra = []
                        if kind == "mla":
                            q2_, q2t = stt["q2"]
                            extra.append((lambda e: e.matmul(sp_[:, n0:], lhsT=k2[:, ks], rhs=q2_[:, qs], start=False,
                                                             stop=True), [t_k2, q2t]))
                        elif kind == "fox":
                            for i3 in range(3):
                                extra.append((lambda e, i3=i3: e.matmul(sp_[:, n0:], lhsT=oh[h][0:H, :],
                                                                        rhs=cs3[i3][:, qs], start=False,
                                                                        stop=(i3 == 2)), [T, TF]))
                        else:
                            sT, sTt = stt["sT"]
                            n = kt // 2
                            extra.append((lambda e: e.matmul(sp_[:, n0:], lhsT=ohm[n][0:65, :], rhs=sT[0:65, qs],
                                                             start=False, stop=True), [T, sTt]))
                        P.op("pe", lambda e: e.matmul(sp_[:, n0:], lhsT=kb_[:, ks], rhs=qb_[:, qs], start=True,
                                                      stop=(len(extra) == 0)), reads=[kbt, qbt], writes=[spt])
                        for fn, rd in extra:
                            P.op("pe", fn, reads=rd, writes=[spt], partial=True)
                        src, srct = sp_, spt
                        if diag:
                            t_, tt_, _ = tmp.next()
                            P.op("dve", lambda e: e.tensor_tensor(out=t_[:, n0:], in0=sp_[:, n0:], in1=negm[di][:, n0:],
                                                                  op=ALU.add),
                                 reads=[spt, T], writes=[tt_])
                            src, srct = t_, tt_
                        if kind == "fox":
                            P.op("act", lambda e: e.activation(out=p_[:, n0:], in_=src[:, n0:], func=AF.Exp,
                                                               bias=cs_tm[:, kt, h:h + 1]),
                                 reads=[srct, TF], writes=[ptk])
                        elif kind == "moba":
                            mb, mbt = stt["mb"]
                            P.op("act", lambda e: e.activation(out=p_[:, n0:], in_=src[:, n0:], func=AF.Exp,
                                                               bias=mb[:, kt:kt + 1]),
                                 reads=[srct, mbt], writes=[ptk])
                        else:
                            P.op("act", lambda e: e.activation(out=p_[:, n0:], in_=src[:, n0:], func=AF.Exp),
                                 reads=[srct], writes=[ptk])

                    def B(c=c, qs=qs, kt=kt, p_=p_, ptk=ptk, nkt=nkt, ops_=ops_, opt=opt, dps=dps, dpt=dpt, n0=n0):
                        P.op("pe", lambda e: e.matmul(ops_[:, n0:], lhsT=vm[:, kt, :], rhs=p_[:, n0:], start=(kt == 0),
                                                      stop=(kt == nkt - 1)),
                             reads=[vmt, ptk], writes=[opt], partial=(kt > 0))
                        P.op("pe", lambda e: e.matmul(dps[:, n0:], lhsT=k.ones_bf[:], rhs=p_[:, n0:], start=(kt == 0),
                                                      stop=(kt == nkt - 1)),
                             reads=[ptk, k.t_const], writes=[dpt], partial=(kt > 0))
                        if kt == nkt - 1:
                            rd, rdt, _ = rden.next()
                            y_, yt, yk = yst.next()
                            ld_, ldt, _ = lnd.next()
                            P.op("act", lambda e: e.activation(out=ld_[:], in_=dps[:], func=AF.Ln), reads=[dpt],
                                 writes=[ldt])
                            P.op("act", lambda e: e.activation(out=rd[:], in_=ld_[:], func=AF.Exp, scale=-1.0),
                                 reads=[ldt], writes=[rdt])
                            P.op("dve", lambda e: e.tensor_tensor(out=y_[:], in0=ops_[:], in1=rd[:], op=ALU.mult),
                                 reads=[opt, rdt], writes=[yt])
                            P.op("sp", lambda e: e.dma_start(out=ydst[:, qs], in_=y_[:]), reads=[yt], writes=[k.t_ycat],
                                 dma_sem=yk, partial=True)

                    blks.append((A, B))
            return blks

        LA = 2
        PE1_AT = 10
        PE2_AT = sum(4 * c + 4 for c in range(NCH - 1))
        pending = []
        fox_prep()
        prep_dma(0)
        prep_pe1(0)
        prep_pe2(0)
        for hi in range(NHD):
            if hi + 1 < NHD:
                prep_dma(hi + 1)
            blks = make_blocks(hi)
            for bi, (A, B) in enumerate(blks):
                if bi == PE1_AT and hi + 1 < NHD:
                    prep_pe1(hi + 1)
                if bi == PE2_AT and hi + 1 < NHD:
                    if heads[hi + 1][0] == "fox" and heads[hi][0] != "fox":
                        fox_prep_pe()
                    prep_pe2(hi + 1)
                A()
                pending.append(B)
                if len(pending) > LA:
                    pending.pop(0)()
        while pending:
            pending.pop(0)()


def phase_gmlp(k, l):
    cfg, P, I = k.cfg, k.P, k.I
    S, H, GW, NCH, NT = cfg.S, cfg.H, cfg.GW, cfg.NCH, cfg.NT
    st, sb = new_phase(k)
    with st:
        lg, lgt = load_vec_fm(k, sb, "lng", I["sgu_ln_g"][l].rearrange("h d -> (h d)"), H)
        lb, lbt = load_vec_fm(k, sb, "lnb", I["sgu_ln_b"][l].rearrange("h d -> (h d)"), H)
        T = Tok("gm")
        io = sb("io", [128, 128])
        m01 = sb("m01", [128, 128])
        P.op("pool", lambda e: e.iota(io[:], pattern=[[1, 128]], base=0, channel_multiplier=-1,
                                      allow_small_or_imprecise_dtypes=True), writes=[T])
        P.op("dve", lambda e: e.tensor_single_scalar(out=m01[:], in_=io[:], scalar=0.0, op=ALU.is_ge), reads=[T],
             writes=[T])
        uf = mkrot(sb, "uf", 3, [128, S], F32, key="hq")
        vf = mkrot(sb, "vf", 3, [128, S], F32, key="hk")
        ws = mkrot(sb, "ws", 3, [128, 128], F32, key="hv")
        bsr = mkrot(sb, "bsr", 3, [1, 128], F32, key="hqb")
        wmT = mkrot(sb, "wmT", 3, [128, 128], BF16)
        bhi = mkrot(sb, "bhi", 3, [1, 128], BF16)
        blo = mkrot(sb, "blo", 3, [1, 128], BF16)
        bfl = mkrot(sb, "bfl", 3, [1, 128], F32)
        vb = mkrot(sb, "vb", 4, [128, 512], BF16)
        sqb = mkrot(sb, "sqb", 4, [128, 512], BF16)
        mean = mkrot(sb, "mean", 4, [128, 512], F32)
        m2 = mkrot(sb, "m2", 4, [128, 512], F32)
        rstd = mkrot(sb, "rstd", 4, [128, 512], F32)
        vn = mkrot(sb, "vn", 8, [128, 512], F32)
        vtm = mkrot(sb, "vtm", 4, [128, 4, 128], BF16)
        yst = mkrot(sb, "yst", 3, [128, 512], F32, key="st")
        b_mean = Rot([(k.ps[0], k.tps[0]), (k.ps[1], k.tps[1])])
        b_msq = Rot([(k.ps[2], k.tps[2]), (k.ps[3], k.tps[3])])
        b_tr = Rot([(k.ps[4], k.tps[4]), (k.ps[5], k.tps[5])])
        b_out = Rot([(k.ps[6], k.tps[6]), (k.ps[7], k.tps[7])])
        HSt = {}

        def loads(g):
            stt = {}
            HSt[g] = stt
            u_, ut, uk = uf.next()
            v_, vt, vk = vf.next()
            w_, wt_, wk = ws.next()
            b_, bt_, bk = bsr.next()
            stt.update(u=(u_, ut), v=(v_, vt), w=(w_, wt_), b=(b_, bt_))
            P.op("sp", lambda e: e.dma_start(out=u_[:], in_=k.projT[cfg.oBu + g * 128:cfg.oBu + (g + 1) * 128, :]),
                 reads=[k.t_proj], writes=[ut], dma_sem=uk)
            P.op("sp", lambda e: e.dma_start(out=v_[:], in_=k.projT[cfg.oBv + g * 128:cfg.oBv + (g + 1) * 128, :]),
                 reads=[k.t_proj], writes=[vt], dma_sem=vk)
            P.op("sp", lambda e: e.dma_start(out=w_[:], in_=I["sgu_w"][l, g]), writes=[wt_], dma_sem=wk)
            P.op("sp", lambda e: e.dma_start(out=b_[:], in_=I["sgu_b"][l, g].rearrange("(o t) -> o t", o=1)),
                 writes=[bt_], dma_sem=bk)
            bh, bht, _ = bhi.next()
            bl, blt, _ = blo.next()
            bf_, bft, _ = bfl.next()
            stt.update(bh=(bh, bht), bl=(bl, blt))
            P.op("dve", lambda e: e.tensor_copy(out=bh[:], in_=b_[:]), reads=[bt_], writes=[bht])
            P.op("dve", lambda e: e.tensor_copy(out=bf_[:], in_=bh[:]), reads=[bht], writes=[bft])
            P.op("dve", lambda e: e.tensor_tensor(out=bf_[:], in0=b_[:], in1=bf_[:], op=ALU.subtract), reads=[bt_, bft],
                 writes=[bft])
            P.op("dve", lambda e: e.tensor_copy(out=bl[:], in_=bf_[:]), reads=[bft], writes=[blt])

        def front(g):
            stt = HSt[g]
            v_, vt = stt["v"]
            w_, wt_ = stt["w"]
            wm, wmt, _ = wmT.next()
            stt["wm"] = (wm, wmt)
            trp, trt = b_tr.next()
            P.op("pe", lambda e: e.transpose(out=trp[:, 0:128], in_=w_[:], identity=k.ident[:]),
                 reads=[wt_, k.t_const], writes=[trt])
            P.op("dve", lambda e: e.tensor_tensor(out=wm[:], in0=trp[:, 0:128], in1=m01[:], op=ALU.mult),
                 reads=[trt, T], writes=[wmt])
            cst = [dict() for _ in range(NCH)]
            stt["c"] = cst
            for c in range(NCH):
                cs = slice(c * 512, (c + 1) * 512)
                vb_, vbt, _ = vb.next()
                sq_, sqt, _ = sqb.next()
                cst[c].update(vb=(vb_, vbt), sq=(sq_, sqt))
                P.op("act", lambda e, vb_=vb_, cs=cs: e.activation(out=vb_[:], in_=v_[:, cs], func=AF.Copy), reads=[vt],
                     writes=[vbt])
                P.op("act", lambda e, sq_=sq_, cs=cs: e.activation(out=sq_[:], in_=v_[:, cs], func=AF.Square), reads=[vt],
                     writes=[sqt])
            for c in range(NCH):
                vb_, vbt = cst[c]["vb"]
                sq_, sqt = cst[c]["sq"]
                pm, pmt = b_mean.next()
                pq, pqt = b_msq.next()
                P.op("pe", lambda e, pm=pm, vb_=vb_: e.matmul(pm[:], lhsT=k.ones_bf[:], rhs=vb_[:], start=True, stop=True),
                     reads=[vbt, k.t_const], writes=[pmt])
                P.op("pe", lambda e, pq=pq, sq_=sq_: e.matmul(pq[:], lhsT=k.ones_bf[:], rhs=sq_[:], start=True, stop=True),
                     reads=[sqt, k.t_const], writes=[pqt])
                mn, mnt, _ = mean.next()
                mm, mmt, _ = m2.next()
                cst[c].update(mn=(mn, mnt), mm=(mm, mmt))
                P.op("act", lambda e, mn=mn, pm=pm: e.activation(out=mn[:], in_=pm[:], func=AF.Copy, scale=1.0 / 128.0),
                     reads=[pmt], writes=[mnt])
                P.op("dve", lambda e, mm=mm, mn=mn: e.tensor_tensor(out=mm[:], in0=mn[:], in1=mn[:], op=ALU.mult),
                     reads=[mnt], writes=[mmt])
                P.op("dve", lambda e, mm=mm, pq=pq: e.scalar_tensor_tensor(out=mm[:], in0=pq[:], scalar=1.0 / 128.0,
                                                                           in1=mm[:], op0=ALU.mult, op1=ALU.subtract),
                     reads=[pqt, mmt], writes=[mmt])

        def front2(g):
            stt = HSt[g]
            v_, vt = stt["v"]
            cst = stt["c"]
            for c in range(NCH):
                mm, mmt = cst[c]["mm"]
                rs, rst, _ = rstd.next()
                cst[c]["rs"] = (rs, rst)
                P.op("act", lambda e, rs=rs, mm=mm: e.activation(out=rs[:], in_=mm[:], func=AF.Ln, bias=EPS), reads=[mmt],
                     writes=[rst])
            for c in range(NCH):
                rs, rst = cst[c]["rs"]
                P.op("act", lambda e, rs=rs: e.activation(out=rs[:], in_=rs[:], func=AF.Exp, scale=-0.5), reads=[rst],
                     writes=[rst])
            for c in range(NCH):
                cs = slice(c * 512, (c + 1) * 512)
                mn, mnt = cst[c]["mn"]
                rs, rst = cst[c]["rs"]
                vn_, vnt, _ = vn.next()
                cst[c]["vn"] = (vn_, vnt)
                P.op("dve", lambda e, vn_=vn_, cs=cs, mn=mn: e.tensor_tensor(out=vn_[:], in0=v_[:, cs], in1=mn[:],
                                                                             op=ALU.subtract),
                     reads=[vt, mnt], writes=[vnt])
                P.op("dve", lambda e, vn_=vn_, rs=rs: e.tensor_tensor(out=vn_[:], in0=vn_[:], in1=rs[:], op=ALU.mult),
                     reads=[vnt, rst], writes=[vnt])
                P.op("dve", lambda e, vn_=vn_: e.tensor_scalar(out=vn_[:], in0=vn_[:], scalar1=lg[:, g:g + 1],
                                                               scalar2=lb[:, g:g + 1], op0=ALU.mult, op1=ALU.add),
                     reads=[vnt, lgt, lbt], writes=[vnt])

        def back(g):
            stt = HSt[g]
            u_, ut = stt["u"]
            bh, bht = stt["bh"]
            bl, blt = stt["bl"]
            wm, wmt = stt["wm"]
            cst = stt["c"]
            for c in range(NCH):
                vn_, vnt = cst[c]["vn"]
                trp, trt = b_tr.next()
                for i in range(4):
                    P.op("pe", lambda e, trp=trp, vn_=vn_, i=i: e.transpose(out=trp[:, i * 128:(i + 1) * 128],
                                                                           in_=vn_[:, i * 128:(i + 1) * 128],
                                                                           identity=k.ident[:]),
                         reads=[vnt, k.t_const], writes=[trt], partial=(i > 0))
                vm, vmt, _ = vtm.next()
                cst[c]["vm"] = (vm, vmt)
                P.op("act", lambda e, vm=vm, trp=trp: e.activation(out=vm[:].rearrange("p a b -> p (a b)"), in_=trp[:],
                                                                  func=AF.Copy), reads=[trt], writes=[vmt])

        def back2(g):
            stt = HSt[g]
            u_, ut = stt["u"]
            bh, bht = stt["bh"]
            bl, blt = stt["bl"]
            wm, wmt = stt["wm"]
            cst = stt["c"]
            for c in range(NCH):
                cs = slice(c * 512, (c + 1) * 512)
                vm, vmt = cst[c]["vm"]
                po, pot = b_out.next()
                for i in range(4):
                    P.op("pe", lambda e, po=po, vm=vm, i=i: e.matmul(po[:, i * 128:(i + 1) * 128], lhsT=vm[:, i, :],
                                                                    rhs=wm[:], start=True, stop=False),
                         reads=[vmt, wmt], writes=[pot], partial=(i > 0))
                    P.op("pe", lambda e, po=po, i=i: e.matmul(po[:, i * 128:(i + 1) * 128], lhsT=k.ones_bf[0:1, :],
                                                             rhs=bh[0:1, :], start=False, stop=False),
                         reads=[bht, k.t_const], writes=[pot], partial=True)
                    P.op("pe", lambda e, po=po, i=i: e.matmul(po[:, i * 128:(i + 1) * 128], lhsT=k.ones_bf[0:1, :],
                                                             rhs=bl[0:1, :], start=False, stop=True),
                         reads=[blt, k.t_const], writes=[pot], partial=True)
                y_, yt, yk = yst.next()
                P.op("dve", lambda e, y_=y_, po=po, cs=cs: e.tensor_tensor(out=y_[:], in0=po[:], in1=u_[:, cs], op=ALU.mult),
                     reads=[pot, ut], writes=[yt])
                P.op("sp", lambda e, y_=y_, cs=cs: e.dma_start(out=k.ycatT[GW + g * 128:GW + (g + 1) * 128, cs], in_=y_[:]),
                     reads=[yt], writes=[k.t_ycat], dma_sem=yk, partial=True)

        loads(0)
        if H > 1:
            loads(1)
        front(0)
        front2(0)
        for g in range(H):
            if g + 2 < H:
                loads(g + 2)
            if g + 1 < H:
                front(g + 1)
            back(g)
            if g + 1 < H:
                front2(g + 1)
            back2(g)


def phase_wo(k, l):
    cfg, P, I = k.cfg, k.P, k.I
    S, D, H, GW, NCH, KC = cfg.S, cfg.D, cfg.H, cfg.GW, cfg.NCH, cfg.KC
    KM = cfg.DMIX // 128
    st, sb = new_phase(k)
    with st:
        ynT = sb("ynT", [128, KM, S], BF16)
        t_yn = Tok("ynT")
        gg, ggt = load_vec_fm(k, sb, "gg", I["group_norm_g"][l].rearrange("a b -> (a b)"), KM)
        for gi in range(4):
            with ExitStack() as st2:
                def sb2(name, shape, dt=F32, st2=st2, gi=gi):
                    return st2.enter_context(k.nc.sbuf_tensor("gn_%d_%d_%s" % (l, gi, name), list(shape), dt))
                rms_norm_fm(k, sb2, lambda g0, n, c, gi=gi: (k.ycatT[(gi * H + g0) * 128:(gi * H + g0 + n) * 128,
                                                                    c * 512:(c + 1) * 512].rearrange(
                    "(g p) t -> p g t", p=128), [k.t_ycat]), H, GW,
                            gg[:, gi * H:(gi + 1) * H], ggt,
                            lambda kc, c, gi=gi: (ynT[:, gi * H + kc, c * 512:(c + 1) * 512], t_yn), NCH, tagp="g%d" % gi)
                P.barrier()
        wrot = mkrot(sb, "w", 3, [128, KM, 128], BF16)
        banks = Rot([(k.ps[i], k.tps[i]) for i in range(4)])
        ssq = SumSq(k, sb, lambda c: (k.ps[4 + c], k.tps[4 + c]), KC)
        ev = make_resid_evac(k, sb, lambda c: c * 512, ssq)
        tiles = [([(j * 128, 128)], 128) for j in range(KC)]
        linear(k, I["w_o"][l], KM, tiles, lambda kc, c: (ynT[:, kc, c * 512:(c + 1) * 512], t_yn), NCH, ev, wrot, banks)
        ssq.flush()
        ssq_finish(k, ssq)


def phase_ffn_up(k, l):
    cfg, P, I = k.cfg, k.P, k.I
    S, D, NCH, KC, NF = cfg.S, cfg.D, cfg.NCH, cfg.KC, cfg.NF
    st, sb = new_phase(k)
    with st:
        hT = sb("h2T", [128, KC, S], BF16)
        t_h = Tok("h2T")
        gT, gtok = load_vec_fm(k, sb, "g2", I["norm_ffn_g"][l], KC)
        with ExitStack() as st2:
            def sb2(name, shape, dt=F32):
                return st2.enter_context(k.nc.sbuf_tensor("n2_%d_%s" % (l, name), list(shape), dt))
            rms_norm_fm(k, sb2, lambda g0, n, c: (k.xT[g0 * 128:(g0 + n) * 128, c * 512:(c + 1) * 512].rearrange("(g p) t -> p g t", p=128), [k.t_xT[g0 + i] for i in range(n)]), KC, D,
                        gT, gtok, lambda kc, c: (hT[:, kc, c * 512:(c + 1) * 512], t_h), NCH, rstd_pre=True)
            P.barrier()
        cw = []
        for j in range(3):
            cw.append(load_vec_fm(k, sb, "cw%d" % j, I["conv_w"][l, j], NF))
        cb, cbt = load_vec_fm(k, sb, "cb", I["conv_b"][l], NF)
        wrot = mkrot(sb, "w", 4, [128, KC, 128], BF16)
        bg = Rot([(k.ps[i], k.tps[i]) for i in range(0, 3)])
        bv = Rot([(k.ps[i], k.tps[i]) for i in range(3, 6)])
        gbuf = mkrot(sb, "gbuf", 2, [128, S + 2], F32)
        for gb, gbt, _ in gbuf.items:
            P.op("dve", lambda e, gb=gb: e.memset(gb[:, 0:2], 0.0), writes=[gbt])
        t1 = mkrot(sb, "t1", 2, [128, 512], F32)
        ast = mkrot(sb, "ast", 3, [128, 512], BF16, key="st")
        wvg = I["w_gate"][l].rearrange("(kc p) n -> p kc n", p=128)
        wvv = I["w_val"][l].rearrange("(kc p) n -> p kc n", p=128)
        for f in range(NF):
            wg, wgt, wgk = wrot.next()
            wv_, wvt, wvk = wrot.next()
            fs = slice(f * 128, (f + 1) * 128)
            P.op("pool", lambda e, wg=wg, fs=fs: e.dma_start(out=wg[:], in_=wvg[:, :, fs]), writes=[wgt], dma_sem=wgk)
            P.op("pool", lambda e, wv_=wv_, fs=fs: e.dma_start(out=wv_[:], in_=wvv[:, :, fs]), writes=[wvt], dma_sem=wvk)
            gb, gbt, _ = gbuf.next()
            for c in range(NCH):
                cs = slice(c * 512, (c + 1) * 512)
                pg, pgt = bg.next()
                pv, pvt = bv.next()
                for kc in range(KC):
                    P.op("pe", lambda e, pg=pg, wg=wg, kc=kc, cs=cs: e.matmul(pg[:], lhsT=wg[:, kc, :], rhs=hT[:, kc, cs],
                                                                             start=(kc == 0), stop=(kc == KC - 1)),
                         reads=[wgt, t_h], writes=[pgt])
                for kc in range(KC):
                    P.op("pe", lambda e, pv=pv, wv_=wv_, kc=kc, cs=cs: e.matmul(pv[:], lhsT=wv_[:, kc, :], rhs=hT[:, kc, cs],
                                                                               start=(kc == 0), stop=(kc == KC - 1)),
                         reads=[wvt, t_h], writes=[pvt])
                c0 = c * 512
                P.op("act", lambda e, gb=gb, pg=pg, c0=c0: e.activation(out=gb[:, 2 + c0:2 + c0 + 512], in_=pg[:],
                                                                       func=AF.Copy), reads=[pgt], writes=[gbt], partial=True)
                t_, tt_, _ = t1.next()
                P.op("dve", lambda e, t_=t_, gb=gb, c0=c0, f=f: e.tensor_scalar(
                    out=t_[:], in0=gb[:, 2 + c0:2 + c0 + 512], scalar1=cw[2][0][:, f:f + 1], scalar2=cb[:, f:f + 1],
                    op0=ALU.mult, op1=ALU.add), reads=[gbt, cw[2][1], cbt], writes=[tt_])
                P.op("dve", lambda e, t_=t_, gb=gb, c0=c0, f=f: e.scalar_tensor_tensor(
                    out=t_[:], in0=gb[:, 1 + c0:1 + c0 + 512], scalar=cw[1][0][:, f:f + 1], in1=t_[:], op0=ALU.mult,
                    op1=ALU.add), reads=[gbt, cw[1][1], tt_], writes=[tt_])
                P.op("dve", lambda e, t_=t_, gb=gb, c0=c0, f=f: e.scalar_tensor_tensor(
                    out=t_[:], in0=gb[:, c0:c0 + 512], scalar=cw[0][0][:, f:f + 1], in1=t_[:], op0=ALU.mult,
                    op1=ALU.add), reads=[gbt, cw[0][1], tt_], writes=[tt_])
                P.op("act", lambda e, t_=t_: e.activation(out=t_[:], in_=t_[:], func=AF.Silu), reads=[tt_], writes=[tt_])
                a_, at_, ak = ast.next()
                P.op("dve", lambda e, a_=a_, t_=t_, pv=pv: e.tensor_tensor(out=a_[:], in0=pv[:], in1=t_[:], op=ALU.mult),
                     reads=[pvt, tt_], writes=[at_])
                P.op("sp", lambda e, a_=a_, fs=fs, cs=cs: e.dma_start(out=k.aT[fs, cs], in_=a_[:]), reads=[at_],
                     writes=[k.t_aT], dma_sem=ak, partial=True)


def phase_ffn_down(k, l):
    cfg, P, I = k.cfg, k.P, k.I
    S, D, NCH, KC, NF = cfg.S, cfg.D, cfg.NCH, cfg.KC, cfg.NF
    st, sb = new_phase(k)
    with st:
        aTc = sb("aTc", [128, NF, 512], BF16)
        nsp = 6
        per = (NF + nsp - 1) // nsp
        t_ap = [Tok("aTc%d" % i) for i in range(nsp)]
        wrot = mkrot(sb, "w", 3, [128, NF, 128], BF16)
        banks = Rot([(k.ps[i], k.tps[i]) for i in range(6)])
        tiles = [([(j * 128, 128)], 128) for j in range(KC)]
        cur_c = [0]
        ssq = SumSq(k, sb, lambda c: (k.ps[6 + c % 2], k.tps[6 + c % 2]), KC)
        ev = make_resid_evac(k, sb, lambda cc: cur_c[0] * 512, ssq)
        for c in range(NCH):
            src = k.aT[:, c * 512:(c + 1) * 512].rearrange("(f p) t -> p f t", p=128)
            for i in range(nsp):
                f0, f1 = i * per, min(NF, (i + 1) * per)
                if f0 >= f1:
                    continue
                P.op("sp" if i % 2 == 0 else "pool",
                     lambda e, f0=f0, f1=f1, src=src: e.dma_start(out=aTc[:, f0:f1, :], in_=src[:, f0:f1, :]),
                     reads=[k.t_aT], writes=[t_ap[i]], dma_sem="aTc%d" % i)
            cur_c[0] = c
            linear(k, None, NF, tiles, lambda kc, cc: (aTc[:, kc, :], t_ap[kc // per]), 1, ev, wrot, banks,
                   wsrc=lambda j: k.wdb[j].rearrange("p (kc n) -> p kc n", n=128), wq="act", wreads=[k.t_wdb])
            ssq.flush()
        ssq_finish(k, ssq)


def make_resid_evac_c(k, sb, c):
    if not hasattr(k, "_dn_rot") or k._dn_phase != k.cnt:
        k._dn_phase = k.cnt
        k._dn_rot = (mkrot(sb, "xr", 3, [128, 512], F32), mkrot(sb, "xo", 3, [128, 512], F32))
    xr, xo = k._dn_rot
    P = k.P

    def ev(j, cc, ps, pt, M):
        dst = k.xT[j * 128:(j + 1) * 128, c * 512:(c + 1) * 512]
        r, rt, rk = xr.next()
        o, ot, ok = xo.next()
        P.op("sp", lambda e: e.dma_start(out=r[:], in_=dst), reads=[k.t_xT[j]], writes=[rt], dma_sem=rk)
        P.op("dve", lambda e: e.tensor_tensor(out=o[:], in0=ps[:], in1=r[:], op=ALU.add), reads=[pt, rt], writes=[ot])
        P.op("sp", lambda e: e.dma_start(out=dst, in_=o[:]), reads=[ot], writes=[k.t_xT[j]], dma_sem=ok, partial=True)

    return ev


def phase_final(k):
    cfg, P, I = k.cfg, k.P, k.I
    S, D, NCH, KC = cfg.S, cfg.D, cfg.NCH, cfg.KC
    st, sb = new_phase(k)
    with st:
        gT, gtok = load_vec_fm(k, sb, "gf", I["final_norm_g"], KC)
        G = min(4, KC)
        xr = mkrot(sb, "nx", 3, [128, G, 512], F32)
        sq = mkrot(sb, "nsq", 3, [128, 512], BF16)
        rs = mkrot(sb, "nrs", 2, [128, 512], F32)
        yn = mkrot(sb, "yn", 3, [128, 512], F32)
        ost = mkrot(sb, "ost", 4, [128, 4, 128], F32, key="st")
        sbank = Rot([(k.ps[6], k.tps[6]), (k.ps[7], k.tps[7])])
        tbank = Rot([(k.ps[i], k.tps[i]) for i in range(6)])
        groups = [(g0, min(G, KC - g0)) for g0 in range(0, KC, G)]

        def src_of(g0, n, c):
            return k.xT[g0 * 128:(g0 + n) * 128, c * 512:(c + 1) * 512].rearrange("(g p) t -> p g t", p=128)

        cnt = 0
        for c in range(NCH):
            r, rt = k.rk[:, c, :], k.t_rk
            for (g0, n) in groups:
                x, xt, xk = xr.next()
                src = src_of(g0, n, c)
                P.op("sp", lambda e, x=x, src=src, n=n: e.dma_start(out=x[:, 0:n, :], in_=src),
                     reads=[k.t_xT[g0 + i] for i in range(n)], writes=[xt], dma_sem=xk)
                for i in range(n):
                    kc = g0 + i
                    y, yt, _ = yn.next()
                    P.op("dve", lambda e, x=x, y=y, kc=kc, r=r, i=i: e.scalar_tensor_tensor(
                        out=y[:], in0=x[:, i, :], scalar=gT[:, kc:kc + 1], in1=r, op0=ALU.mult, op1=ALU.mult),
                         reads=[xt, rt, gtok], writes=[yt])
                    tp, tpt = tbank.next()
                    for q in range(4):
                        P.op("pe", lambda e, tp=tp, y=y, q=q: e.transpose(out=tp[:, q * 128:(q + 1) * 128],
                                                                         in_=y[:, q * 128:(q + 1) * 128],
                                                                         identity=k.ident[:]),
                             reads=[yt, k.t_const], writes=[tpt], partial=(q > 0))
                    o, ot, ok = ost.next()
                    cnt += 1
                    if cnt % 3 == 0:
                        P.op("dve", lambda e, o=o, tp=tp: e.tensor_copy(out=o[:].rearrange("p a b -> p (a b)"), in_=tp[:]),
                             reads=[tpt], writes=[ot])
                    else:
                        P.op("act", lambda e, o=o, tp=tp: e.activation(out=o[:].rearrange("p a b -> p (a b)"), in_=tp[:],
                                                                      func=AF.Copy), reads=[tpt], writes=[ot])
                    dst = k.out[c * 512:(c + 1) * 512, kc * 128:(kc + 1) * 128].rearrange("(i p) f -> p i f", p=128)
                    P.op("act", lambda e, dst=dst, o=o: e.dma_start(out=dst, in_=o[:]), reads=[ot], writes=[k.t_out],
                         dma_sem=ok, partial=True)


_NC_CACHE = {}


def kernel(**inputs):
    cfg = Cfg()
    if "nc" not in _NC_CACHE:
        _NC_CACHE["nc"] = build_program(cfg)
    nc = _NC_CACHE["nc"]
    x = np.ascontiguousarray(inputs["x"], dtype=np.float32)
    B = x.shape[0]
    shared = {n: np.ascontiguousarray(inputs[n], dtype=np.float32) for n in inputs if n != "x"}
    in_maps = []
    for b in range(B):
        m = dict(shared)
        m["x"] = x[b]
        in_maps.append(m)
    res = run_bass_kernel_spmd(nc, in_maps, core_ids=list(range(B)))
    return np.stack([r["out"] for r in res.results], axis=0).astype(np.float32)
```
